# Optimizing a Trainium2 kernel written in Bass

```python
import jax, jax.numpy as jnp
from jax import lax
import numpy as np

D_MODEL = 1024
BATCH = 8
SEQ = 4096
DEPTH = 1

GRID_W = 64
CTX_LEN = 256
D_RWKV = 512
D_CONV = 512
RWKV_HEAD = 64
RWKV_HEADS = D_RWKV // RWKV_HEAD
CONV_WIDTH = 3
LORA_W = 64
LORA_A = 64
LORA_G = 128
N_EXPERTS = 16
EC_CAPACITY = 2
D_EXPERT = 1024
NORM_EPS = 1e-6
GN_EPS = 64e-5
RWKV_COLS = 3 * D_RWKV + LORA_W + LORA_A + LORA_G
CONV_COLS = 3 * D_CONV
D_IN = RWKV_COLS + CONV_COLS

kernel_name = "hybrid_rwkv7_shortconv_ecmoe_dit_block"


def rmsnorm(x, g):
    xf = x.astype(jnp.float32)
    y = xf * lax.rsqrt(jnp.mean(xf * xf, axis=-1, keepdims=True) + NORM_EPS)
    return (y * g.astype(jnp.float32)).astype(x.dtype)


def modulate(h, shift, scale):
    return h * (1 + scale) + shift


def grid_shift(p, rows):
    b, t, ch = p.shape
    q = p.reshape(b, rows, GRID_W, ch // 4, 4)
    zc = jnp.zeros_like(q[:, :, :1, :, 0])
    zr = jnp.zeros_like(q[:, :1, :, :, 0])
    left = jnp.concatenate([zc, q[:, :, :-1, :, 0]], axis=2)
    right = jnp.concatenate([q[:, :, 1:, :, 1], zc], axis=2)
    up = jnp.concatenate([zr, q[:, :-1, :, :, 2]], axis=1)
    down = jnp.concatenate([q[:, 1:, :, :, 3], zr], axis=1)
    return jnp.stack([left, right, up, down], axis=-1).reshape(b, t, ch)


def seq_shift(p):
    b, t, ch = p.shape
    q = p.reshape(b, t, ch // 2, 2)
    z = jnp.zeros_like(q[:, :1, :, 0])
    prev = jnp.concatenate([z, q[:, :-1, :, 0]], axis=1)
    nxt = jnp.concatenate([q[:, 1:, :, 1], z], axis=1)
    return jnp.stack([prev, nxt], axis=-1).reshape(b, t, ch)


def split_rwkv(p, mu, shifted):
    p = p + mu * (shifted - p)
    o = 3 * D_RWKV
    return jnp.split(p, [D_RWKV, 2 * D_RWKV, o, o + LORA_W, o + LORA_W + LORA_A], axis=-1)


def wkv_scan(s0, r, w, k, v, a, b, reverse):
    def step(s, inp):
        r_t, w_t, k_t, v_t, a_t, b_t = inp
        sa = jnp.einsum('bhvk,bhk->bhv', s, a_t)
        s = s * w_t[:, :, None, :] + sa[..., :, None] * b_t[:, :, None, :] + v_t[..., :, None] * k_t[:, :, None, :]
        return s, jnp.einsum('bhvk,bhk->bhv', s, r_t)
    xs = tuple(jnp.moveaxis(z, 1, 0) for z in (r, w, k, v, a, b))
    s, y = lax.scan(step, s0, xs, reverse=reverse)
    return jnp.moveaxis(y, 0, 1), s


def rwkv_bidir(xr, xk, xv, xw, xa, w0, w_up, a0, a_up, k_k, k_a, s0_f, s0_b):
    bsz, t = xr.shape[0], xr.shape[1]
    f32 = jnp.float32
    heads = lambda z: z.astype(f32).reshape(bsz, t, RWKV_HEADS, RWKV_HEAD)
    per_head = lambda z: z.astype(f32).reshape(RWKV_HEADS, RWKV_HEAD)
    r, k, v = heads(xr), heads(xk), heads(xv)
    kk = k * per_head(k_k)
    kk = kk / jnp.maximum(jnp.sqrt(jnp.sum(kk * kk, axis=-1, keepdims=True)), 1e-12)
    tw = jnp.tanh(xw.astype(f32))
    xa = xa.astype(f32)
    k_a_h = per_head(k_a)
    y_sum, k_sum, states = 0.0, 0.0, []
    for d, (rev, s0) in enumerate(((False, s0_f), (True, s0_b))):
        wl = -jax.nn.softplus(-(w0[d].astype(f32) + tw @ w_up[d].astype(f32))) - 0.5
        decay = jnp.exp(-jnp.exp(heads(wl)))
        a = heads(jax.nn.sigmoid(a0[d].astype(f32) + xa @ a_up[d].astype(f32)))
        kd = k * (1 + (a - 1) * k_a_h)
        y, s = wkv_scan(s0, r, decay, kd, v, -kk, kk * a, rev)
        y_sum = y_sum + y
        k_sum = k_sum + kd
        states.append(s)
    return y_sum, states[0], states[1], r, k_sum, v


def rwkv_out(y, r, k_sum, v, xg, g_up, r_k, gn_g, gn_b):
    bsz, t = y.shape[0], y.shape[1]
    f32 = jnp.float32
    mu = jnp.mean(y, axis=-1, keepdims=True)
    var = jnp.mean(jnp.square(y - mu), axis=-1, keepdims=True)
    yn = ((y - mu) * lax.rsqrt(var + GN_EPS)).reshape(bsz, t, D_RWKV) * gn_g.astype(f32) + gn_b.astype(f32)
    bonus = (jnp.sum(r * k_sum * r_k.astype(f32), axis=-1, keepdims=True) * v).reshape(bsz, t, D_RWKV)
    g = jax.nn.sigmoid(xg.astype(f32)) @ g_up.astype(f32)
    return (yn + bonus) * g


def dwconv3(u, w):
    return lax.conv_general_dilated(u, w[:, None, :].astype(u.dtype), window_strides=(1,), padding=((1, 1),),
                                    dimension_numbers=('NWC', 'WIO', 'NWC'), feature_group_count=u.shape[-1])


def short_conv(p, conv_w):
    b_gate, c_gate, u = jnp.split(p, 3, axis=-1)
    return b_gate * dwconv3(c_gate * u, conv_w)


def expert_choice_ffn(h, router_w, w_gate, w_up, w_down):
    bsz, t, d = h.shape
    cap = EC_CAPACITY * t // N_EXPERTS
    aff = jax.nn.softmax((h @ router_w).astype(jnp.float32), axis=-1)
    vals, idx = lax.top_k(jnp.swapaxes(aff, 1, 2), cap)
    xs = jax.vmap(lambda hb, ib: hb[ib])(h, idx)
    hid = jax.nn.silu(jnp.einsum('becd,edf->becf', xs, w_gate)) * jnp.einsum('becd,edf->becf', xs, w_up)
    out = jnp.einsum('becf,efd->becd', hid, w_down) * vals[..., None].astype(h.dtype)
    return jax.vmap(lambda ob, ib: jnp.zeros((t, d), h.dtype).at[ib.reshape(-1)].add(ob.reshape(-1, d)))(out, idx)


def setup_inputs(seed: int = 0) -> dict:
    key = jax.random.key(seed)
    ks = jax.random.split(key, 28)
    L = DEPTH
    nrm = lambda k, shape, scale: jax.random.normal(k, shape, jnp.float32) * scale
    return {
        'x': nrm(ks[0], (BATCH, SEQ, D_MODEL), 1.0),
        'c': nrm(ks[1], (BATCH, D_MODEL), 1.0),
        'ctx': nrm(ks[2], (BATCH, CTX_LEN, D_MODEL), 1.0),
        'c_ctx': nrm(ks[3], (D_MODEL,), 1.0),
        'ada_w': nrm(ks[4], (L, D_MODEL, 6 * D_MODEL), 0.5 * D_MODEL ** -0.5),
        'ada_b': nrm(ks[5], (L, 6 * D_MODEL), 0.02),
        'norm1_g': 1.0 + nrm(ks[6], (L, D_MODEL), 0.02),
        'norm2_g': 1.0 + nrm(ks[7], (L, D_MODEL), 0.02),
        'w_in': nrm(ks[8], (L, D_MODEL, D_IN), D_MODEL ** -0.5),
        'shift_mu': jax.random.uniform(ks[9], (L, RWKV_COLS), jnp.float32),
        'w0': jax.random.uniform(ks[10], (L, 2, D_RWKV), jnp.float32, minval=-6.0, maxval=1.0),
        'w_lora_up': nrm(ks[11], (L, 2, LORA_W, D_RWKV), 0.1 * LORA_W ** -0.5),
        'a0': nrm(ks[12], (L, 2, D_RWKV), 0.5),
        'a_lora_up': nrm(ks[13], (L, 2, LORA_A, D_RWKV), LORA_A ** -0.5),
        'k_k': 0.85 + nrm(ks[14], (L, D_RWKV), 0.05),
        'k_a': 1.0 + nrm(ks[15], (L, D_RWKV), 0.05),
        'r_k': nrm(ks[16], (L, RWKV_HEADS, RWKV_HEAD), 0.1),
        'g_lora_up': nrm(ks[17], (L, LORA_G, D_RWKV), LORA_G ** -0.5),
        'gn_g': 1.0 + nrm(ks[18], (L, D_RWKV), 0.02),
        'gn_b': nrm(ks[19], (L, D_RWKV), 0.02),
        'conv_w': nrm(ks[20], (L, CONV_WIDTH, D_CONV), CONV_WIDTH ** -0.5),
        'w_out': nrm(ks[21], (L, D_RWKV + D_CONV, D_MODEL), (D_RWKV + D_CONV) ** -0.5),
        'router_w': nrm(ks[22], (L, D_MODEL, N_EXPERTS), D_MODEL ** -0.5),
        'exp_w_gate': nrm(ks[23], (L, N_EXPERTS, D_MODEL, D_EXPERT), D_MODEL ** -0.5),
        'exp_w_up': nrm(ks[24], (L, N_EXPERTS, D_MODEL, D_EXPERT), D_MODEL ** -0.5),
        'exp_w_down': nrm(ks[25], (L, N_EXPERTS, D_EXPERT, D_MODEL), D_EXPERT ** -0.5),
        'final_g': 1.0 + nrm(ks[26], (D_MODEL,), 0.02),
    }


def reference(x, c, ctx, c_ctx, ada_w, ada_b, norm1_g, norm2_g, w_in, shift_mu, w0, w_lora_up, a0, a_lora_up,
              k_k, k_a, r_k, g_lora_up, gn_g, gn_b, conv_w, w_out, router_w, exp_w_gate, exp_w_up, exp_w_down,
              final_g):
    bsz, t, _ = x.shape
    rows = t // GRID_W
    s_zero = jnp.zeros((bsz, RWKV_HEADS, RWKV_HEAD, RWKV_HEAD), jnp.float32)
    for l in range(DEPTH):
        last = l == DEPTH - 1
        mod = jax.nn.silu(c) @ ada_w[l] + ada_b[l]
        mod_c = jax.nn.silu(c_ctx) @ ada_w[l] + ada_b[l]
        sh1, sc1, g1, sh2, sc2, g2 = jnp.split(mod[:, None, :], 6, axis=-1)
        csh1, csc1, cg1, csh2, csc2, cg2 = jnp.split(mod_c, 6, axis=-1)

        pc = modulate(rmsnorm(ctx, norm1_g[l]), csh1, csc1) @ w_in[l]
        px = modulate(rmsnorm(x, norm1_g[l]), sh1, sc1) @ w_in[l]
        pc_rw, pc_cv = pc[..., :RWKV_COLS], pc[..., RWKV_COLS:]
        px_rw, px_cv = px[..., :RWKV_COLS], px[..., RWKV_COLS:]

        cr, ck, cv, cw, ca, cgl = split_rwkv(pc_rw, shift_mu[l], seq_shift(pc_rw))
        xr, xk, xv, xw, xa, xgl = split_rwkv(px_rw, shift_mu[l], grid_shift(px_rw, rows))

        yc, sf, sb, rc, kc, vc = rwkv_bidir(cr, ck, cv, cw, ca, w0[l], w_lora_up[l], a0[l], a_lora_up[l],
                                            k_k[l], k_a[l], s_zero, s_zero)
        yx, _, _, rx, kx, vx = rwkv_bidir(xr, xk, xv, xw, xa, w0[l], w_lora_up[l], a0[l], a_lora_up[l],
                                          k_k[l], k_a[l], sf, sb)
        a_x = rwkv_out(yx, rx, kx, vx, xgl, g_lora_up[l], r_k[l], gn_g[l], gn_b[l]).astype(x.dtype)
        b_x = short_conv(px_cv, conv_w[l])
        x = x + g1 * (jnp.concatenate([a_x, b_x], axis=-1) @ w_out[l])
        if not last:
            a_c = rwkv_out(yc, rc, kc, vc, cgl, g_lora_up[l], r_k[l], gn_g[l], gn_b[l]).astype(ctx.dtype)
            b_c = short_conv(pc_cv, conv_w[l])
            ctx = ctx + cg1 * (jnp.concatenate([a_c, b_c], axis=-1) @ w_out[l])

        hx = modulate(rmsnorm(x, norm2_g[l]), sh2, sc2)
        x = x + g2 * expert_choice_ffn(hx, router_w[l], exp_w_gate[l], exp_w_up[l], exp_w_down[l])
        if not last:
            hc = modulate(rmsnorm(ctx, norm2_g[l]), csh2, csc2)
            ctx = ctx + cg2 * expert_choice_ffn(hc, router_w[l], exp_w_gate[l], exp_w_up[l], exp_w_down[l])
    return rmsnorm(x, final_g)
```

```python
import numpy as np
import ml_dtypes
from contextlib import ExitStack
import concourse.bass as bass
import concourse.mybir as mybir
from concourse.bass_utils import run_bass_kernel_spmd

F32 = mybir.dt.float32
BF16 = mybir.dt.bfloat16
I32 = mybir.dt.int32
U32 = mybir.dt.uint32
AF = mybir.ActivationFunctionType
ALU = mybir.AluOpType
AX = mybir.AxisListType

COMPUTE = ("pe", "act", "dve", "pool")
QUEUES = ("sp", "act", "pool")
NDMA_SEM = 8


class T:
    def __init__(self, handle, name):
        self.h = handle
        self.name = name
        self.st = {}

    def __getitem__(self, k):
        return self.h[k]


class Op:
    __slots__ = ("eng", "fn", "deps", "is_dma", "sig", "signo", "dsem", "dval", "idx", "bar")

    def __init__(self, eng, fn, is_dma):
        self.eng = eng
        self.fn = fn
        self.deps = []
        self.is_dma = is_dma
        self.sig = False
        self.signo = None
        self.dsem = None
        self.dval = None
        self.bar = False


class Prog:
    def __init__(self, nc):
        self.nc = nc
        self.ops = []
        self.ntile = 0
        self.stacks = []
        self.defer = None

    def sb(self, shape, dtype, name="t"):
        self.ntile += 1
        if self.stacks:
            h = self.stacks[-1].enter_context(self.nc.sbuf_tensor(f"{name}_{self.ntile}", list(shape), dtype))
        else:
            h = self.nc.alloc_sbuf_tensor(f"{name}_{self.ntile}", list(shape), dtype)
        return T(h, name)

    def scope(self):
        prog = self

        class _Scope:
            def __enter__(s):
                prog.stacks.append(ExitStack())

            def __exit__(s, *a):
                prog.barrier()
                prog.stacks.pop().close()
                return False
        return _Scope()

    def push(self):
        self.stacks.append(ExitStack())

    def pop(self):
        self.barrier()
        self.stacks.pop().close()

    def barrier(self):
        o = Op(None, None, False)
        o.bar = True
        self.ops.append(o)

    def ps(self, shape, dtype=F32, name="p"):
        self.ntile += 1
        return T(self.nc.alloc_psum_tensor(f"{name}_{self.ntile}", list(shape), dtype), name)

    def dram(self, name, shape, dtype, kind="Internal"):
        h = self.nc.dram_tensor(name, list(shape), dtype, kind=kind)
        t = T(h, name)
        t.a = h.ap()
        return t

    @staticmethod
    def _norm(acc):
        return [(a, None) if isinstance(a, T) else a for a in acc]

    def replay(self, lst):
        for (eng, fn, reads, writes, dma) in lst:
            self.op(eng, fn, reads, writes, dma)

    def op(self, eng, fn, reads=(), writes=(), dma=False):
        if self.defer is not None:
            self.defer.append((eng, fn, reads, writes, dma))
            return None
        o = Op(eng, fn, dma)
        deps = set()

        def matching(t, key):
            if key is None:
                return list(t.st.values())
            res = []
            if key in t.st:
                res.append(t.st[key])
            if None in t.st:
                res.append(t.st[None])
            return res

        reads = self._norm(reads)
        writes = self._norm(writes)
        for (t, key) in reads:
            for ent in matching(t, key):
                if ent[0] is not None:
                    deps.add(ent[0])
        for (t, key) in writes:
            for ent in matching(t, key):
                if ent[0] is not None:
                    deps.add(ent[0])
                deps.update(ent[1].values())
                deps.update(ent[2])
        for (t, key) in reads:
            ent = t.st.setdefault(key, [None, {}, []])
            if dma:
                ent[2].append(o)
            else:
                ent[1][eng] = o
        for (t, key) in writes:
            if key is None:
                t.st = {None: [o, {}, []]}
            else:
                t.st[key] = [o, {}, []]
        deps.discard(o)
        for d in deps:
            if (not d.is_dma) and (not o.is_dma) and d.eng == o.eng and d.eng == "pe":
                continue
            o.deps.append(d)
            d.sig = True
        self.ops.append(o)
        return o

    def dma(self, q, out, in_, reads=(), writes=(), **kw):
        return self.op(q, lambda e: e.dma_start(out=out, in_=in_, **kw), reads, writes, dma=True)

    def emit(self):
        nc = self.nc
        sems = {e: nc.alloc_semaphore(name=f"sem_{e}") for e in COMPUTE}
        dsems = {q: [nc.alloc_semaphore(name=f"dsem_{q}_{i}") for i in range(NDMA_SEM)] for q in QUEUES}
        cnt = {e: 0 for e in COMPUTE}
        dcnt = {q: 0 for q in QUEUES}
        lastop = {}
        for o in self.ops:
            if o.bar:
                for lo in lastop.values():
                    lo.sig = True
            elif not o.is_dma:
                lastop[o.eng] = o
        dlast = {}
        for o in self.ops:
            if o.bar:
                o.deps = (dict(cnt), dict(dlast))
                continue
            if o.is_dma:
                i = dcnt[o.eng]
                dcnt[o.eng] += 1
                o.dsem = dsems[o.eng][i % NDMA_SEM]
                o.dval = 16 * (i // NDMA_SEM + 1)
                dlast[id(o.dsem)] = (o.dsem, o.dval)
            elif o.sig:
                cnt[o.eng] += 1
                o.signo = cnt[o.eng]
        per_eng = {e: [] for e in set(COMPUTE) | set(QUEUES)}
        for o in self.ops:
            if o.bar:
                for e in per_eng:
                    per_eng[e].append(o)
            else:
                per_eng[o.eng].append(o)

        def run_engine(ename, eng):
            waited = {}

            def wait(sem, val):
                k = id(sem)
                if waited.get(k, 0) >= val:
                    return
                waited[k] = val
                eng.wait_ge(sem, val)

            last_dma = {}
            for o in per_eng[ename]:
                if o.bar:
                    for (ce, v) in o.deps[0].items():
                        if v > 0:
                            wait(sems[ce], v)
                    for (s, v) in o.deps[1].values():
                        wait(s, v)
                    continue
                for d in o.deps:
                    if d.is_dma:
                        wait(d.dsem, d.dval)
                    else:
                        wait(sems[d.eng], d.signo)
                if o.is_dma:
                    if o.dval > 16:
                        wait(o.dsem, o.dval - 16)
                    ins = o.fn(eng)
                    ins.then_inc(o.dsem, 16)
                    last_dma[id(o.dsem)] = (o.dsem, o.dval)
                else:
                    ins = o.fn(eng)
                    if o.sig:
                        ins.then_inc(sems[ename], 1)
            for (s, v) in last_dma.values():
                wait(s, v)

        with nc.Block() as block:
            @block.tensor
            def _(e):
                run_engine("pe", e)

            @block.scalar
            def _(e):
                run_engine("act", e)

            @block.vector
            def _(e):
                run_engine("dve", e)

            @block.gpsimd
            def _(e):
                run_engine("pool", e)

            @block.sync
            def _(e):
                run_engine("sp", e)
        return {e: len([o for o in v if not o.bar]) for e, v in per_eng.items()}


D = 1024
DR = 512
NH = 8
HD = 64
NE = 16
C0 = float(np.exp(-0.5))
GN_EPS = 64e-5
NORM_EPS = 1e-6
CH = 64
import os as _os
YVAR = int(_os.environ.get('YVAR', '0'))


def build(TX=4096, CL=256, debug=False, stop=None):
    nc = bass.Bass("TRN2", target_bir_lowering=False)
    P = Prog(nc)
    TT = CL + TX
    NTX = TX // 128
    CAP = 2 * TX // NE
    ST = min(128, CAP)
    NST = CAP // ST
    GW = 64
    ROWS = TX // GW

    def fin():
        while P.stacks:
            P.pop()
        return nc, P.emit()

    def din(name, shape, dt=F32):
        return P.dram(name, shape, dt, kind="ExternalInput")

    x_d = din("x", [TX, D])
    ctx_d = din("ctx", [CL, D])
    cT_d = din("cT", [128, 16])
    adaw_d = din("ada_w", [D, 6 * D])
    adab_d = din("adabT", [128, 48])
    gn_d = din("gnT", [128, 24])
    finalg_d = din("final_g", [1, D])
    win_d = din("w_in_p", [D, 3328])
    mu_d = din("muT", [128, 14])
    lora_d = din("loraW", [2, 128, DR])
    wa0_d = din("wa0T", [128, 2, 2, 4])
    kvec_d = din("kvecT", [128, 3, 4])
    gup_d = din("g_up", [128, DR])
    gnrow_d = din("gnrow", [2, DR])
    cw_d = din("cwT", [128, 4, 3])
    wout_d = din("w_out", [D, D])
    rw_d = din("router_w", [D, NE])
    wg_d = din("exp_w_gate", [NE, D, D])
    wu_d = din("exp_w_up", [NE, D, D])
    wd_d = din("exp_w_down", [NE, D, D])
    cst_d = din("consts", [128, 9, 128])
    iota_d = din("iotas", [128, 512 + 2])
    rmask_d = din("rmask", [128, 512])
    tokhl_d = din("tokhl", [128, (TX // 128) * 2])
    out_d = P.dram("out", [TX, D], F32, kind="ExternalOutput")
    dbg = {}
    if debug:
        dbg["pxm"] = P.dram("dbg_pxm", [1792, TT], BF16, kind="ExternalOutput")
        dbg["bx"] = P.dram("dbg_bx", [512, TX], F32, kind="ExternalOutput")
        dbg["y"] = P.dram("dbg_y", [2, TX, DR], F32, kind="ExternalOutput")
        dbg["x1"] = P.dram("dbg_x1", [TX, D], F32, kind="ExternalOutput")
        dbg["aff"] = P.dram("dbg_aff", [128, NTX * NE], F32, kind="ExternalOutput")

    PXM = dbg["pxm"] if debug else P.dram("pxm", [1792, TT], BF16)
    YD = dbg["y"] if debug else P.dram("yd", [2, TX, DR], F32)
    X1 = dbg["x1"] if debug else P.dram("x1", [TX, D], F32)
    XH2 = P.dram("xh2", [TX, D], BF16)

    cst = P.sb([128, 9, 128], F32, "cst")
    identf = cst[:, 0, :]
    cstb = P.sb([128, 9, 128], BF16, "cstb")
    identb = cstb[:, 0, :]
    onesb = cstb[:, 1, :]
    blkones = cstb[:, 2, :]
    modT = P.sb([128, 48, 2], F32, "modT")
    gnT = P.sb([128, 24], F32, "gnT")
    sc1x = P.sb([128, 8], F32, "sc1x")
    sc1c = P.sb([128, 8], F32, "sc1c")
    sc2 = P.sb([128, 8], F32, "sc2")
    epsn = P.sb([128, 1], F32, "epsn")
    epsg = P.sb([128, 1], F32, "epsg")
    g1bc = P.sb([128, D], F32, "g1bc")
    g2bc = P.sb([128, D], F32, "g2bc")
    fgbc = P.sb([128, D], F32, "fgbc")
    affall = P.sb([128, NTX, NE], F32, "affall")
    junk2 = P.sb([128, D], F32, "junk2")
    P.push()
    bxT = P.sb([128, 4, TX], BF16, "bxT")

    q2 = ["sp", "pool"]
    P.dma("sp", cst[:], cst_d.a, reads=[cst_d], writes=[cst])
    P.op("dve", lambda e: e.tensor_copy(out=cstb[:], in_=cst[:]), [cst], [cstb])
    P.dma("sp", gnT[:], gn_d.a, reads=[gn_d], writes=[gnT])
    P.op("dve", lambda e: e.memset(epsn[:], NORM_EPS), [], [epsn])
    P.op("dve", lambda e: e.memset(epsg[:], GN_EPS), [], [epsg])
    P.dma("pool", fgbc[:], finalg_d.a.partition_broadcast(128), reads=[finalg_d], writes=[fgbc])

    P.push()
    cT = P.sb([128, 16], F32, "cT")
    scT = P.sb([128, 16], F32, "scT")
    adab = P.sb([128, 48], F32, "adab")
    P.dma("sp", cT[:], cT_d.a, reads=[cT_d], writes=[cT])
    P.dma("sp", adab[:], adab_d.a, reads=[adab_d], writes=[adab])
    P.op("act", lambda e: e.activation(out=scT[:], in_=cT[:], func=AF.Silu), [cT], [scT])
    adaw = [P.sb([128, 8, 512], F32, f"adaw{i}") for i in range(2)]
    psA = P.ps([128, 512], F32, "psA")
    adaw_v = adaw_d.a.rearrange("(k p) c -> p k c", p=128)
    for jb in range(12):
        aw = adaw[jb % 2]
        P.dma(q2[jb % 2], aw[:], adaw_v[:, :, jb * 512:(jb + 1) * 512], reads=[adaw_d], writes=[aw])
        for jj in range(4):
            j = jb * 4 + jj
            for k in range(8):
                P.op("pe", lambda e, aw=aw, jj=jj, j=j, k=k: e.matmul(out=psA[:, 2 * j:2 * j + 2], lhsT=aw[:, k, jj * 128:(jj + 1) * 128], rhs=scT[:, k::8], start=(k == 0), stop=(k == 7)), [aw, scT], [psA])
    P.op("dve", lambda e: e.tensor_tensor(out=modT[:], in0=psA[:, 0:96].rearrange("p (j m) -> p j m", m=2), in1=adab[:].unsqueeze(2).to_broadcast([128, 48, 2]), op=ALU.add), [psA, adab], [modT])
    for (dst, gofs, cofs, m) in ((sc1x, 0, 8, 0), (sc1c, 0, 8, 1), (sc2, 8, 32, 0)):
        P.op("dve", lambda e, dst=dst, gofs=gofs, cofs=cofs, m=m: e.scalar_tensor_tensor(out=dst[:], in0=modT[:, cofs:cofs + 8, m], scalar=1.0, in1=gnT[:, gofs:gofs + 8], op0=ALU.add, op1=ALU.mult), [modT, gnT], [dst])
    dg = P.sb([128, 8, 128], F32, "dg")
    for (dst, ofs) in ((g1bc, 16), (g2bc, 40)):
        for k in range(8):
            P.op("dve", lambda e, k=k, ofs=ofs: e.tensor_scalar(out=dg[:, k, :], in0=identf, scalar1=modT[:, ofs + k, 0:1], scalar2=None, op0=ALU.mult), [cst, modT], [dg])
        for hf in range(2):
            P.op("pe", lambda e, hf=hf: e.matmul(out=psA[:], lhsT=cst[:, 1, :], rhs=dg[:, hf * 4:(hf + 1) * 4, :].rearrange("p k c -> p (k c)"), start=True, stop=True), [cst, dg], [psA])
            P.op("act", lambda e, hf=hf, dst=dst: e.activation(out=dst[:, hf * 512:(hf + 1) * 512], in_=psA[:], func=AF.Copy), [psA], [dst])

    if stop == 'A':
        return fin()
    P.pop()
    P.push()
    h1T = P.sb([128, 8, TT], BF16, "h1T")
    P.push()
    xin = [P.sb([128, 4, D], F32, f"xin{i}") for i in range(2)]
    xhf = [P.sb([128, 4, D], F32, f"xhf{i}") for i in range(2)]
    junk = P.sb([128, D], F32, "junk")
    ssq = P.sb([128, 4], F32, "ssq")
    stdv = P.sb([128, 4], F32, "stdv")
    rstd = P.sb([128, 4], F32, "rstd")
    psT = [P.ps([128, 512], F32, f"psT{i}") for i in range(4)]
    groups = [(ctx_d, t0, min(512, CL - t0), t0) for t0 in range(0, CL, 512)] + [(x_d, t0, 512, CL + t0) for t0 in range(0, TX, 512)]
    for gi, (src, t0, n, dst0) in enumerate(groups):
        nt = n // 128
        xi = xin[gi % 2]
        xh = xhf[gi % 2]
        isctx = src is ctx_d
        P.dma(q2[gi % 2], xi[:, 0:nt, :], src.a[t0:t0 + n, :].rearrange("(t p) d -> p t d", p=128), reads=[src], writes=[xi])
        for t in range(nt):
            P.op("act", lambda e, t=t, xi=xi: e.activation(out=junk[:], in_=xi[:, t, :], func=AF.Square, accum_out=ssq[:, t:t + 1]), [xi], [junk, ssq])
        P.op("act", lambda e, nt=nt: e.activation(out=stdv[:, 0:nt], in_=ssq[:, 0:nt], func=AF.Sqrt, bias=epsn[:], scale=1.0 / D), [ssq, epsn], [stdv])
        P.op("dve", lambda e, nt=nt: e.reciprocal(out=rstd[:, 0:nt], in_=stdv[:, 0:nt]), [stdv], [rstd])
        for t in range(nt):
            P.op("dve" if t % 2 else "pool", lambda e, t=t, xi=xi, xh=xh: e.tensor_scalar(out=xh[:, t, :], in0=xi[:, t, :], scalar1=rstd[:, t:t + 1], scalar2=None, op0=ALU.mult), [xi, rstd], [(xh, t)])
        for k in range(8):
            pt = psT[k % 4]
            for t in range(nt):
                P.op("pe", lambda e, t=t, k=k, pt=pt, xh=xh: e.transpose(out=pt[:, t * 128:(t + 1) * 128], in_=xh[:, t, k * 128:(k + 1) * 128], identity=identf), [(xh, t), cst], [pt])
            scv = sc1c if isctx else sc1x
            m = 1 if isctx else 0
            if k % 2:
                P.op("act", lambda e, k=k, pt=pt, n=n, dst0=dst0, scv=scv, m=m: e.activation(out=h1T[:, k, dst0:dst0 + n], in_=pt[:, 0:n], func=AF.Identity, bias=modT[:, k, m:m + 1], scale=scv[:, k:k + 1]), [pt, modT, scv], [(h1T, gi)])
            else:
                P.op("dve", lambda e, k=k, pt=pt, n=n, dst0=dst0, scv=scv, m=m: e.tensor_scalar(out=h1T[:, k, dst0:dst0 + n], in0=pt[:, 0:n], scalar1=scv[:, k:k + 1], scalar2=modT[:, k, m:m + 1], op0=ALU.mult, op1=ALU.add), [pt, modT, scv], [(h1T, gi)])

    P.pop()
    P.push()
    muT = P.sb([128, 14], F32, "muT")
    omuT = P.sb([128, 14], F32, "omuT")
    cwT = P.sb([128, 4, 3], F32, "cwT")
    P.dma("sp", muT[:], mu_d.a, reads=[mu_d], writes=[muT])
    P.dma("sp", cwT[:], cw_d.a, reads=[cw_d], writes=[cwT])
    P.op("dve", lambda e: e.tensor_scalar(out=omuT[:], in0=muT[:], scalar1=-1.0, scalar2=1.0, op0=ALU.mult, op1=ALU.add), [muT], [omuT])
    wst = [P.sb([128, 8, 128], F32, f"wst{i}") for i in range(2)]
    wbf = [P.sb([128, 8, 128], BF16, f"wbf{i}") for i in range(2)]
    pxc = P.sb([128, TT], F32, "pxc")
    pxo = [P.sb([128, TT], BF16, f"pxo{i}") for i in range(2)]
    cvb = P.sb([128, TX], BF16, "cvb")
    cvc = P.sb([128, TX], F32, "cvc")
    cva = pxc
    win_v = win_d.a.rearrange("(k p) c -> p k c", p=128)
    tok_groups = [(dst0, n) for (_, _, n, dst0) in groups]
    pxm_v = PXM.a

    def project(cc, col0, toks, sink):
        w = wst[cc % 2]
        wb = wbf[cc % 2]
        P.dma(q2[cc % 2], w[:], win_v[:, :, col0:col0 + 128], reads=[win_d], writes=[w])
        P.op("pool", lambda e, w=w, wb=wb: e.tensor_copy(out=wb[:], in_=w[:]), [w], [wb])
        for gi, (tok0, n) in enumerate(toks):
            pt = psT[gi % 4]
            for k in range(8):
                P.op("pe", lambda e, k=k, pt=pt, wb=wb, tok0=tok0, n=n: e.matmul(out=pt[:, 0:n], lhsT=wb[:, k, :], rhs=h1T[:, k, tok0:tok0 + n], start=(k == 0), stop=(k == 7)), [wb, h1T], [pt])
            sink(gi, pt, tok0, n)

    for ci in range(14):
        def sink(gi, pt, tok0, n):
            P.op("act" if gi % 2 else "dve", (lambda e, pt=pt, tok0=tok0, n=n: e.activation(out=pxc[:, tok0:tok0 + n], in_=pt[:, 0:n], func=AF.Copy)) if gi % 2 else (lambda e, pt=pt, tok0=tok0, n=n: e.tensor_copy(out=pxc[:, tok0:tok0 + n], in_=pt[:, 0:n])), [pt], [(pxc, gi)])
        project(ci, ci * 128, tok_groups, sink)
        po = pxo[ci % 2]
        P.op("act", lambda e, ci=ci, po=po: e.activation(out=po[:], in_=pxc[:], func=AF.Copy, scale=omuT[:, ci:ci + 1]), [pxc, omuT], [po])
        if ci < 12:
            parts = [(0, 128, ci % 4)]
        else:
            parts = [(0, 64, 0 if ci == 12 else 2), (64, 128, 1 if ci == 12 else 3)]
        for (p0, p1, q) in parts:
            mu_s = muT[p0:p1, ci:ci + 1]
            Xv = pxc[p0:p1, CL:TT].rearrange("p (r c) -> p r c", c=GW)
            Ov = po[p0:p1, CL:TT].rearrange("p (r c) -> p r c", c=GW)
            if q == 0:
                oa, ia = Ov[:, :, 1:], Xv[:, :, :-1]
            elif q == 1:
                oa, ia = Ov[:, :, :-1], Xv[:, :, 1:]
            elif q == 2:
                oa, ia = po[p0:p1, CL + GW:TT], pxc[p0:p1, CL:TT - GW]
            else:
                oa, ia = po[p0:p1, CL:TT - GW], pxc[p0:p1, CL + GW:TT]
            P.op("dve", lambda e, oa=oa, ia=ia, mu_s=mu_s: e.scalar_tensor_tensor(out=oa, in0=ia, scalar=mu_s, in1=oa, op0=ALU.mult, op1=ALU.add), [pxc, po, muT], [po])
            if q % 2 == 0:
                oc, ic = po[p0:p1, 1:CL], pxc[p0:p1, 0:CL - 1]
            else:
                oc, ic = po[p0:p1, 0:CL - 1], pxc[p0:p1, 1:CL]
            P.op("dve", lambda e, oc=oc, ic=ic, mu_s=mu_s: e.scalar_tensor_tensor(out=oc, in0=ic, scalar=mu_s, in1=oc, op0=ALU.mult, op1=ALU.add), [pxc, po, muT], [po])
        if ci < 12:
            Q, q = ci // 4, ci % 4
            P.dma("sp", pxm_v[Q * 512 + q:(Q + 1) * 512:4, :], po[:], reads=[po], writes=[(PXM, ci)])
        else:
            qa = 0 if ci == 12 else 2
            P.dma("sp", pxm_v[1536 + qa:1792:4, :], po[0:64, :], reads=[po], writes=[(PXM, ci)])
            P.dma("sp", pxm_v[1536 + qa + 1:1792:4, :], po[64:128, :], reads=[po], writes=[(PXM, ci)])

    x_groups = [(tok0, n) for (tok0, n) in tok_groups if tok0 >= CL]
    for i in range(4):
        def sink_b(gi, pt, tok0, n):
            P.op("act", lambda e, pt=pt, tok0=tok0, n=n: e.activation(out=cvb[:, tok0 - CL:tok0 - CL + n], in_=pt[:, 0:n], func=AF.Copy), [pt], [(cvb, gi)])
        project(14 + i * 3, 1792 + i * 128, x_groups, sink_b)

        def sink_c(gi, pt, tok0, n):
            P.op("act", lambda e, pt=pt, tok0=tok0, n=n: e.activation(out=cvc[:, tok0 - CL:tok0 - CL + n], in_=pt[:, 0:n], func=AF.Copy), [pt], [(cvc, gi)])
        project(15 + i * 3, 1792 + 512 + i * 128, x_groups, sink_c)

        def sink_u(gi, pt, tok0, n):
            P.op("dve", lambda e, pt=pt, tok0=tok0, n=n: e.tensor_tensor(out=cvc[:, tok0 - CL:tok0 - CL + n], in0=cvc[:, tok0 - CL:tok0 - CL + n], in1=pt[:, 0:n], op=ALU.mult), [pt, (cvc, gi)], [(cvc, gi)])
        project(16 + i * 3, 1792 + 1024 + i * 128, x_groups, sink_u)
        P.op("act", lambda e, i=i: e.activation(out=cva[:, 0:TX], in_=cvc[:], func=AF.Copy, scale=cwT[:, i, 1:2]), [cvc, cwT], [cva])
        P.op("dve", lambda e, i=i: e.scalar_tensor_tensor(out=cva[:, 1:TX], in0=cvc[:, 0:TX - 1], scalar=cwT[:, i, 0:1], in1=cva[:, 1:TX], op0=ALU.mult, op1=ALU.add), [cvc, cva, cwT], [cva])
        P.op("dve", lambda e, i=i: e.scalar_tensor_tensor(out=cva[:, 0:TX - 1], in0=cvc[:, 1:TX], scalar=cwT[:, i, 2:3], in1=cva[:, 0:TX - 1], op0=ALU.mult, op1=ALU.add), [cvc, cva, cwT], [cva])
        P.op("pool", lambda e, i=i: e.tensor_tensor(out=bxT[:, i, :], in0=cva[:, 0:TX], in1=cvb[:], op=ALU.mult), [cva, cvb], [(bxT, i)])
        if debug:
            P.op("pool", lambda e: e.tensor_tensor(out=cva[:, 0:TX], in0=cva[:, 0:TX], in1=cvb[:], op=ALU.mult), [cva, cvb], [cva])
            P.dma("sp", dbg["bx"].a[i * 128:(i + 1) * 128, :], cva[:, 0:TX], reads=[cva], writes=[dbg["bx"]])


    if stop == 'B':
        return fin()
    P.pop()
    P.pop()
    NB = 512
    lorab = P.sb([128, 2, DR], BF16, "lorab")
    wa0 = P.sb([128, 2, 2, 4], F32, "wa0")
    kvec = P.sb([128, 3, 4], F32, "kvec")
    omka = P.sb([128, 4], F32, "omka")
    wab = P.sb([128, NB], BF16, "wab")
    P.push()
    psX = [P.ps([128, 512], F32, f"psX{i}") for i in range(3)]
    rmask = P.sb([128, 512], F32, "rmask")
    P.dma("sp", rmask[:], rmask_d.a, reads=[rmask_d], writes=[rmask])
    loraf = P.sb([128, 2, DR], F32, "loraf")
    P.dma("sp", loraf[:], lora_d.a.rearrange("d p c -> p d c"), reads=[lora_d], writes=[loraf])
    P.op("pool", lambda e: e.tensor_copy(out=lorab[:], in_=loraf[:]), [loraf], [lorab])
    P.dma("sp", wa0[:], wa0_d.a, reads=[wa0_d], writes=[wa0])
    P.dma("sp", kvec[:], kvec_d.a, reads=[kvec_d], writes=[kvec])
    P.op("dve", lambda e: e.tensor_scalar(out=omka[:], in0=kvec[:, 1, :], scalar1=-1.0, scalar2=1.0, op0=ALU.mult, op1=ALU.add), [kvec], [omka])
    mpair = P.sb([128, 2, 2, 128], F32, "mpair")
    P.op("dve", lambda e: e.tensor_copy(out=mpair[:, 0, 0, :], in_=cst[:, 4, :]), [cst], [mpair])
    P.op("dve", lambda e: e.tensor_copy(out=mpair[:, 0, 1, :], in_=cst[:, 5, :]), [cst], [mpair])
    P.op("dve", lambda e: e.tensor_copy(out=mpair[:, 1, 0, :], in_=cst[:, 3, :]), [cst], [mpair])
    P.op("dve", lambda e: e.tensor_copy(out=mpair[:, 1, 1, :], in_=cst[:, 6, :]), [cst], [mpair])
    NB = 512
    rkv = P.sb([128, 3, NB], BF16, "rkv")
    twb = P.sb([64, NB], BF16, "twb")
    s_t = P.sb([128, NB], F32, "s_t")
    ag_t = P.sb([128, NB], F32, "ag_t")
    Pc = P.sb([128, NB], F32, "Pc")
    cin = P.sb([128, NB], F32, "cin")
    cex = P.sb([128, NB], F32, "cex")
    tmc = P.sb([128, NB], F32, "tmc")
    E1 = P.sb([128, NB], F32, "E1")
    E2 = P.sb([128, NB], F32, "E2")
    E3 = P.sb([128, NB], F32, "E3")
    E4 = P.sb([128, NB], F32, "E4")
    GC2 = [P.sb([128, 8], F32, f"GC{i}") for i in range(3)]
    kk = P.sb([128, NB], F32, "kk")
    sqb = P.sb([128, NB], BF16, "sqb")
    nrm = P.sb([128, NB], F32, "nrm")
    kkn = P.sb([128, NB], F32, "kkn")
    b_t = P.sb([128, NB], F32, "b_t")
    kd_t = P.sb([128, NB], F32, "kd_t")
    ARbd2 = [P.sb([128, 8, 2, 128], BF16, f"ARbd{i}") for i in range(3)]
    BKbd2 = [P.sb([128, 8, 2, 128], BF16, f"BKbd{i}") for i in range(2)]
    HVbd = P.sb([128, 8, 3, 128], BF16, "HVbd")
    TM2 = [P.sb([128, 8, 3, 128], BF16, f"TM{i}") for i in range(3)]
    for tz in (ARbd2[0], ARbd2[1], ARbd2[2], BKbd2[0], BKbd2[1], HVbd):
        P.op("pool", lambda e, tz=tz: e.memset(tz[:], 0.0), [], [tz])
    Sx = [[P.sb([128, 3, 128], BF16, f"S{c}_{i}") for i in range(2)] for c in range(8)]
    Lts2 = [[P.sb([128, 3, 128], BF16, f"Lt{c}_{i}") for c in range(8)] for i in range(2)]
    TTs2 = [[P.sb([128, 128], BF16, f"TT{c}_{i}") for c in range(8)] for i in range(2)]
    Qb = P.sb([128, 128], BF16, "Qb")
    Ub = P.sb([128, 128], BF16, "Ub")
    Hf = P.sb([128, 128], F32, "Hf")
    Hb = P.sb([128, 128], BF16, "Hb")
    Yst = [P.sb([128, 8, 128], F32, f"Yst{i}") for i in range(2)]
    psZw, psZa, psSS, psTM = psT[0], psT[1], psT[2], psT[3]
    psLA, psLB, psN, psC = psA, psX[0], psX[1], psX[2]
    pxm3 = pxm_v[0:1536, :].rearrange("(q c) t -> c q t", q=3)
    ctx_blocks = [(0, CL)]
    x_blocks = [(CL + i * NB, NB) for i in range(TX // NB)]
    ev = ["dve", "act"]
    def do_block(g, d, tok0, n, kk_, first, Nl, N2l, Cl):
                P.defer = Nl
                par = kk_ % 2
                ARbd, TM, GC = ARbd2[kk_ % 3], TM2[kk_ % 3], GC2[kk_ % 3]
                BKbd = BKbd2[par]
                Lts, TTs = Lts2[par], TTs2[par]
                Mx = cst[:, 3, :] if d == 0 else cst[:, 4, :]
                nch = n // CH
                isx = tok0 >= CL
                yst = Yst[par]
                P.dma("sp", rkv[:, :, 0:n], pxm3[g * 128:(g + 1) * 128, :, tok0:tok0 + n], reads=[PXM], writes=[rkv])
                P.dma("pool", wab[:, 0:n], pxm_v[1536:1664, tok0:tok0 + n], reads=[PXM], writes=[wab])
                P.op("act", lambda e, n=n: e.activation(out=twb[:, 0:n], in_=wab[0:64, 0:n], func=AF.Tanh), [wab], [twb])
                P.op("pe", lambda e, n=n, g=g, d=d: e.matmul(out=psZw[:, 0:n], lhsT=lorab[0:64, d, g * 128:(g + 1) * 128], rhs=twb[0:64, 0:n], start=True, stop=True), [lorab, twb], [psZw])
                P.op("pe", lambda e, n=n, g=g, d=d: e.matmul(out=psZa[:, 0:n], lhsT=lorab[64:128, d, g * 128:(g + 1) * 128], rhs=wab[64:128, 0:n], start=True, stop=True), [lorab, wab], [psZa])
                P.op("act", lambda e, n=n, g=g, d=d: e.activation(out=s_t[:, 0:n], in_=psZw[:, 0:n], func=AF.Sigmoid, bias=wa0[:, d, 0, g:g + 1]), [psZw, wa0], [s_t])
                P.op("act", lambda e, n=n, g=g, d=d: e.activation(out=ag_t[:, 0:n], in_=psZa[:, 0:n], func=AF.Sigmoid, bias=wa0[:, d, 1, g:g + 1]), [psZa, wa0], [ag_t])
                P.op("dve", lambda e, n=n: e.tensor_tensor_scan(out=Pc[:, 0:n], data0=rmask[:, 0:n], data1=s_t[:, 0:n], initial=0.0, op0=ALU.mult, op1=ALU.add), [rmask, s_t], [Pc])
                v3 = lambda t_, n=n: t_[:, 0:n].rearrange("p (c t) -> p c t", t=CH)
                totb = v3(Pc)[:, :, CH - 1:CH].to_broadcast([128, nch, CH])
                if d == 0:
                    P.op("pool", lambda e, n=n: e.tensor_tensor(out=cex[:, 0:n], in0=Pc[:, 0:n], in1=s_t[:, 0:n], op=ALU.subtract), [Pc, s_t], [cex])
                    cin_ = Pc
                else:
                    P.op("dve", lambda e, totb=totb, v3=v3: e.tensor_tensor(out=v3(cex), in0=totb, in1=v3(Pc), op=ALU.subtract), [Pc], [cex])
                    P.op("pool", lambda e, n=n: e.tensor_tensor(out=cin[:, 0:n], in0=cex[:, 0:n], in1=s_t[:, 0:n], op=ALU.add), [cex, s_t], [cin])
                    cin_ = cin
                P.op("dve", lambda e, totb=totb, v3=v3, cin_=cin_: e.tensor_tensor(out=v3(tmc), in0=totb, in1=v3(cin_), op=ALU.subtract), [Pc, cin_], [tmc])
                P.op("act", lambda e, n=n, cin_=cin_: e.activation(out=E1[:, 0:n], in_=cin_[:, 0:n], func=AF.Exp, scale=-C0), [cin_], [E1])
                P.op("act", lambda e, n=n: e.activation(out=E2[:, 0:n], in_=cex[:, 0:n], func=AF.Exp, scale=-C0), [cex], [E2])
                P.op("act", lambda e, n=n, cin_=cin_: e.activation(out=E3[:, 0:n], in_=cin_[:, 0:n], func=AF.Exp, scale=C0), [cin_], [E3])
                P.op("act", lambda e, n=n: e.activation(out=E4[:, 0:n], in_=tmc[:, 0:n], func=AF.Exp, scale=-C0), [tmc], [E4])
                P.op("act", lambda e, nch=nch, v3=v3: e.activation(out=GC[:, 0:nch], in_=v3(Pc)[:, :, CH - 1], func=AF.Exp, scale=-C0), [Pc], [GC])
                P.op("pool", lambda e, n=n, g=g: e.tensor_scalar(out=kk[:, 0:n], in0=rkv[:, 1, 0:n], scalar1=kvec[:, 0, g:g + 1], scalar2=None, op0=ALU.mult), [rkv, kvec], [kk])
                P.op("pool", lambda e, n=n: e.tensor_tensor(out=sqb[:, 0:n], in0=kk[:, 0:n], in1=kk[:, 0:n], op=ALU.mult), [kk], [sqb])
                P.op("pe", lambda e, n=n: e.matmul(out=psSS[:, 0:n], lhsT=blkones, rhs=sqb[:, 0:n], start=True, stop=True), [cstb, sqb], [psSS])
                P.op("act", lambda e, n=n: e.activation(out=nrm[:, 0:n], in_=psSS[:, 0:n], func=AF.Sqrt), [psSS], [nrm])
                P.op("dve", lambda e, n=n: e.tensor_scalar(out=nrm[:, 0:n], in0=nrm[:, 0:n], scalar1=1e-12, scalar2=None, op0=ALU.max), [nrm], [nrm])
                P.op("dve", lambda e, n=n: e.reciprocal(out=nrm[:, 0:n], in_=nrm[:, 0:n]), [nrm], [nrm])
                P.op("pool", lambda e, n=n: e.tensor_tensor(out=kkn[:, 0:n], in0=kk[:, 0:n], in1=nrm[:, 0:n], op=ALU.mult), [kk, nrm], [kkn])
                P.op("pool", lambda e, n=n: e.tensor_tensor(out=b_t[:, 0:n], in0=kkn[:, 0:n], in1=ag_t[:, 0:n], op=ALU.mult), [kkn, ag_t], [b_t])
                P.op("dve", lambda e, n=n, g=g: e.tensor_scalar(out=kd_t[:, 0:n], in0=ag_t[:, 0:n], scalar1=kvec[:, 1, g:g + 1], scalar2=omka[:, g:g + 1], op0=ALU.mult, op1=ALU.add), [ag_t, kvec, omka], [kd_t])
                P.op("pool", lambda e, n=n: e.tensor_tensor(out=kd_t[:, 0:n], in0=kd_t[:, 0:n], in1=rkv[:, 1, 0:n], op=ALU.mult), [kd_t, rkv], [kd_t])
                oi = 0
                for hh in range(2):
                    ps_ = slice(hh * 64, hh * 64 + 64)
                    v3h = lambda t_, n=n, ps_=ps_: t_[ps_, 0:n].rearrange("p (c t) -> p c t", t=CH)
                    r3 = rkv[ps_, 0, 0:n].rearrange("p (c t) -> p c t", t=CH)
                    vv3 = rkv[ps_, 2, 0:n].rearrange("p (c t) -> p c t", t=CH)
                    specs = [
                        (ARbd, 0, None, kkn, E2, -1.0), (ARbd, 1, r3, None, E1, None),
                        (BKbd, 0, None, b_t, E3, None), (BKbd, 1, None, kd_t, E3, None),
                        (HVbd, 1, None, b_t, E4, None), (HVbd, 2, None, kd_t, E4, None),
                    ]
                    for (dst, slot, a_ap, a_t, e_t, neg) in specs:
                        o_ap = dst[ps_, 0:nch, slot, hh * 64:hh * 64 + 64]
                        in0 = a_ap if a_ap is not None else v3h(a_t)
                        rd = [e_t] + ([rkv] if a_t is None else [a_t])
                        eng = ("dve", "pool")[oi % 2]
                        oi += 1
                        if neg is not None:
                            P.op("dve", lambda e, o_ap=o_ap, in0=in0, e_t=e_t, v3h=v3h: e.scalar_tensor_tensor(out=o_ap, in0=in0, scalar=-1.0, in1=v3h(e_t), op0=ALU.mult, op1=ALU.mult), rd, [(dst, slot)])
                        else:
                            P.op(eng, lambda e, o_ap=o_ap, in0=in0, e_t=e_t, v3h=v3h: e.tensor_tensor(out=o_ap, in0=in0, in1=v3h(e_t), op=ALU.mult), rd, [(dst, slot)])
                    P.op("pool", lambda e, hh=hh, ps_=ps_, vv3=vv3, nch=nch: e.tensor_copy(out=HVbd[ps_, 0:nch, 0, hh * 64:hh * 64 + 64], in_=vv3), [rkv], [(HVbd, 0)])
                for c in range(nch):
                    for j in range(3):
                        P.op("pe", lambda e, c=c, j=j: e.matmul(out=psTM[:, j * 128:(j + 1) * 128], lhsT=HVbd[:, c, j, :], rhs=identb, start=True, stop=True), [HVbd, cstb], [psTM])
                    P.op(ev[c % 2], (lambda e, c=c: e.activation(out=TM[:, c, :, :].rearrange("p j t -> p (j t)"), in_=psTM[:, 0:384], func=AF.Copy)) if ev[c % 2] == "act" else (lambda e, c=c: e.tensor_copy(out=TM[:, c, :, :].rearrange("p j t -> p (j t)"), in_=psTM[:, 0:384])), [psTM], [(TM, c)])
                P.defer = N2l
                for c in range(nch):
                    AT = ARbd[:, c, 0, :]
                    AR2 = ARbd[:, c, :, :].rearrange("p j t -> p (j t)")
                    bA, bB = ((psLA, psLB), (psTM, psN))[c % 2]
                    S0 = Sx[c][0]
                    Lc = Lts[c]
                    P.op("pe", lambda e, AT=AT, c=c, bA=bA: e.matmul(out=bA[:, 0:128], lhsT=AT, rhs=BKbd[:, c, 0, :], start=True, stop=True), [ARbd, BKbd], [bA])
                    P.op("pe", lambda e, AR2=AR2, c=c, bA=bA: e.matmul(out=bA[:, 128:384], lhsT=BKbd[:, c, 0, :], rhs=AR2, start=True, stop=True), [ARbd, BKbd], [bA])
                    P.op("pe", lambda e, AR2=AR2, c=c, bB=bB: e.matmul(out=bB[:, 0:256], lhsT=BKbd[:, c, 1, :], rhs=AR2, start=True, stop=True), [ARbd, BKbd], [bB])
                    P.op("dve", lambda e, Mx=Mx, S0=S0, bA=bA: e.tensor_tensor(out=S0[:, 2, :], in0=bA[:, 0:128], in1=Mx, op=ALU.mult), [bA, cst], [S0])
                    P.op("dve", lambda e, d=d, S0=S0, bA=bA: e.tensor_tensor(out=S0[:, 0, :], in0=bA[:, 128:256], in1=mpair[:, d, 0, :], op=ALU.mult), [bA, mpair], [S0])
                    P.op("pool", lambda e, S0=S0: e.tensor_copy(out=S0[:, 1, :], in_=identb), [cstb], [S0])
                    P.op("dve", lambda e, d=d, Lc=Lc, bA=bA: e.tensor_tensor(out=Lc[:, 0, :], in0=bA[:, 256:384], in1=mpair[:, d, 1, :], op=ALU.mult), [bA, mpair], [Lc])
                    P.op("dve", lambda e, d=d, Lc=Lc, bB=bB: e.tensor_tensor(out=Lc[:, 1:3, :].rearrange("p j t -> p (j t)"), in0=bB[:, 0:256], in1=mpair[:, d, :, :].rearrange("p j t -> p (j t)"), op=ALU.mult), [bB, mpair], [Lc])
                NBK = (psN, psZw, psZa, psSS)
                for lv in range(6):
                    for c in range(nch):
                        cur, nxt = Sx[c][lv % 2], Sx[c][(lv + 1) % 2]
                        pn = NBK[c % 4]
                        engn = "act"
                        if lv < 5:
                            P.op("pe", lambda e, cur=cur, pn=pn: e.matmul(out=pn[:, 0:128], lhsT=cur[:, 2, :], rhs=cur[:, 0, :], start=True, stop=True), [cur], [pn])
                            P.op("pe", lambda e, cur=cur, pn=pn: e.matmul(out=pn[:, 256:384], lhsT=cur[:, 0, :], rhs=cur[:, 2, :], start=True, stop=True), [cur], [pn])
                        P.op("pe", lambda e, cur=cur, pn=pn: e.matmul(out=pn[:, 128:256], lhsT=cur[:, 2, :], rhs=cur[:, 1, :], start=True, stop=False), [cur], [pn])
                        P.op("pe", lambda e, cur=cur, pn=pn: e.matmul(out=pn[:, 128:256], lhsT=identb, rhs=cur[:, 1, :], start=False, stop=True), [cur, cstb], [pn])
                        if lv < 5:
                            P.op(engn, (lambda e, nxt=nxt, pn=pn: e.activation(out=nxt[:].rearrange("p j t -> p (j t)"), in_=pn[:, 0:384], func=AF.Copy)) if engn == "act" else (lambda e, nxt=nxt, pn=pn: e.tensor_copy(out=nxt[:].rearrange("p j t -> p (j t)"), in_=pn[:, 0:384])), [pn], [nxt])
                        else:
                            TTc = TTs[c]
                            P.op(engn, (lambda e, TTc=TTc, pn=pn: e.activation(out=TTc[:], in_=pn[:, 128:256], func=AF.Copy)) if engn == "act" else (lambda e, TTc=TTc, pn=pn: e.tensor_copy(out=TTc[:], in_=pn[:, 128:256])), [pn], [TTc])
                P.defer = Cl
                if first:
                    P.op("dve", lambda e: e.memset(Hf[:], 0.0), [], [Hf])
                    P.op("dve", lambda e: e.memset(Hb[:], 0.0), [], [Hb])
                corder = range(nch) if d == 0 else range(nch - 1, -1, -1)
                for c in corder:
                    AT = ARbd[:, c, 0, :]
                    RT = ARbd[:, c, 1, :]
                    Lc = Lts[c]
                    TTc = TTs[c]
                    Vb = TM[:, c, 0, :]
                    P.op("pe", lambda e, AT=AT: e.matmul(out=psC[:, 0:128], lhsT=AT, rhs=Hb[:], start=True, stop=False), [ARbd, Hb], [psC])
                    P.op("pe", lambda e, Vb=Vb, Lc=Lc: e.matmul(out=psC[:, 0:128], lhsT=Lc[:, 1, :], rhs=Vb, start=False, stop=True), [Lc, (TM, c)], [psC])
                    P.op("dve", lambda e: e.tensor_copy(out=Qb[:], in_=psC[:, 0:128]), [psC], [Qb])
                    P.op("pe", lambda e, TTc=TTc: e.matmul(out=psC[:, 128:256], lhsT=TTc[:], rhs=Qb[:], start=True, stop=True), [TTc, Qb], [psC])
                    P.op("dve", lambda e: e.tensor_copy(out=Ub[:], in_=psC[:, 128:256]), [psC], [Ub])
                    if isx:
                        P.op("pe", lambda e, RT=RT: e.matmul(out=psC[:, 256:384], lhsT=RT, rhs=Hb[:], start=True, stop=False), [ARbd, Hb], [psC])
                        P.op("pe", lambda e, Lc=Lc: e.matmul(out=psC[:, 256:384], lhsT=Lc[:, 0, :], rhs=Ub[:], start=False, stop=False), [Lc, Ub], [psC])
                        P.op("pe", lambda e, Vb=Vb, Lc=Lc: e.matmul(out=psC[:, 256:384], lhsT=Lc[:, 2, :], rhs=Vb, start=False, stop=True), [Lc, (TM, c)], [psC])
                    P.op("pe", lambda e, c=c: e.matmul(out=psC[:, 384:512], lhsT=TM[:, c, 1, :], rhs=Ub[:], start=True, stop=False), [(TM, c), Ub], [psC])
                    P.op("pe", lambda e, c=c, Vb=Vb: e.matmul(out=psC[:, 384:512], lhsT=TM[:, c, 2, :], rhs=Vb, start=False, stop=True), [(TM, c)], [psC])
                    P.op("dve", lambda e, c=c: e.scalar_tensor_tensor(out=Hb[:], in0=Hf[:], scalar=GC[:, c:c + 1], in1=psC[:, 384:512], op0=ALU.mult, op1=ALU.add), [Hf, GC, psC], [Hb])
                    P.op("dve", lambda e, c=c: e.scalar_tensor_tensor(out=Hf[:], in0=Hf[:], scalar=GC[:, c:c + 1], in1=psC[:, 384:512], op0=ALU.mult, op1=ALU.add), [Hf, GC, psC], [Hf])
                    if isx:
                        P.op("dve", lambda e, c=c, yst=yst: e.tensor_copy(out=yst[:, c, :], in_=psC[:, 256:384]), [psC], [yst])
                if isx:
                    xt0 = tok0 - CL
                    for hh in range(2):
                        P.dma("sp" if hh else "pool", YD.a[d, xt0:xt0 + n, g * 128 + hh * 64:g * 128 + hh * 64 + 64].rearrange("(c t) v -> t c v", t=CH), yst[hh * 64:hh * 64 + 64, 0:nch, hh * 64:hh * 64 + 64], reads=[yst], writes=[(YD, (d, g, hh, xt0))])


    items = []
    for g in range(4):
        for d in range(2):
            chain = (ctx_blocks + x_blocks) if d == 0 else (ctx_blocks + x_blocks[::-1])
            for bi_, (tok0, n) in enumerate(chain):
                items.append((g, d, tok0, n, bi_ == 0))
    N1s, N2s, Cs = [], [], []
    for k_, (g, d, tok0, n, first) in enumerate(items):
        Nl, N2l, Cl = [], [], []
        do_block(g, d, tok0, n, k_, first, Nl, N2l, Cl)
        N1s.append(Nl)
        N2s.append(N2l)
        Cs.append(Cl)
    P.defer = None

    def merge(*lists):
        lists = [l for l in lists if l]
        idx = [0] * len(lists)
        while True:
            best, bi_ = None, -1
            for i_, l in enumerate(lists):
                if idx[i_] < len(l):
                    frac = idx[i_] / len(l)
                    if best is None or frac < best:
                        best, bi_ = frac, i_
            if bi_ < 0:
                break
            P.replay([lists[bi_][idx[bi_]]])
            idx[bi_] += 1

    nit = len(items)
    P.replay(N1s[0])
    merge(N2s[0], N1s[1] if nit > 1 else [])
    for k_ in range(nit):
        merge(Cs[k_], N2s[k_ + 1] if k_ + 1 < nit else [], N1s[k_ + 2] if k_ + 2 < nit else [])

    if stop == 'C':
        return fin()
    P.pop()
    P.push()
    IOA = bass.IndirectOffsetOnAxis
    gupf = P.sb([128, DR], F32, "gupf")
    gupb = P.sb([128, DR], BF16, "gupb")
    P.dma("sp", gupf[:], gup_d.a, reads=[gup_d], writes=[gupf])
    P.op("pool", lambda e: e.tensor_copy(out=gupb[:], in_=gupf[:]), [gupf], [gupb])
    gng = P.sb([128, DR], F32, "gng")
    gnb = P.sb([128, DR], F32, "gnb")
    P.dma("pool", gng[:], gnrow_d.a[0:1, :].partition_broadcast(128), reads=[gnrow_d], writes=[gng])
    P.dma("pool", gnb[:], gnrow_d.a[1:2, :].partition_broadcast(128), reads=[gnrow_d], writes=[gnb])
    woutb = P.sb([128, 8, D], BF16, "woutb")
    wstage = P.sb([128, 4, D], F32, "wstage")
    wout_v = wout_d.a.rearrange("(k p) c -> p k c", p=128)
    for hf in range(2):
        P.dma("sp", wstage[:], wout_v[:, hf * 4:(hf + 1) * 4, :], reads=[wout_d], writes=[wstage])
        P.op("pool", lambda e, hf=hf: e.tensor_copy(out=woutb[:, hf * 4:(hf + 1) * 4, :], in_=wstage[:]), [wstage], [woutb])
    rwf = P.sb([128, 8, NE], F32, "rwf")
    P.dma("sp", rwf[:], rw_d.a.rearrange("(k p) c -> p k c", p=128), reads=[rw_d], writes=[rwf])
    omka2 = P.sb([128, 4], F32, "omka2")
    P.op("dve", lambda e: e.tensor_scalar(out=omka2[:], in0=kvec[:, 1, :], scalar1=-2.0, scalar2=2.0, op0=ALU.mult, op1=ALU.add), [kvec], [omka2])
    rkv4 = P.sb([128, 4, 3, NB], BF16, "rkv4")
    xgb = P.sb([128, NB], BF16, "xgb")
    sgb = P.sb([128, NB], BF16, "sgb")
    agf = P.sb([128, NB], F32, "agf")
    agb = P.sb([128, NB], F32, "agb")
    ksum = P.sb([128, NB], F32, "ksum")
    rkT = P.sb([128, 4, NB], BF16, "rkT")
    y0 = [P.sb([128, DR], F32, f"y0{i}") for i in range(2)]
    y1 = [P.sb([128, DR], F32, f"y1{i}") for i in range(2)]
    ysq = P.sb([128, DR], F32, "ysq")
    st1 = P.sb([128, 8], F32, "st1")
    st2 = P.sb([128, 8], F32, "st2")
    st3 = P.sb([128, 8], F32, "st3")
    rks = P.sb([128, 8], F32, "rks")
    bon = P.sb([128, DR], F32, "bon")
    axT = P.sb([128, 4, 128], BF16, "axT")
    xres = [P.sb([128, D], F32, f"xres{i}") for i in range(2)]
    x1t = P.sb([128, D], F32, "x1t")
    xh2f = P.sb([128, D], F32, "xh2f")
    xh2b = P.sb([128, D], BF16, "xh2b")
    hxT = P.sb([128, 8, 128], F32, "hxT")
    ssq2 = P.sb([128, 1], F32, "ssq2")
    std2 = P.sb([128, 1], F32, "std2")
    rstd2 = P.sb([128, 1], F32, "rstd2")
    lmx = P.sb([128, 1], F32, "lmx")
    lex = P.sb([128, NE], F32, "lex")
    lsum = P.sb([128, 1], F32, "lsum")
    headsel = cstb[:, 2, 0:128:64]
    psV, psG, psR, psAx, psO0, psO1, psH, psL = psT[0], psT[1], psT[2], psT[3], psA, psX[0], psX[1], psX[2]
    v38 = lambda ap: ap.rearrange("p (h v) -> p h v", v=HD)
    for bi in range(TX // NB):
        tokp = CL + bi * NB
        for q_ in range(3):
            P.dma(("sp", "pool", "sp")[q_], rkv4[:, :, q_, :], pxm_v[q_ * 512:(q_ + 1) * 512, tokp:tokp + NB].rearrange("(g c) t -> c g t", g=4), reads=[PXM], writes=[rkv4])
        P.dma("pool", wab[:, 0:NB], pxm_v[1536:1664, tokp:tokp + NB], reads=[PXM], writes=[wab])
        P.dma("pool", xgb[:], pxm_v[1664:1792, tokp:tokp + NB], reads=[PXM], writes=[xgb])
        P.op("act", lambda e: e.activation(out=sgb[:], in_=xgb[:], func=AF.Sigmoid), [xgb], [sgb])
        for g in range(4):
            for d, agt, pz in ((0, agf, psV), (1, agb, psG)):
                P.op("pe", lambda e, g=g, d=d, pz=pz: e.matmul(out=pz[:, 0:NB], lhsT=lorab[64:128, d, g * 128:(g + 1) * 128], rhs=wab[64:128, 0:NB], start=True, stop=True), [lorab, wab], [pz])
                P.op("act", lambda e, g=g, d=d, pz=pz, agt=agt: e.activation(out=agt[:], in_=pz[:, 0:NB], func=AF.Sigmoid, bias=wa0[:, d, 1, g:g + 1]), [pz, wa0], [agt])
            P.op("dve", lambda e: e.tensor_tensor(out=ksum[:], in0=agf[:], in1=agb[:], op=ALU.add), [agf, agb], [ksum])
            P.op("dve", lambda e, g=g: e.tensor_scalar(out=ksum[:], in0=ksum[:], scalar1=kvec[:, 1, g:g + 1], scalar2=omka2[:, g:g + 1], op0=ALU.mult, op1=ALU.add), [ksum, kvec, omka2], [ksum])
            P.op("pool", lambda e, g=g: e.tensor_tensor(out=ksum[:], in0=ksum[:], in1=rkv4[:, g, 1, :], op=ALU.mult), [ksum, rkv4], [ksum])
            P.op("dve", lambda e, g=g: e.scalar_tensor_tensor(out=rkT[:, g, :], in0=ksum[:], scalar=kvec[:, 2, g:g + 1], in1=rkv4[:, g, 0, :], op0=ALU.mult, op1=ALU.mult), [ksum, kvec, rkv4], [(rkT, g)])
        for ti in range(NB // 128):
            tt = bi * (NB // 128) + ti
            t0 = tt * 128
            ts = slice(ti * 128, (ti + 1) * 128)
            ya, yb, xr_ = y0[tt % 2], y1[tt % 2], xres[tt % 2]
            P.dma("sp", ya[:], YD.a[0, t0:t0 + 128, :], reads=[YD], writes=[ya])
            P.dma("pool", yb[:], YD.a[1, t0:t0 + 128, :], reads=[YD], writes=[yb])
            P.dma("sp", xr_[:], x_d.a[t0:t0 + 128, :], reads=[x_d], writes=[xr_])
            for g in range(4):
                P.op("pe", lambda e, g=g, ts=ts: e.matmul(out=psV[:, g * 128:(g + 1) * 128], lhsT=rkv4[:, g, 2, ts], rhs=identb, start=True, stop=True), [rkv4, cstb], [psV])
                P.op("pe", lambda e, g=g, ts=ts: e.matmul(out=psR[:, 2 * g:2 * g + 2], lhsT=rkT[:, g, ts], rhs=headsel, start=True, stop=True), [(rkT, g), cstb], [psR])
            P.op("pe", lambda e, ts=ts: e.matmul(out=psG[:, 0:DR], lhsT=sgb[:, ts], rhs=gupb[:], start=True, stop=True), [sgb, gupb], [psG])
            P.op("dve", lambda e, ya=ya, yb=yb: e.tensor_tensor(out=ya[:], in0=ya[:], in1=yb[:], op=ALU.add), [ya, yb], [ya])
            P.op("dve", lambda e, ya=ya: e.tensor_reduce(out=st1[:], in_=v38(ya[:]), axis=AX.X, op=ALU.add), [ya], [st1])
            P.op("act", lambda e, ya=ya: e.activation(out=ysq[:], in_=ya[:], func=AF.Square), [ya], [ysq])
            P.op("dve", lambda e: e.tensor_reduce(out=st2[:], in_=v38(ysq[:]), axis=AX.X, op=ALU.add), [ysq], [st2])
            P.op("dve", lambda e: e.tensor_scalar(out=st1[:], in0=st1[:], scalar1=1.0 / HD, scalar2=None, op0=ALU.mult), [st1], [st1])
            P.op("dve", lambda e: e.tensor_tensor(out=st3[:], in0=st1[:], in1=st1[:], op=ALU.mult), [st1], [st3])
            P.op("dve", lambda e: e.scalar_tensor_tensor(out=st2[:], in0=st2[:], scalar=1.0 / HD, in1=st3[:], op0=ALU.mult, op1=ALU.subtract), [st2, st3], [st2])
            P.op("act", lambda e: e.activation(out=st2[:], in_=st2[:], func=AF.Sqrt, bias=epsg[:]), [st2, epsg], [st2])
            P.op("dve", lambda e: e.reciprocal(out=st2[:], in_=st2[:]), [st2], [st2])
            P.op("dve", lambda e, ya=ya: e.tensor_tensor(out=v38(ya[:]), in0=v38(ya[:]), in1=st1[:].unsqueeze(2).to_broadcast([128, NH, HD]), op=ALU.subtract), [ya, st1], [ya])
            P.op("dve", lambda e, ya=ya: e.tensor_tensor(out=v38(ya[:]), in0=v38(ya[:]), in1=st2[:].unsqueeze(2).to_broadcast([128, NH, HD]), op=ALU.mult), [ya, st2], [ya])
            P.op("pool", lambda e, ya=ya: e.tensor_tensor(out=ya[:], in0=ya[:], in1=gng[:], op=ALU.mult), [ya, gng], [ya])
            P.op("pool", lambda e, ya=ya: e.tensor_tensor(out=ya[:], in0=ya[:], in1=gnb[:], op=ALU.add), [ya, gnb], [ya])
            P.op("act", lambda e: e.activation(out=rks[:], in_=psR[:, 0:8], func=AF.Copy), [psR], [rks])
            P.op("dve", lambda e: e.tensor_tensor(out=v38(bon[:]), in0=v38(psV[:, 0:DR]), in1=rks[:].unsqueeze(2).to_broadcast([128, NH, HD]), op=ALU.mult), [psV, rks], [bon])
            P.op("pool", lambda e, ya=ya: e.tensor_tensor(out=ya[:], in0=ya[:], in1=bon[:], op=ALU.add), [ya, bon], [ya])
            P.op("dve", lambda e, ya=ya: e.tensor_tensor(out=ya[:], in0=ya[:], in1=psG[:, 0:DR], op=ALU.mult), [ya, psG], [ya])
            for kc in range(4):
                P.op("pe", lambda e, kc=kc, ya=ya: e.transpose(out=psAx[:, kc * 128:(kc + 1) * 128], in_=ya[:, kc * 128:(kc + 1) * 128], identity=identf), [ya, cst], [psAx])
            P.op("act", lambda e: e.activation(out=axT[:].rearrange("p k t -> p (k t)"), in_=psAx[:], func=AF.Copy), [psAx], [axT])
            for hf, po_ in ((0, psO0), (1, psO1)):
                for kc in range(8):
                    lhs = axT[:, kc, :] if kc < 4 else bxT[:, kc - 4, t0:t0 + 128]
                    rdl = [axT] if kc < 4 else [(bxT, kc - 4)]
                    P.op("pe", lambda e, lhs=lhs, kc=kc, hf=hf, po_=po_: e.matmul(out=po_[:], lhsT=lhs, rhs=woutb[:, kc, hf * 512:(hf + 1) * 512], start=(kc == 0), stop=(kc == 7)), rdl + [woutb], [po_])
                P.op("dve", lambda e, hf=hf, po_=po_: e.tensor_tensor(out=x1t[:, hf * 512:(hf + 1) * 512], in0=po_[:], in1=g1bc[:, hf * 512:(hf + 1) * 512], op=ALU.mult), [po_, g1bc], [(x1t, hf)])
            P.op("pool", lambda e, xr_=xr_: e.tensor_tensor(out=x1t[:], in0=x1t[:], in1=xr_[:], op=ALU.add), [x1t, xr_], [x1t])
            P.dma("sp", X1.a[t0:t0 + 128, :], x1t[:], reads=[x1t], writes=[(X1, tt)])
            P.op("act", lambda e: e.activation(out=junk2[:], in_=x1t[:], func=AF.Square, accum_out=ssq2[:]), [x1t], [junk2, ssq2])
            P.op("act", lambda e: e.activation(out=std2[:], in_=ssq2[:], func=AF.Sqrt, bias=epsn[:], scale=1.0 / D), [ssq2, epsn], [std2])
            P.op("dve", lambda e: e.reciprocal(out=rstd2[:], in_=std2[:]), [std2], [rstd2])
            P.op("dve", lambda e: e.tensor_scalar(out=xh2f[:], in0=x1t[:], scalar1=rstd2[:, 0:1], scalar2=None, op0=ALU.mult), [x1t, rstd2], [xh2f])
            P.op("pool", lambda e: e.tensor_copy(out=xh2b[:], in_=xh2f[:]), [xh2f], [xh2b])
            P.dma("pool", XH2.a[t0:t0 + 128, :], xh2b[:], reads=[xh2b], writes=[(XH2, tt)])
            for k in range(8):
                pz = psH if k < 4 else psAx
                P.op("pe", lambda e, k=k, pz=pz: e.transpose(out=pz[:, (k % 4) * 128:(k % 4 + 1) * 128], in_=xh2f[:, k * 128:(k + 1) * 128], identity=identf), [xh2f, cst], [pz])
            for k in range(8):
                pz = psH if k < 4 else psAx
                P.op("act" if k >= 4 else "dve", (lambda e, k=k, pz=pz: e.activation(out=hxT[:, k, :], in_=pz[:, (k % 4) * 128:(k % 4 + 1) * 128], func=AF.Identity, bias=modT[:, 24 + k, 0:1], scale=sc2[:, k:k + 1])) if k >= 4 else (lambda e, k=k, pz=pz: e.tensor_scalar(out=hxT[:, k, :], in0=pz[:, (k % 4) * 128:(k % 4 + 1) * 128], scalar1=sc2[:, k:k + 1], scalar2=modT[:, 24 + k, 0:1], op0=ALU.mult, op1=ALU.add)), [pz, modT, sc2], [(hxT, k)])
            for k in range(8):
                P.op("pe", lambda e, k=k: e.matmul(out=psL[:, 0:NE], lhsT=hxT[:, k, :], rhs=rwf[:, k, :], start=(k == 0), stop=(k == 7)), [(hxT, k), rwf], [psL])
            P.op("dve", lambda e: e.tensor_reduce(out=lmx[:], in_=psL[:, 0:NE], axis=AX.X, op=ALU.max, negate=True), [psL], [lmx])
            P.op("act", lambda e: e.activation(out=lex[:], in_=psL[:, 0:NE], func=AF.Exp, bias=lmx[:], accum_out=lsum[:]), [psL, lmx], [lex, lsum])
            P.op("dve", lambda e: e.reciprocal(out=lsum[:], in_=lsum[:]), [lsum], [lsum])
            P.op("dve", lambda e, tt=tt: e.tensor_scalar(out=affall[:, tt, :], in0=lex[:], scalar1=lsum[:, 0:1], scalar2=None, op0=ALU.mult), [lex, lsum], [(affall, tt)])
    if debug:
        P.dma("sp", dbg["aff"].a, affall[:].rearrange("p t e -> p (t e)"), reads=[affall], writes=[dbg["aff"]])


    if stop == 'D':
        return fin()
    P.pop()
    P.pop()
    P.push()
    NTE = NTX * NE
    idxi_all = P.sb([128, NE, NST], I32, "idxi_all")
    valt_all = P.sb([128, NE, NST], F32, "valt_all")
    P.push()
    tokhl = P.sb([128, NTX, 2], F32, "tokhl")
    iot = P.sb([128, 512], F32, "iot")
    P.dma("sp", iot[:], iota_d.a[:, 0:512], reads=[iota_d], writes=[iot])
    P.dma("sp", tokhl[:].rearrange("p t c -> p (t c)"), tokhl_d.a, reads=[tokhl_d], writes=[tokhl])
    lo = P.sb([128, NE], F32, "lo")
    mid = P.sb([128, NE], F32, "mid")
    ge = P.sb([128, NE], F32, "ge")
    cntp = P.sb([128, NE], F32, "cntp")
    cmpt = P.sb([128, NTX, NE], F32, "cmpt")
    maskb = P.sb([128, NTX, NE], BF16, "maskb")
    wsel = P.sb([128, NTX, NE], F32, "wsel")
    offs = P.sb([128, NTX, NE], F32, "offs")
    pos = P.sb([128, NTX, NE], F32, "pos")
    P.op("dve", lambda e: e.memset(lo[:], 0.0), [], [lo])
    for k in range(30):
        h = 2.0 ** -(k + 1)
        P.op("dve", lambda e, h=h: e.tensor_scalar(out=mid[:], in0=lo[:], scalar1=h, scalar2=None, op0=ALU.add), [lo], [mid])
        P.op("dve", lambda e: e.tensor_tensor(out=cmpt[:], in0=affall[:], in1=mid[:].unsqueeze(1).to_broadcast([128, NTX, NE]), op=ALU.is_ge), [affall, mid], [cmpt])
        P.op("dve", lambda e: e.tensor_reduce(out=cntp[:], in_=cmpt[:].rearrange("p t e -> p e t"), axis=AX.X, op=ALU.add), [cmpt], [cntp])
        P.op("pe", lambda e: e.matmul(out=psL[:, 0:NE], lhsT=cst[:, 1, :], rhs=cntp[:], start=True, stop=True), [cst, cntp], [psL])
        P.op("dve", lambda e, h=h: e.tensor_scalar(out=ge[:], in0=psL[:, 0:NE], scalar1=float(CAP) - 0.5, scalar2=h, op0=ALU.is_ge, op1=ALU.mult), [psL], [ge])
        P.op("dve", lambda e: e.tensor_tensor(out=lo[:], in0=lo[:], in1=ge[:], op=ALU.add), [lo, ge], [lo])
    P.op("dve", lambda e: e.tensor_tensor(out=cmpt[:], in0=affall[:], in1=lo[:].unsqueeze(1).to_broadcast([128, NTX, NE]), op=ALU.is_ge), [affall, lo], [cmpt])
    P.op("pool", lambda e: e.tensor_copy(out=maskb[:], in_=cmpt[:]), [cmpt], [maskb])
    P.op("dve", lambda e: e.tensor_tensor(out=wsel[:], in0=cmpt[:], in1=affall[:], op=ALU.mult), [cmpt, affall], [wsel])
    P.op("pe", lambda e: e.matmul(out=psV[:, 0:NTE], lhsT=cstb[:, 8, :], rhs=maskb[:].rearrange("p t e -> p (t e)"), start=True, stop=True), [cstb, maskb], [psV])
    P.op("pe", lambda e: e.matmul(out=psG[:, 0:NTE], lhsT=onesb, rhs=maskb[:].rearrange("p t e -> p (t e)"), start=True, stop=True), [cstb, maskb], [psG])
    P.op("dve", lambda e: e.memset(offs[:, 0, :], 0.0), [], [offs])
    for tt in range(1, NTX):
        P.op("dve", lambda e, tt=tt: e.tensor_tensor(out=offs[:, tt, :], in0=offs[:, tt - 1, :], in1=psG[:, (tt - 1) * NE:tt * NE], op=ALU.add), [offs, psG], [offs])
    P.op("dve", lambda e: e.tensor_tensor(out=pos[:].rearrange("p t e -> p (t e)"), in0=offs[:].rearrange("p t e -> p (t e)"), in1=psV[:, 0:NTE], op=ALU.add), [offs, psV], [pos])
    OH2 = [P.sb([128, NTX, CAP], BF16, f"OH{i}") for i in range(2)]
    rhsE2 = [P.sb([128, NTX, 4], BF16, f"rhsE{i}") for i in range(2)]
    for rhsE in rhsE2:
        P.op("pool", lambda e, rhsE=rhsE: e.tensor_copy(out=rhsE[:, :, 0:2], in_=tokhl[:]), [tokhl], [rhsE])
    idxs = P.sb([128, 4 * NST], F32, "idxs")
    idxf = P.sb([128, NST], F32, "idxf")
    for ex in range(NE):
        OH = OH2[ex % 2]
        rhsE = rhsE2[ex % 2]
        P.op("dve", lambda e, ex=ex, rhsE=rhsE: e.tensor_copy(out=rhsE[:, :, 2], in_=wsel[:, :, ex]), [wsel], [rhsE])
        P.op("dve", lambda e, ex=ex, rhsE=rhsE: e.tensor_tensor(out=rhsE[:, :, 3], in0=wsel[:, :, ex], in1=rhsE[:, :, 2], op=ALU.subtract), [wsel, rhsE], [rhsE])
        for tt in range(NTX):
            P.op("dve" if tt % 4 else "pool", lambda e, tt=tt, ex=ex, OH=OH: e.tensor_scalar(out=OH[:, tt, :], in0=iot[:, 0:CAP], scalar1=pos[:, tt, ex:ex + 1], scalar2=cmpt[:, tt, ex:ex + 1], op0=ALU.is_equal, op1=ALU.mult), [iot, pos, cmpt], [(OH, tt)])
        for j in range(NST):
            for tt in range(NTX):
                P.op("pe", lambda e, j=j, tt=tt, OH=OH, rhsE=rhsE: e.matmul(out=psR[0:ST, 4 * j:4 * j + 4], lhsT=OH[:, tt, j * ST:(j + 1) * ST], rhs=rhsE[:, tt, :], start=(tt == 0), stop=(tt == NTX - 1)), [(OH, tt), rhsE], [psR])
        P.op("act", lambda e: e.activation(out=idxs[0:ST, :], in_=psR[0:ST, 0:4 * NST], func=AF.Copy), [psR], [idxs])
        i3 = idxs[0:ST, :].rearrange("p (j c) -> p j c", c=4)
        P.op("dve", lambda e, i3=i3: e.scalar_tensor_tensor(out=idxf[0:ST, :], in0=i3[:, :, 0], scalar=128.0, in1=i3[:, :, 1], op0=ALU.mult, op1=ALU.add), [idxs], [idxf])
        P.op("dve", lambda e, ex=ex: e.tensor_copy(out=idxi_all[0:ST, ex, :], in_=idxf[0:ST, :]), [idxf], [(idxi_all, ex)])
        P.op("dve", lambda e, i3=i3, ex=ex: e.tensor_tensor(out=valt_all[0:ST, ex, :], in0=i3[:, :, 2], in1=i3[:, :, 3], op=ALU.add), [idxs], [(valt_all, ex)])
    if stop == 'E1':
        return fin()
    P.pop()
    P.push()
    Xg = [P.sb([128, NST, D], BF16, f"Xg{i}") for i in range(2)]
    XeT = P.sb([128, 8, CAP], BF16, "XeT")
    hidT = P.sb([128, 8, CAP], BF16, "hidT")
    sgt = P.sb([128, CAP], F32, "sgt")
    NSTG = 4
    wstg = [P.sb([128, 2, D], F32, f"wstg{i}") for i in range(NSTG)]
    Wg2 = [P.sb([128, 8, D], BF16, f"Wg{i}") for i in range(2)]
    Wu2 = [P.sb([128, 8, D], BF16, f"Wu{i}") for i in range(2)]
    Wd1 = P.sb([128, 8, D], BF16, "Wd")
    rows = [P.sb([128, D], F32, f"rows{i}") for i in range(3)]
    wsrc = [wg_d, wu_d, wd_d]
    stg_c = [0]
    sc_i = 0

    def load_w(ex, which=(0, 1, 2)):
        for wi, dst in ((0, Wg2[ex % 2]), (1, Wu2[ex % 2]), (2, Wd1)):
            if wi not in which:
                continue
            wv = wsrc[wi].a[ex].rearrange("(k p) c -> p k c", p=128)
            for hq in range(4):
                stg = wstg[stg_c[0] % NSTG]
                stg_c[0] += 1
                P.dma("sp", stg[:], wv[:, hq * 2:(hq + 1) * 2, :], reads=[wsrc[wi]], writes=[stg])
                P.op("pool", lambda e, stg=stg, dst=dst, hq=hq: e.tensor_copy(out=dst[:, hq * 2:(hq + 1) * 2, :], in_=stg[:]), [stg], [(dst, hq // 2)])

    def gather(ex):
        for j in range(NST):
            P.op("pool", lambda e, j=j, ex=ex: e.indirect_dma_start(out=Xg[ex % 2][0:ST, j, :], out_offset=None, in_=XH2.a, in_offset=IOA(ap=idxi_all[0:ST, ex, j:j + 1], axis=0)), [XH2, (idxi_all, ex)], [(Xg[ex % 2], j)], dma=True)

    gather(0)
    load_w(0)
    for ex in range(NE):
        xg = Xg[ex % 2]
        Wg_, Wu_ = Wg2[ex % 2], Wu2[ex % 2]
        if ex + 1 < NE:
            gather(ex + 1)
            load_w(ex + 1, (0, 1))
        for k in range(8):
            pz = (psV, psG)[k % 2]
            for j in range(NST):
                P.op("pe", lambda e, k=k, j=j, pz=pz, xg=xg: e.matmul(out=pz[:, j * ST:(j + 1) * ST], lhsT=xg[0:ST, j, k * 128:(k + 1) * 128], rhs=cstb[0:ST, 0, 0:ST], start=True, stop=True), [(xg, j), cstb], [pz])
            P.op("dve" if k % 2 else "act", (lambda e, k=k, pz=pz: e.tensor_scalar(out=XeT[:, k, :], in0=pz[:, 0:CAP], scalar1=sc2[:, k:k + 1], scalar2=modT[:, 24 + k, 0:1], op0=ALU.mult, op1=ALU.add)) if k % 2 else (lambda e, k=k, pz=pz: e.activation(out=XeT[:, k, :], in_=pz[:, 0:CAP], func=AF.Identity, bias=modT[:, 24 + k, 0:1], scale=sc2[:, k:k + 1])), [pz, sc2, modT], [(XeT, k)])
        for fc in range(8):
            for k in range(8):
                P.op("pe", lambda e, fc=fc, k=k, Wg_=Wg_: e.matmul(out=psAx[:, 0:CAP], lhsT=Wg_[:, k, fc * 128:(fc + 1) * 128], rhs=XeT[:, k, :], start=(k == 0), stop=(k == 7)), [(Wg_, k // 4), (XeT, k)], [psAx])
            for k in range(8):
                P.op("pe", lambda e, fc=fc, k=k, Wu_=Wu_: e.matmul(out=psH[:, 0:CAP], lhsT=Wu_[:, k, fc * 128:(fc + 1) * 128], rhs=XeT[:, k, :], start=(k == 0), stop=(k == 7)), [(Wu_, k // 4), (XeT, k)], [psH])
            P.op("act", lambda e: e.activation(out=sgt[:], in_=psAx[:, 0:CAP], func=AF.Silu), [psAx], [sgt])
            P.op("dve", lambda e, fc=fc: e.tensor_tensor(out=hidT[:, fc, :], in0=sgt[:], in1=psH[:, 0:CAP], op=ALU.mult), [sgt, psH], [(hidT, fc)])
        for j in range(NST):
            rw_ = rows[sc_i % 3]
            sc_i += 1
            for hf, po_ in ((0, psO0), (1, psO1)):
                for fc in range(8):
                    P.op("pe", lambda e, j=j, hf=hf, fc=fc, po_=po_: e.matmul(out=po_[0:ST, :], lhsT=hidT[:, fc, j * ST:(j + 1) * ST], rhs=Wd1[:, fc, hf * 512:(hf + 1) * 512], start=(fc == 0), stop=(fc == 7)), [(hidT, fc), (Wd1, fc // 4)], [po_])
                P.op("dve", lambda e, j=j, hf=hf, po_=po_, rw_=rw_, ex=ex: e.scalar_tensor_tensor(out=rw_[0:ST, hf * 512:(hf + 1) * 512], in0=po_[0:ST, :], scalar=valt_all[0:ST, ex, j:j + 1], in1=g2bc[0:ST, hf * 512:(hf + 1) * 512], op0=ALU.mult, op1=ALU.mult), [po_, (valt_all, ex), g2bc], [(rw_, hf)])
            P.op("pool", lambda e, j=j, ex=ex, rw_=rw_: e.indirect_dma_start(out=X1.a, out_offset=IOA(ap=idxi_all[0:ST, ex, j:j + 1], axis=0), in_=rw_[0:ST, :], in_offset=None, compute_op=ALU.add), [rw_, (idxi_all, ex)], [X1], dma=True)
        if ex + 1 < NE:
            load_w(ex + 1, (2,))
    P.pop()

    if stop == 'E':
        return fin()
    P.pop()
    P.push()
    fx = [P.sb([128, D], F32, f"fx{i}") for i in range(2)]
    fo = [P.sb([128, D], F32, f"fo{i}") for i in range(2)]
    fs = P.sb([128, 1], F32, "fs")
    fd = P.sb([128, 1], F32, "fd")
    fr = P.sb([128, 1], F32, "fr")
    for tt in range(NTX):
        t0 = tt * 128
        a, o_ = fx[tt % 2], fo[tt % 2]
        P.dma("sp", a[:], X1.a[t0:t0 + 128, :], reads=[X1], writes=[a])
        P.op("act", lambda e, a=a: e.activation(out=junk2[:], in_=a[:], func=AF.Square, accum_out=fs[:]), [a], [junk2, fs])
        P.op("act", lambda e: e.activation(out=fd[:], in_=fs[:], func=AF.Sqrt, bias=epsn[:], scale=1.0 / D), [fs, epsn], [fd])
        P.op("dve", lambda e: e.reciprocal(out=fr[:], in_=fd[:]), [fd], [fr])
        P.op("dve", lambda e, a=a, o_=o_: e.scalar_tensor_tensor(out=o_[:], in0=a[:], scalar=fr[:, 0:1], in1=fgbc[:], op0=ALU.mult, op1=ALU.mult), [a, fr, fgbc], [o_])
        P.dma("pool", out_d.a[t0:t0 + 128, :], o_[:], reads=[o_], writes=[(out_d, tt)])

    P.pop()
    stats = P.emit()
    return nc, stats


def host_consts():
    c = np.zeros((128, 9, 128), np.float32)
    i = np.arange(128)
    c[:, 0, :] = np.eye(128)
    c[:, 1, :] = 1.0
    c[:, 2, :] = (i[:, None] // 64 == i[None, :] // 64)
    a, b = i[:, None] % 64, i[None, :] % 64
    c[:, 3, :] = a > b
    c[:, 4, :] = a < b
    c[:, 5, :] = a <= b
    c[:, 6, :] = a >= b
    c[:, 7, :] = (i[:, None] // 64 == i[None, :] // 64)
    c[:, 8, :] = i[:, None] < i[None, :]
    return c


def col_perm():
    cols = []
    for Q in range(3):
        for q in range(4):
            cols += [Q * 512 + 4 * j + q for j in range(128)]
    for qa in (0, 2):
        cols += [1536 + 4 * j + qa for j in range(64)]
        cols += [1536 + 4 * j + qa + 1 for j in range(64)]
    for i in range(4):
        pass
    cols += list(range(1792, 3328))
    return np.array(cols)


def prep_core(inp, b):
    f = lambda a: np.ascontiguousarray(a, dtype=np.float32)
    perm = col_perm()
    m = {}
    m["x"] = f(inp["x"][b])
    m["ctx"] = f(inp["ctx"][b])
    cT = np.zeros((128, 16), np.float32)
    cT[:, 0:8] = inp["c"][b].reshape(8, 128).T
    cT[:, 8:16] = inp["c_ctx"].reshape(8, 128).T
    m["cT"] = cT
    m["ada_w"] = f(inp["ada_w"][0])
    m["adabT"] = f(inp["ada_b"][0].reshape(48, 128).T)
    gnT = np.zeros((128, 24), np.float32)
    gnT[:, 0:8] = inp["norm1_g"][0].reshape(8, 128).T
    gnT[:, 8:16] = inp["norm2_g"][0].reshape(8, 128).T
    m["gnT"] = gnT
    m["final_g"] = f(inp["final_g"].reshape(1, D))
    m["w_in_p"] = f(inp["w_in"][0][:, perm])
    mu = inp["shift_mu"][0]
    m["muT"] = f(mu[perm[:1792]].reshape(14, 128).T)
    lw = np.zeros((2, 128, DR), np.float32)
    lw[:, 0:64] = inp["w_lora_up"][0]
    lw[:, 64:128] = inp["a_lora_up"][0]
    m["loraW"] = lw
    wa0 = np.zeros((128, 2, 2, 4), np.float32)
    for d in range(2):
        wa0[:, d, 0, :] = inp["w0"][0, d].reshape(4, 128).T
        wa0[:, d, 1, :] = inp["a0"][0, d].reshape(4, 128).T
    m["wa0T"] = wa0
    kv = np.zeros((128, 3, 4), np.float32)
    kv[:, 0, :] = inp["k_k"][0].reshape(4, 128).T
    kv[:, 1, :] = inp["k_a"][0].reshape(4, 128).T
    kv[:, 2, :] = inp["r_k"][0].reshape(4, 128).T
    m["kvecT"] = kv
    m["g_up"] = f(inp["g_lora_up"][0])
    m["gnrow"] = f(np.stack([inp["gn_g"][0], inp["gn_b"][0]]))
    m["cwT"] = f(inp["conv_w"][0].reshape(3, 4, 128).transpose(2, 1, 0))
    m["w_out"] = f(inp["w_out"][0])
    m["router_w"] = f(inp["router_w"][0])
    m["exp_w_gate"] = f(inp["exp_w_gate"][0])
    m["exp_w_up"] = f(inp["exp_w_up"][0])
    m["exp_w_down"] = f(inp["exp_w_down"][0])
    m["consts"] = host_consts()
    io = np.zeros((128, 514), np.float32)
    io[:, 0:512] = np.arange(512)[None, :]
    m["iotas"] = io
    rm = np.ones((128, 512), np.float32)
    rm[:, ::64] = 0.0
    m["rmask"] = rm
    ntx = m["x"].shape[0] // 128
    th = np.zeros((128, ntx, 2), np.float32)
    th[:, :, 0] = np.arange(ntx)[None, :]
    th[:, :, 1] = np.arange(128)[:, None]
    m["tokhl"] = th.reshape(128, ntx * 2)
    return m


def kernel(**inputs):
    nc, _ = build()
    in_maps = [prep_core(inputs, b) for b in range(8)]
    res = run_bass_kernel_spmd(nc, in_maps, core_ids=list(range(8)))
    return np.stack([np.asarray(r["out"], dtype=np.float32) for r in res.results], axis=0)
```

```python
import numpy as np
import ml_dtypes
from contextlib import ExitStack
import concourse.bass as bass
import concourse.mybir as mybir
from concourse.bass_utils import run_bass_kernel_spmd

F32 = mybir.dt.float32
BF16 = mybir.dt.bfloat16
I32 = mybir.dt.int32
U32 = mybir.dt.uint32
AF = mybir.ActivationFunctionType
ALU = mybir.AluOpType
AX = mybir.AxisListType

COMPUTE = ("pe", "act", "dve", "pool")
QUEUES = ("sp", "act", "pool")
NDMA_SEM = 8


class T:
    def __init__(self, handle, name):
        self.h = handle
        self.name = name
        self.st = {}

    def __getitem__(self, k):
        return self.h[k]


class Op:
    __slots__ = ("eng", "fn", "deps", "is_dma", "sig", "signo", "dsem", "dval", "idx", "bar")

    def __init__(self, eng, fn, is_dma):
        self.eng = eng
        self.fn = fn
        self.deps = []
        self.is_dma = is_dma
        self.sig = False
        self.signo = None
        self.dsem = None
        self.dval = None
        self.bar = False


class Prog:
    def __init__(self, nc):
        self.nc = nc
        self.ops = []
        self.ntile = 0
        self.stacks = []
        self.defer = None

    def sb(self, shape, dtype, name="t"):
        self.ntile += 1
        if self.stacks:
            h = self.stacks[-1].enter_context(self.nc.sbuf_tensor(f"{name}_{self.ntile}", list(shape), dtype))
        else:
            h = self.nc.alloc_sbuf_tensor(f"{name}_{self.ntile}", list(shape), dtype)
        return T(h, name)

    def scope(self):
        prog = self

        class _Scope:
            def __enter__(s):
                prog.stacks.append(ExitStack())

            def __exit__(s, *a):
                prog.barrier()
                prog.stacks.pop().close()
                return False
        return _Scope()

    def push(self):
        self.stacks.append(ExitStack())

    def pop(self):
        self.barrier()
        self.stacks.pop().close()

    def barrier(self):
        o = Op(None, None, False)
        o.bar = True
        self.ops.append(o)

    def ps(self, shape, dtype=F32, name="p"):
        self.ntile += 1
        return T(self.nc.alloc_psum_tensor(f"{name}_{self.ntile}", list(shape), dtype), name)

    def dram(self, name, shape, dtype, kind="Internal"):
        h = self.nc.dram_tensor(name, list(shape), dtype, kind=kind)
        t = T(h, name)
        t.a = h.ap()
        return t

    @staticmethod
    def _norm(acc):
        return [(a, None) if isinstance(a, T) else a for a in acc]

    def replay(self, lst):
        for (eng, fn, reads, writes, dma) in lst:
            self.op(eng, fn, reads, writes, dma)

    def op(self, eng, fn, reads=(), writes=(), dma=False):
        if self.defer is not None:
            self.defer.append((eng, fn, reads, writes, dma))
            return None
        o = Op(eng, fn, dma)
        deps = set()

        def matching(t, key):
            if key is None:
                return list(t.st.values())
            res = []
            if key in t.st:
                res.append(t.st[key])
            if None in t.st:
                res.append(t.st[None])
            return res

        reads = self._norm(reads)
        writes = self._norm(writes)
        for (t, key) in reads:
            for ent in matching(t, key):
                if ent[0] is not None:
                    deps.add(ent[0])
        for (t, key) in writes:
            for ent in matching(t, key):
                if ent[0] is not None:
                    deps.add(ent[0])
                deps.update(ent[1].values())
                deps.update(ent[2])
        for (t, key) in reads:
            ent = t.st.setdefault(key, [None, {}, []])
            if dma:
                ent[2].append(o)
            else:
                ent[1][eng] = o
        for (t, key) in writes:
            if key is None:
                t.st = {None: [o, {}, []]}
            else:
                t.st[key] = [o, {}, []]
        deps.discard(o)
        for d in deps:
            if (not d.is_dma) and (not o.is_dma) and d.eng == o.eng and d.eng == "pe":
                continue
            o.deps.append(d)
            d.sig = True
        self.ops.append(o)
        return o

    def dma(self, q, out, in_, reads=(), writes=(), **kw):
        return self.op(q, lambda e: e.dma_start(out=out, in_=in_, **kw), reads, writes, dma=True)

    def emit(self):
        nc = self.nc
        sems = {e: nc.alloc_semaphore(name=f"sem_{e}") for e in COMPUTE}
        dsems = {q: [nc.alloc_semaphore(name=f"dsem_{q}_{i}") for i in range(NDMA_SEM)] for q in QUEUES}
        cnt = {e: 0 for e in COMPUTE}
        dcnt = {q: 0 for q in QUEUES}
        lastop = {}
        for o in self.ops:
            if o.bar:
                for lo in lastop.values():
                    lo.sig = True
            elif not o.is_dma:
                lastop[o.eng] = o
        dlast = {}
        for o in self.ops:
            if o.bar:
                o.deps = (dict(cnt), dict(dlast))
                continue
            if o.is_dma:
                i = dcnt[o.eng]
                dcnt[o.eng] += 1
                o.dsem = dsems[o.eng][i % NDMA_SEM]
                o.dval = 16 * (i // NDMA_SEM + 1)
                dlast[id(o.dsem)] = (o.dsem, o.dval)
            elif o.sig:
                cnt[o.eng] += 1
                o.signo = cnt[o.eng]
        per_eng = {e: [] for e in set(COMPUTE) | set(QUEUES)}
        for o in self.ops:
            if o.bar:
                for e in per_eng:
                    per_eng[e].append(o)
            else:
                per_eng[o.eng].append(o)

        def run_engine(ename, eng):
            waited = {}

            def wait(sem, val):
                k = id(sem)
                if waited.get(k, 0) >= val:
                    return
                waited[k] = val
                eng.wait_ge(sem, val)

            last_dma = {}
            for o in per_eng[ename]:
                if o.bar:
                    for (ce, v) in o.deps[0].items():
                        if v > 0:
                            wait(sems[ce], v)
                    for (s, v) in o.deps[1].values():
                        wait(s, v)
                    continue
                for d in o.deps:
                    if d.is_dma:
                        wait(d.dsem, d.dval)
                    else:
                        wait(sems[d.eng], d.signo)
                if o.is_dma:
                    if o.dval > 16:
                        wait(o.dsem, o.dval - 16)
                    ins = o.fn(eng)
                    ins.then_inc(o.dsem, 16)
                    last_dma[id(o.dsem)] = (o.dsem, o.dval)
                else:
                    ins = o.fn(eng)
                    if o.sig:
                        ins.then_inc(sems[ename], 1)
            for (s, v) in last_dma.values():
                wait(s, v)

        with nc.Block() as block:
            @block.tensor
            def _(e):
                run_engine("pe", e)

            @block.scalar
            def _(e):
                run_engine("act", e)

            @block.vector
            def _(e):
                run_engine("dve", e)

            @block.gpsimd
            def _(e):
                run_engine("pool", e)

            @block.sync
            def _(e):
                run_engine("sp", e)
        return {e: len([o for o in v if not o.bar]) for e, v in per_eng.items()}


D = 1024
DR = 512
NH = 8
HD = 64
NE = 16
C0 = float(np.exp(-0.5))
GN_EPS = 64e-5
NORM_EPS = 1e-6
CH = 64
import os as _os
YVAR = int(_os.environ.get('YVAR', '0'))


def build(TX=4096, CL=256, debug=False, stop=None):
    nc = bass.Bass("TRN2", target_bir_lowering=False)
    P = Prog(nc)
    TT = CL + TX
    NTX = TX // 128
    CAP = 2 * TX // NE
    ST = min(128, CAP)
    NST = CAP // ST
    GW = 64
    ROWS = TX // GW

    def fin():
        while P.stacks:
            P.pop()
        return nc, P.emit()

    def din(name, shape, dt=F32):
        return P.dram(name, shape, dt, kind="ExternalInput")

    x_d = din("x", [TX, D])
    ctx_d = din("ctx", [CL, D])
    cT_d = din("cT", [128, 16])
    adaw_d = din("ada_w", [D, 6 * D])
    adab_d = din("adabT", [128, 48])
    gn_d = din("gnT", [128, 24])
    finalg_d = din("final_g", [1, D])
    win_d = din("w_in_p", [D, 3328])
    mu_d = din("muT", [128, 14])
    lora_d = din("loraW", [2, 128, DR])
    wa0_d = din("wa0T", [128, 2, 2, 4])
    kvec_d = din("kvecT", [128, 3, 4])
    gup_d = din("g_up", [128, DR])
    gnrow_d = din("gnrow", [2, DR])
    cw_d = din("cwT", [128, 4, 3])
    wout_d = din("w_out", [D, D])
    rw_d = din("router_w", [D, NE])
    wg_d = din("exp_w_gate", [NE, D, D])
    wu_d = din("exp_w_up", [NE, D, D])
    wd_d = din("exp_w_down", [NE, D, D])
    cst_d = din("consts", [128, 9, 128])
    iota_d = din("iotas", [128, 512 + 2])
    rmask_d = din("rmask", [128, 512])
    tokhl_d = din("tokhl", [128, (TX // 128) * 2])
    out_d = P.dram("out", [TX, D], F32, kind="ExternalOutput")
    dbg = {}
    if debug:
        dbg["pxm"] = P.dram("dbg_pxm", [1792, TT], BF16, kind="ExternalOutput")
        dbg["bx"] = P.dram("dbg_bx", [512, TX], F32, kind="ExternalOutput")
        dbg["y"] = P.dram("dbg_y", [2, TX, DR], F32, kind="ExternalOutput")
        dbg["x1"] = P.dram("dbg_x1", [TX, D], F32, kind="ExternalOutput")
        dbg["aff"] = P.dram("dbg_aff", [128, NTX * NE], F32, kind="ExternalOutput")

    PXM = dbg["pxm"] if debug else P.dram("pxm", [1792, TT], BF16)
    YD = dbg["y"] if debug else P.dram("yd", [2, TX, DR], F32)
    X1 = dbg["x1"] if debug else P.dram("x1", [TX, D], F32)
    XH2 = P.dram("xh2", [TX, D], BF16)

    cst = P.sb([128, 9, 128], F32, "cst")
    identf = cst[:, 0, :]
    cstb = P.sb([128, 9, 128], BF16, "cstb")
    identb = cstb[:, 0, :]
    onesb = cstb[:, 1, :]
    blkones = cstb[:, 2, :]
    modT = P.sb([128, 48, 2], F32, "modT")
    gnT = P.sb([128, 24], F32, "gnT")
    sc1x = P.sb([128, 8], F32, "sc1x")
    sc1c = P.sb([128, 8], F32, "sc1c")
    sc2 = P.sb([128, 8], F32, "sc2")
    epsn = P.sb([128, 1], F32, "epsn")
    epsg = P.sb([128, 1], F32, "epsg")
    g1bc = P.sb([128, D], F32, "g1bc")
    g2bc = P.sb([128, D], F32, "g2bc")
    fgbc = P.sb([128, D], F32, "fgbc")
    affall = P.sb([128, NTX, NE], F32, "affall")
    junk2 = P.sb([128, D], F32, "junk2")
    P.push()
    bxT = P.sb([128, 4, TX], BF16, "bxT")

    q2 = ["sp", "pool"]
    P.dma("sp", cst[:], cst_d.a, reads=[cst_d], writes=[cst])
    P.op("dve", lambda e: e.tensor_copy(out=cstb[:], in_=cst[:]), [cst], [cstb])
    P.dma("sp", gnT[:], gn_d.a, reads=[gn_d], writes=[gnT])
    P.op("dve", lambda e: e.memset(epsn[:], NORM_EPS), [], [epsn])
    P.op("dve", lambda e: e.memset(epsg[:], GN_EPS), [], [epsg])
    P.dma("pool", fgbc[:], finalg_d.a.partition_broadcast(128), reads=[finalg_d], writes=[fgbc])

    P.push()
    cT = P.sb([128, 16], F32, "cT")
    scT = P.sb([128, 16], F32, "scT")
    adab = P.sb([128, 48], F32, "adab")
    P.dma("sp", cT[:], cT_d.a, reads=[cT_d], writes=[cT])
    P.dma("sp", adab[:], adab_d.a, reads=[adab_d], writes=[adab])
    P.op("act", lambda e: e.activation(out=scT[:], in_=cT[:], func=AF.Silu), [cT], [scT])
    adaw = [P.sb([128, 8, 512], F32, f"adaw{i}") for i in range(2)]
    psA = P.ps([128, 512], F32, "psA")
    adaw_v = adaw_d.a.rearrange("(k p) c -> p k c", p=128)
    for jb in range(12):
        aw = adaw[jb % 2]
        P.dma(q2[jb % 2], aw[:], adaw_v[:, :, jb * 512:(jb + 1) * 512], reads=[adaw_d], writes=[aw])
        for jj in range(4):
            j = jb * 4 + jj
            for k in range(8):
                P.op("pe", lambda e, aw=aw, jj=jj, j=j, k=k: e.matmul(out=psA[:, 2 * j:2 * j + 2], lhsT=aw[:, k, jj * 128:(jj + 1) * 128], rhs=scT[:, k::8], start=(k == 0), stop=(k == 7)), [aw, scT], [psA])
    P.op("dve", lambda e: e.tensor_tensor(out=modT[:], in0=psA[:, 0:96].rearrange("p (j m) -> p j m", m=2), in1=adab[:].unsqueeze(2).to_broadcast([128, 48, 2]), op=ALU.add), [psA, adab], [modT])
    for (dst, gofs, cofs, m) in ((sc1x, 0, 8, 0), (sc1c, 0, 8, 1), (sc2, 8, 32, 0)):
        P.op("dve", lambda e, dst=dst, gofs=gofs, cofs=cofs, m=m: e.scalar_tensor_tensor(out=dst[:], in0=modT[:, cofs:cofs + 8, m], scalar=1.0, in1=gnT[:, gofs:gofs + 8], op0=ALU.add, op1=ALU.mult), [modT, gnT], [dst])
    dg = P.sb([128, 8, 128], F32, "dg")
    for (dst, ofs) in ((g1bc, 16), (g2bc, 40)):
        for k in range(8):
            P.op("dve", lambda e, k=k, ofs=ofs: e.tensor_scalar(out=dg[:, k, :], in0=identf, scalar1=modT[:, ofs + k, 0:1], scalar2=None, op0=ALU.mult), [cst, modT], [dg])
        for hf in range(2):
            P.op("pe", lambda e, hf=hf: e.matmul(out=psA[:], lhsT=cst[:, 1, :], rhs=dg[:, hf * 4:(hf + 1) * 4, :].rearrange("p k c -> p (k c)"), start=True, stop=True), [cst, dg], [psA])
            P.op("act", lambda e, hf=hf, dst=dst: e.activation(out=dst[:, hf * 512:(hf + 1) * 512], in_=psA[:], func=AF.Copy), [psA], [dst])

    if stop == 'A':
        return fin()
    P.pop()
    P.push()
    h1T = P.sb([128, 8, TT], BF16, "h1T")
    P.push()
    xin = [P.sb([128, 4, D], F32, f"xin{i}") for i in range(2)]
    xhf = [P.sb([128, 4, D], F32, f"xhf{i}") for i in range(2)]
    junk = P.sb([128, D], F32, "junk")
    ssq = P.sb([128, 4], F32, "ssq")
    stdv = P.sb([128, 4], F32, "stdv")
    rstd = P.sb([128, 4], F32, "rstd")
    psT = [P.ps([128, 512], F32, f"psT{i}") for i in range(4)]
    groups = [(ctx_d, t0, min(512, CL - t0), t0) for t0 in range(0, CL, 512)] + [(x_d, t0, 512, CL + t0) for t0 in range(0, TX, 512)]
    for gi, (src, t0, n, dst0) in enumerate(groups):
        nt = n // 128
        xi = xin[gi % 2]
        xh = xhf[gi % 2]
        isctx = src is ctx_d
        P.dma(q2[gi % 2], xi[:, 0:nt, :], src.a[t0:t0 + n, :].rearrange("(t p) d -> p t d", p=128), reads=[src], writes=[xi])
        for t in range(nt):
            P.op("act", lambda e, t=t, xi=xi: e.activation(out=junk[:], in_=xi[:, t, :], func=AF.Square, accum_out=ssq[:, t:t + 1]), [xi], [junk, ssq])
        P.op("act", lambda e, nt=nt: e.activation(out=stdv[:, 0:nt], in_=ssq[:, 0:nt], func=AF.Sqrt, bias=epsn[:], scale=1.0 / D), [ssq, epsn], [stdv])
        P.op("dve", lambda e, nt=nt: e.reciprocal(out=rstd[:, 0:nt], in_=stdv[:, 0:nt]), [stdv], [rstd])
        for t in range(nt):
            P.op("dve" if t % 2 else "pool", lambda e, t=t, xi=xi, xh=xh: e.tensor_scalar(out=xh[:, t, :], in0=xi[:, t, :], scalar1=rstd[:, t:t + 1], scalar2=None, op0=ALU.mult), [xi, rstd], [(xh, t)])
        for k in range(8):
            pt = psT[k % 4]
            for t in range(nt):
                P.op("pe", lambda e, t=t, k=k, pt=pt, xh=xh: e.transpose(out=pt[:, t * 128:(t + 1) * 128], in_=xh[:, t, k * 128:(k + 1) * 128], identity=identf), [(xh, t), cst], [pt])
            scv = sc1c if isctx else sc1x
            m = 1 if isctx else 0
            if k % 2:
                P.op("act", lambda e, k=k, pt=pt, n=n, dst0=dst0, scv=scv, m=m: e.activation(out=h1T[:, k, dst0:dst0 + n], in_=pt[:, 0:n], func=AF.Identity, bias=modT[:, k, m:m + 1], scale=scv[:, k:k + 1]), [pt, modT, scv], [(h1T, gi)])
            else:
                P.op("dve", lambda e, k=k, pt=pt, n=n, dst0=dst0, scv=scv, m=m: e.tensor_scalar(out=h1T[:, k, dst0:dst0 + n], in0=pt[:, 0:n], scalar1=scv[:, k:k + 1], scalar2=modT[:, k, m:m + 1], op0=ALU.mult, op1=ALU.add), [pt, modT, scv], [(h1T, gi)])

    P.pop()
    P.push()
    muT = P.sb([128, 14], F32, "muT")
    omuT = P.sb([128, 14], F32, "omuT")
    cwT = P.sb([128, 4, 3], F32, "cwT")
    P.dma("sp", muT[:], mu_d.a, reads=[mu_d], writes=[muT])
    P.dma("sp", cwT[:], cw_d.a, reads=[cw_d], writes=[cwT])
    P.op("dve", lambda e: e.tensor_scalar(out=omuT[:], in0=muT[:], scalar1=-1.0, scalar2=1.0, op0=ALU.mult, op1=ALU.add), [muT], [omuT])
    wst = [P.sb([128, 8, 128], F32, f"wst{i}") for i in range(2)]
    wbf = [P.sb([128, 8, 128], BF16, f"wbf{i}") for i in range(2)]
    pxc = P.sb([128, TT], F32, "pxc")
    pxo = [P.sb([128, TT], BF16, f"pxo{i}") for i in range(2)]
    cvb = P.sb([128, TX], BF16, "cvb")
    cvc = P.sb([128, TX], F32, "cvc")
    cva = pxc
    win_v = win_d.a.rearrange("(k p) c -> p k c", p=128)
    tok_groups = [(dst0, n) for (_, _, n, dst0) in groups]
    pxm_v = PXM.a

    def project(cc, col0, toks, sink):
        w = wst[cc % 2]
        wb = wbf[cc % 2]
        P.dma(q2[cc % 2], w[:], win_v[:, :, col0:col0 + 128], reads=[win_d], writes=[w])
        P.op("pool", lambda e, w=w, wb=wb: e.tensor_copy(out=wb[:], in_=w[:]), [w], [wb])
        for gi, (tok0, n) in enumerate(toks):
            pt = psT[gi % 4]
            for k in range(8):
                P.op("pe", lambda e, k=k, pt=pt, wb=wb, tok0=tok0, n=n: e.matmul(out=pt[:, 0:n], lhsT=wb[:, k, :], rhs=h1T[:, k, tok0:tok0 + n], start=(k == 0), stop=(k == 7)), [wb, h1T], [pt])
            sink(gi, pt, tok0, n)

    for ci in range(14):
        def sink(gi, pt, tok0, n):
            P.op("act" if gi % 2 else "dve", (lambda e, pt=pt, tok0=tok0, n=n: e.activation(out=pxc[:, tok0:tok0 + n], in_=pt[:, 0:n], func=AF.Copy)) if gi % 2 else (lambda e, pt=pt, tok0=tok0, n=n: e.tensor_copy(out=pxc[:, tok0:tok0 + n], in_=pt[:, 0:n])), [pt], [(pxc, gi)])
        project(ci, ci * 128, tok_groups, sink)
        po = pxo[ci % 2]
        P.op("act", lambda e, ci=ci, po=po: e.activation(out=po[:], in_=pxc[:], func=AF.Copy, scale=omuT[:, ci:ci + 1]), [pxc, omuT], [po])
        if ci < 12:
            parts = [(0, 128, ci % 4)]
        else:
            parts = [(0, 64, 0 if ci == 12 else 2), (64, 128, 1 if ci == 12 else 3)]
        for (p0, p1, q) in parts:
            mu_s = muT[p0:p1, ci:ci + 1]
            Xv = pxc[p0:p1, CL:TT].rearrange("p (r c) -> p r c", c=GW)
            Ov = po[p0:p1, CL:TT].rearrange("p (r c) -> p r c", c=GW)
            if q == 0:
                oa, ia = Ov[:, :, 1:], Xv[:, :, :-1]
            elif q == 1:
                oa, ia = Ov[:, :, :-1], Xv[:, :, 1:]
            elif q == 2:
                oa, ia = po[p0:p1, CL + GW:TT], pxc[p0:p1, CL:TT - GW]
            else:
                oa, ia = po[p0:p1, CL:TT - GW], pxc[p0:p1, CL + GW:TT]
            P.op("dve", lambda e, oa=oa, ia=ia, mu_s=mu_s: e.scalar_tensor_tensor(out=oa, in0=ia, scalar=mu_s, in1=oa, op0=ALU.mult, op1=ALU.add), [pxc, po, muT], [po])
            if q % 2 == 0:
                oc, ic = po[p0:p1, 1:CL], pxc[p0:p1, 0:CL - 1]
            else:
                oc, ic = po[p0:p1, 0:CL - 1], pxc[p0:p1, 1:CL]
            P.op("dve", lambda e, oc=oc, ic=ic, mu_s=mu_s: e.scalar_tensor_tensor(out=oc, in0=ic, scalar=mu_s, in1=oc, op0=ALU.mult, op1=ALU.add), [pxc, po, muT], [po])
        if ci < 12:
            Q, q = ci // 4, ci % 4
            P.dma("sp", pxm_v[Q * 512 + q:(Q + 1) * 512:4, :], po[:], reads=[po], writes=[(PXM, ci)])
        else:
            qa = 0 if ci == 12 else 2
            P.dma("sp", pxm_v[1536 + qa:1792:4, :], po[0:64, :], reads=[po], writes=[(PXM, ci)])
            P.dma("sp", pxm_v[1536 + qa + 1:1792:4, :], po[64:128, :], reads=[po], writes=[(PXM, ci)])

    x_groups = [(tok0, n) for (tok0, n) in tok_groups if tok0 >= CL]
    for i in range(4):
        def sink_b(gi, pt, tok0, n):
            P.op("act", lambda e, pt=pt, tok0=tok0, n=n: e.activation(out=cvb[:, tok0 - CL:tok0 - CL + n], in_=pt[:, 0:n], func=AF.Copy), [pt], [(cvb, gi)])
        project(14 + i * 3, 1792 + i * 128, x_groups, sink_b)

        def sink_c(gi, pt, tok0, n):
            P.op("act", lambda e, pt=pt, tok0=tok0, n=n: e.activation(out=cvc[:, tok0 - CL:tok0 - CL + n], in_=pt[:, 0:n], func=AF.Copy), [pt], [(cvc, gi)])
        project(15 + i * 3, 1792 + 512 + i * 128, x_groups, sink_c)

        def sink_u(gi, pt, tok0, n):
            P.op("dve", lambda e, pt=pt, tok0=tok0, n=n: e.tensor_tensor(out=cvc[:, tok0 - CL:tok0 - CL + n], in0=cvc[:, tok0 - CL:tok0 - CL + n], in1=pt[:, 0:n], op=ALU.mult), [pt, (cvc, gi)], [(cvc, gi)])
        project(16 + i * 3, 1792 + 1024 + i * 128, x_groups, sink_u)
        P.op("act", lambda e, i=i: e.activation(out=cva[:, 0:TX], in_=cvc[:], func=AF.Copy, scale=cwT[:, i, 1:2]), [cvc, cwT], [cva])
        P.op("dve", lambda e, i=i: e.scalar_tensor_tensor(out=cva[:, 1:TX], in0=cvc[:, 0:TX - 1], scalar=cwT[:, i, 0:1], in1=cva[:, 1:TX], op0=ALU.mult, op1=ALU.add), [cvc, cva, cwT], [cva])
        P.op("dve", lambda e, i=i: e.scalar_tensor_tensor(out=cva[:, 0:TX - 1], in0=cvc[:, 1:TX], scalar=cwT[:, i, 2:3], in1=cva[:, 0:TX - 1], op0=ALU.mult, op1=ALU.add), [cvc, cva, cwT], [cva])
        P.op("pool", lambda e, i=i: e.tensor_tensor(out=bxT[:, i, :], in0=cva[:, 0:TX], in1=cvb[:], op=ALU.mult), [cva, cvb], [(bxT, i)])
        if debug:
            P.op("pool", lambda e: e.tensor_tensor(out=cva[:, 0:TX], in0=cva[:, 0:TX], in1=cvb[:], op=ALU.mult), [cva, cvb], [cva])
            P.dma("sp", dbg["bx"].a[i * 128:(i + 1) * 128, :], cva[:, 0:TX], reads=[cva], writes=[dbg["bx"]])


    if stop == 'B':
        return fin()
    P.pop()
    P.pop()
    NB = 512
    lorab = P.sb([128, 2, DR], BF16, "lorab")
    wa0 = P.sb([128, 2, 2, 4], F32, "wa0")
    kvec = P.sb([128, 3, 4], F32, "kvec")
    omka = P.sb([128, 4], F32, "omka")
    wab = P.sb([128, NB], BF16, "wab")
    P.push()
    psX = [P.ps([128, 512], F32, f"psX{i}") for i in range(3)]
    rmask = P.sb([128, 512], F32, "rmask")
    P.dma("sp", rmask[:], rmask_d.a, reads=[rmask_d], writes=[rmask])
    loraf = P.sb([128, 2, DR], F32, "loraf")
    P.dma("sp", loraf[:], lora_d.a.rearrange("d p c -> p d c"), reads=[lora_d], writes=[loraf])
    P.op("pool", lambda e: e.tensor_copy(out=lorab[:], in_=loraf[:]), [loraf], [lorab])
    P.dma("sp", wa0[:], wa0_d.a, reads=[wa0_d], writes=[wa0])
    P.dma("sp", kvec[:], kvec_d.a, reads=[kvec_d], writes=[kvec])
    P.op("dve", lambda e: e.tensor_scalar(out=omka[:], in0=kvec[:, 1, :], scalar1=-1.0, scalar2=1.0, op0=ALU.mult, op1=ALU.add), [kvec], [omka])
    mpair = P.sb([128, 2, 2, 128], F32, "mpair")
    P.op("dve", lambda e: e.tensor_copy(out=mpair[:, 0, 0, :], in_=cst[:, 4, :]), [cst], [mpair])
    P.op("dve", lambda e: e.tensor_copy(out=mpair[:, 0, 1, :], in_=cst[:, 5, :]), [cst], [mpair])
    P.op("dve", lambda e: e.tensor_copy(out=mpair[:, 1, 0, :], in_=cst[:, 3, :]), [cst], [mpair])
    P.op("dve", lambda e: e.tensor_copy(out=mpair[:, 1, 1, :], in_=cst[:, 6, :]), [cst], [mpair])
    NB = 512
    rkv = P.sb([128, 3, NB], BF16, "rkv")
    twb = P.sb([64, NB], BF16, "twb")
    s_t = P.sb([128, NB], F32, "s_t")
    ag_t = P.sb([128, NB], F32, "ag_t")
    Pc = P.sb([128, NB], F32, "Pc")
    cin = P.sb([128, NB], F32, "cin")
    cex = P.sb([128, NB], F32, "cex")
    tmc = P.sb([128, NB], F32, "tmc")
    E1 = P.sb([128, NB], F32, "E1")
    E2 = P.sb([128, NB], F32, "E2")
    E3 = P.sb([128, NB], F32, "E3")
    E4 = P.sb([128, NB], F32, "E4")
    GC2 = [P.sb([128, 8], F32, f"GC{i}") for i in range(4)]
    kk = cex
    sqb = P.sb([128, NB], BF16, "sqb")
    nrm = tmc
    kkn = P.sb([128, NB], F32, "kkn")
    b_t = P.sb([128, NB], F32, "b_t")
    kd_t = cin
    ARbd2 = [P.sb([128, 8, 2, 128], BF16, f"ARbd{i}") for i in range(4)]
    BKbd2 = [P.sb([128, 8, 2, 128], BF16, f"BKbd{i}") for i in range(2)]
    HVbd = P.sb([128, 8, 3, 128], BF16, "HVbd")
    TM2 = [P.sb([128, 8, 3, 128], BF16, f"TM{i}") for i in range(4)]
    for tz in (ARbd2[0], ARbd2[1], ARbd2[2], ARbd2[3], BKbd2[0], BKbd2[1], HVbd):
        P.op("pool", lambda e, tz=tz: e.memset(tz[:], 0.0), [], [tz])
    Sx2 = [[[P.sb([128, 3, 128], BF16, f"S{c}_{i}_{b}") for i in range(2)] for c in range(8)] for b in range(2)]
    Lts2 = [[P.sb([128, 3, 128], BF16, f"Lt{c}_{i}") for c in range(8)] for i in range(3)]
    TTs2 = [[P.sb([128, 128], BF16, f"TT{c}_{i}") for c in range(8)] for i in range(2)]
    Qb = P.sb([128, 128], BF16, "Qb")
    Ub = P.sb([128, 128], BF16, "Ub")
    Hf = P.sb([128, 128], F32, "Hf")
    Hb = P.sb([128, 128], BF16, "Hb")
    Yst = [P.sb([128, 8, 128], F32, f"Yst{i}") for i in range(2)]
    psZw, psZa, psSS, psTM = psT[0], psT[1], psT[2], psT[3]
    psLA, psLB, psN, psC = psA, psX[0], psX[1], psX[2]
    pxm3 = pxm_v[0:1536, :].rearrange("(q c) t -> c q t", q=3)
    ctx_blocks = [(0, CL)]
    x_blocks = [(CL + i * NB, NB) for i in range(TX // NB)]
    ev = ["dve", "act"]
    def do_block(g, d, tok0, n, kk_, first, Nl, N2l, N3l, Cl):
                P.defer = Nl
                par = kk_ % 2
                ARbd, TM, GC = ARbd2[kk_ % 4], TM2[kk_ % 4], GC2[kk_ % 4]
                Sx = Sx2[par]
                BKbd = BKbd2[par]
                Lts, TTs = Lts2[kk_ % 3], TTs2[par]
                Mx = cst[:, 3, :] if d == 0 else cst[:, 4, :]
                nch = n // CH
                isx = tok0 >= CL
                yst = Yst[par]
                P.dma("sp", rkv[:, :, 0:n], pxm3[g * 128:(g + 1) * 128, :, tok0:tok0 + n], reads=[PXM], writes=[rkv])
                P.dma("pool", wab[:, 0:n], pxm_v[1536:1664, tok0:tok0 + n], reads=[PXM], writes=[wab])
                P.op("act", lambda e, n=n: e.activation(out=twb[:, 0:n], in_=wab[0:64, 0:n], func=AF.Tanh), [wab], [twb])
                P.op("pe", lambda e, n=n, g=g, d=d: e.matmul(out=psZw[:, 0:n], lhsT=lorab[0:64, d, g * 128:(g + 1) * 128], rhs=twb[0:64, 0:n], start=True, stop=True), [lorab, twb], [psZw])
                P.op("act", lambda e, n=n, g=g, d=d: e.activation(out=s_t[:, 0:n], in_=psZw[:, 0:n], func=AF.Sigmoid, bias=wa0[:, d, 0, g:g + 1]), [psZw, wa0], [s_t])
                P.op("pe", lambda e, n=n, g=g, d=d: e.matmul(out=psZw[:, 0:n], lhsT=lorab[64:128, d, g * 128:(g + 1) * 128], rhs=wab[64:128, 0:n], start=True, stop=True), [lorab, wab], [psZw])
                P.op("act", lambda e, n=n, g=g, d=d: e.activation(out=ag_t[:, 0:n], in_=psZw[:, 0:n], func=AF.Sigmoid, bias=wa0[:, d, 1, g:g + 1]), [psZw, wa0], [ag_t])
                P.op("dve", lambda e, n=n: e.tensor_tensor_scan(out=Pc[:, 0:n], data0=rmask[:, 0:n], data1=s_t[:, 0:n], initial=0.0, op0=ALU.mult, op1=ALU.add), [rmask, s_t], [Pc])
                v3 = lambda t_, n=n: t_[:, 0:n].rearrange("p (c t) -> p c t", t=CH)
                totb = v3(Pc)[:, :, CH - 1:CH].to_broadcast([128, nch, CH])
                if d == 0:
                    P.op("pool", lambda e, n=n: e.tensor_tensor(out=cex[:, 0:n], in0=Pc[:, 0:n], in1=s_t[:, 0:n], op=ALU.subtract), [Pc, s_t], [cex])
                    cin_ = Pc
                else:
                    P.op("dve", lambda e, totb=totb, v3=v3: e.tensor_tensor(out=v3(cex), in0=totb, in1=v3(Pc), op=ALU.subtract), [Pc], [cex])
                    P.op("pool", lambda e, n=n: e.tensor_tensor(out=cin[:, 0:n], in0=cex[:, 0:n], in1=s_t[:, 0:n], op=ALU.add), [cex, s_t], [cin])
                    cin_ = cin
                P.op("dve", lambda e, totb=totb, v3=v3, cin_=cin_: e.tensor_tensor(out=v3(tmc), in0=totb, in1=v3(cin_), op=ALU.subtract), [Pc, cin_], [tmc])
                P.op("act", lambda e, n=n, cin_=cin_: e.activation(out=E1[:, 0:n], in_=cin_[:, 0:n], func=AF.Exp, scale=-C0), [cin_], [E1])
                P.op("act", lambda e, n=n: e.activation(out=E2[:, 0:n], in_=cex[:, 0:n], func=AF.Exp, scale=-C0), [cex], [E2])
                P.op("act", lambda e, n=n, cin_=cin_: e.activation(out=E3[:, 0:n], in_=cin_[:, 0:n], func=AF.Exp, scale=C0), [cin_], [E3])
                P.op("act", lambda e, n=n: e.activation(out=E4[:, 0:n], in_=tmc[:, 0:n], func=AF.Exp, scale=-C0), [tmc], [E4])
                P.op("act", lambda e, nch=nch, v3=v3: e.activation(out=GC[:, 0:nch], in_=v3(Pc)[:, :, CH - 1], func=AF.Exp, scale=-C0), [Pc], [GC])
                P.op("dve", lambda e, n=n, g=g: e.tensor_scalar(out=kk[:, 0:n], in0=rkv[:, 1, 0:n], scalar1=kvec[:, 0, g:g + 1], scalar2=None, op0=ALU.mult), [rkv, kvec], [kk])
                P.op("pool", lambda e, n=n: e.tensor_tensor(out=sqb[:, 0:n], in0=kk[:, 0:n], in1=kk[:, 0:n], op=ALU.mult), [kk], [sqb])
                P.op("pe", lambda e, n=n: e.matmul(out=psZw[:, 0:n], lhsT=blkones, rhs=sqb[:, 0:n], start=True, stop=True), [cstb, sqb], [psZw])
                P.op("act", lambda e, n=n: e.activation(out=nrm[:, 0:n], in_=psZw[:, 0:n], func=AF.Sqrt), [psZw], [nrm])
                P.op("dve", lambda e, n=n: e.tensor_scalar(out=nrm[:, 0:n], in0=nrm[:, 0:n], scalar1=1e-12, scalar2=None, op0=ALU.max), [nrm], [nrm])
                P.op("dve", lambda e, n=n: e.reciprocal(out=nrm[:, 0:n], in_=nrm[:, 0:n]), [nrm], [nrm])
                P.op("dve", lambda e, n=n: e.tensor_tensor(out=kkn[:, 0:n], in0=kk[:, 0:n], in1=nrm[:, 0:n], op=ALU.mult), [kk, nrm], [kkn])
                P.op("pool", lambda e, n=n: e.tensor_tensor(out=b_t[:, 0:n], in0=kkn[:, 0:n], in1=ag_t[:, 0:n], op=ALU.mult), [kkn, ag_t], [b_t])
                P.op("dve", lambda e, n=n, g=g: e.tensor_scalar(out=kd_t[:, 0:n], in0=ag_t[:, 0:n], scalar1=kvec[:, 1, g:g + 1], scalar2=omka[:, g:g + 1], op0=ALU.mult, op1=ALU.add), [ag_t, kvec, omka], [kd_t])
                P.op("dve", lambda e, n=n: e.tensor_tensor(out=kd_t[:, 0:n], in0=kd_t[:, 0:n], in1=rkv[:, 1, 0:n], op=ALU.mult), [kd_t, rkv], [kd_t])
                oi = 0
                for hh in range(2):
                    ps_ = slice(hh * 64, hh * 64 + 64)
                    v3h = lambda t_, n=n, ps_=ps_: t_[ps_, 0:n].rearrange("p (c t) -> p c t", t=CH)
                    r3 = rkv[ps_, 0, 0:n].rearrange("p (c t) -> p c t", t=CH)
                    vv3 = rkv[ps_, 2, 0:n].rearrange("p (c t) -> p c t", t=CH)
                    specs = [
                        (ARbd, 0, None, kkn, E2, -1.0), (ARbd, 1, r3, None, E1, None),
                        (BKbd, 0, None, b_t, E3, None), (BKbd, 1, None, kd_t, E3, None),
                        (HVbd, 1, None, b_t, E4, None), (HVbd, 2, None, kd_t, E4, None),
                    ]
                    for (dst, slot, a_ap, a_t, e_t, neg) in specs:
                        o_ap = dst[ps_, 0:nch, slot, hh * 64:hh * 64 + 64]
                        in0 = a_ap if a_ap is not None else v3h(a_t)
                        rd = [e_t] + ([rkv] if a_t is None else [a_t])
                        eng = ("dve", "pool")[oi % 2]
                        oi += 1
                        if neg is not None:
                            P.op("dve", lambda e, o_ap=o_ap, in0=in0, e_t=e_t, v3h=v3h: e.scalar_tensor_tensor(out=o_ap, in0=in0, scalar=-1.0, in1=v3h(e_t), op0=ALU.mult, op1=ALU.mult), rd, [(dst, slot)])
                        else:
                            P.op(eng, lambda e, o_ap=o_ap, in0=in0, e_t=e_t, v3h=v3h: e.tensor_tensor(out=o_ap, in0=in0, in1=v3h(e_t), op=ALU.mult), rd, [(dst, slot)])
                    P.op("pool", lambda e, hh=hh, ps_=ps_, vv3=vv3, nch=nch: e.tensor_copy(out=HVbd[ps_, 0:nch, 0, hh * 64:hh * 64 + 64], in_=vv3), [rkv], [(HVbd, 0)])
                for c in range(nch):
                    for j in range(3):
                        P.op("pe", lambda e, c=c, j=j: e.matmul(out=psTM[:, j * 128:(j + 1) * 128], lhsT=HVbd[:, c, j, :], rhs=identb, start=True, stop=True), [HVbd, cstb], [psTM])
                    P.op(ev[c % 2], (lambda e, c=c: e.activation(out=TM[:, c, :, :].rearrange("p j t -> p (j t)"), in_=psTM[:, 0:384], func=AF.Copy)) if ev[c % 2] == "act" else (lambda e, c=c: e.tensor_copy(out=TM[:, c, :, :].rearrange("p j t -> p (j t)"), in_=psTM[:, 0:384])), [psTM], [(TM, c)])
                P.defer = N2l
                for c in range(nch):
                    AT = ARbd[:, c, 0, :]
                    AR2 = ARbd[:, c, :, :].rearrange("p j t -> p (j t)")
                    bA, bB = psLA, psLB
                    S0 = Sx[c][0]
                    Lc = Lts[c]
                    P.op("pe", lambda e, AT=AT, c=c, bA=bA: e.matmul(out=bA[:, 0:128], lhsT=AT, rhs=BKbd[:, c, 0, :], start=True, stop=True), [ARbd, BKbd], [bA])
                    P.op("pe", lambda e, AR2=AR2, c=c, bA=bA: e.matmul(out=bA[:, 128:384], lhsT=BKbd[:, c, 0, :], rhs=AR2, start=True, stop=True), [ARbd, BKbd], [bA])
                    P.op("pe", lambda e, AR2=AR2, c=c, bB=bB: e.matmul(out=bB[:, 0:256], lhsT=BKbd[:, c, 1, :], rhs=AR2, start=True, stop=True), [ARbd, BKbd], [bB])
                    P.op("dve", lambda e, Mx=Mx, S0=S0, bA=bA: e.tensor_tensor(out=S0[:, 2, :], in0=bA[:, 0:128], in1=Mx, op=ALU.mult), [bA, cst], [S0])
                    P.op("dve", lambda e, d=d, S0=S0, bA=bA: e.tensor_tensor(out=S0[:, 0, :], in0=bA[:, 128:256], in1=mpair[:, d, 0, :], op=ALU.mult), [bA, mpair], [S0])
                    P.op("pool", lambda e, S0=S0: e.tensor_copy(out=S0[:, 1, :], in_=identb), [cstb], [S0])
                    P.op("dve", lambda e, d=d, Lc=Lc, bA=bA: e.tensor_tensor(out=Lc[:, 0, :], in0=bA[:, 256:384], in1=mpair[:, d, 1, :], op=ALU.mult), [bA, mpair], [Lc])
                    P.op("dve", lambda e, d=d, Lc=Lc, bB=bB: e.tensor_tensor(out=Lc[:, 1:3, :].rearrange("p j t -> p (j t)"), in0=bB[:, 0:256], in1=mpair[:, d, :, :].rearrange("p j t -> p (j t)"), op=ALU.mult), [bB, mpair], [Lc])
                P.defer = N3l
                NBK = (psN, psZa, psSS)
                for lv in range(6):
                    for c in range(nch):
                        cur, nxt = Sx[c][lv % 2], Sx[c][(lv + 1) % 2]
                        pn = NBK[c % 3]
                        engn = ("act", "act", "dve")[c % 3]
                        if lv < 5:
                            P.op("pe", lambda e, cur=cur, pn=pn: e.matmul(out=pn[:, 0:128], lhsT=cur[:, 2, :], rhs=cur[:, 0, :], start=True, stop=True), [cur], [pn])
                            P.op("pe", lambda e, cur=cur, pn=pn: e.matmul(out=pn[:, 256:384], lhsT=cur[:, 0, :], rhs=cur[:, 2, :], start=True, stop=True), [cur], [pn])
                        P.op("pe", lambda e, cur=cur, pn=pn: e.matmul(out=pn[:, 128:256], lhsT=cur[:, 2, :], rhs=cur[:, 1, :], start=True, stop=False), [cur], [pn])
                        P.op("pe", lambda e, cur=cur, pn=pn: e.matmul(out=pn[:, 128:256], lhsT=identb, rhs=cur[:, 1, :], start=False, stop=True), [cur, cstb], [pn])
                        if lv < 5:
                            P.op(engn, (lambda e, nxt=nxt, pn=pn: e.activation(out=nxt[:].rearrange("p j t -> p (j t)"), in_=pn[:, 0:384], func=AF.Copy)) if engn == "act" else (lambda e, nxt=nxt, pn=pn: e.tensor_copy(out=nxt[:].rearrange("p j t -> p (j t)"), in_=pn[:, 0:384])), [pn], [nxt])
                        else:
                            TTc = TTs[c]
                            P.op(engn, (lambda e, TTc=TTc, pn=pn: e.activation(out=TTc[:], in_=pn[:, 128:256], func=AF.Copy)) if engn == "act" else (lambda e, TTc=TTc, pn=pn: e.tensor_copy(out=TTc[:], in_=pn[:, 128:256])), [pn], [TTc])
                P.defer = Cl
                if first:
                    P.op("dve", lambda e: e.memset(Hf[:], 0.0), [], [Hf])
                    P.op("dve", lambda e: e.memset(Hb[:], 0.0), [], [Hb])
                corder = range(nch) if d == 0 else range(nch - 1, -1, -1)
                for c in corder:
                    AT = ARbd[:, c, 0, :]
                    RT = ARbd[:, c, 1, :]
                    Lc = Lts[c]
                    TTc = TTs[c]
                    Vb = TM[:, c, 0, :]
                    P.op("pe", lambda e, AT=AT: e.matmul(out=psC[:, 0:128], lhsT=AT, rhs=Hb[:], start=True, stop=False), [ARbd, Hb], [psC])
                    P.op("pe", lambda e, Vb=Vb, Lc=Lc: e.matmul(out=psC[:, 0:128], lhsT=Lc[:, 1, :], rhs=Vb, start=False, stop=True), [Lc, (TM, c)], [psC])
                    P.op("dve", lambda e: e.tensor_copy(out=Qb[:], in_=psC[:, 0:128]), [psC], [Qb])
                    P.op("pe", lambda e, TTc=TTc: e.matmul(out=psC[:, 128:256], lhsT=TTc[:], rhs=Qb[:], start=True, stop=True), [TTc, Qb], [psC])
                    P.op("dve", lambda e: e.tensor_copy(out=Ub[:], in_=psC[:, 128:256]), [psC], [Ub])
                    if isx:
                        P.op("pe", lambda e, RT=RT: e.matmul(out=psC[:, 256:384], lhsT=RT, rhs=Hb[:], start=True, stop=False), [ARbd, Hb], [psC])
                        P.op("pe", lambda e, Lc=Lc: e.matmul(out=psC[:, 256:384], lhsT=Lc[:, 0, :], rhs=Ub[:], start=False, stop=False), [Lc, Ub], [psC])
                        P.op("pe", lambda e, Vb=Vb, Lc=Lc: e.matmul(out=psC[:, 256:384], lhsT=Lc[:, 2, :], rhs=Vb, start=False, stop=True), [Lc, (TM, c)], [psC])
                    P.op("pe", lambda e, c=c: e.matmul(out=psC[:, 384:512], lhsT=TM[:, c, 1, :], rhs=Ub[:], start=True, stop=False), [(TM, c), Ub], [psC])
                    P.op("pe", lambda e, c=c, Vb=Vb: e.matmul(out=psC[:, 384:512], lhsT=TM[:, c, 2, :], rhs=Vb, start=False, stop=True), [(TM, c)], [psC])
                    P.op("dve", lambda e, c=c: e.scalar_tensor_tensor(out=Hb[:], in0=Hf[:], scalar=GC[:, c:c + 1], in1=psC[:, 384:512], op0=ALU.mult, op1=ALU.add), [Hf, GC, psC], [Hb])
                    P.op("dve", lambda e, c=c: e.scalar_tensor_tensor(out=Hf[:], in0=Hf[:], scalar=GC[:, c:c + 1], in1=psC[:, 384:512], op0=ALU.mult, op1=ALU.add), [Hf, GC, psC], [Hf])
                    if isx:
                        P.op("dve", lambda e, c=c, yst=yst: e.tensor_copy(out=yst[:, c, :], in_=psC[:, 256:384]), [psC], [yst])
                if isx:
                    xt0 = tok0 - CL
                    for hh in range(2):
                        P.dma("sp" if hh else "pool", YD.a[d, xt0:xt0 + n, g * 128 + hh * 64:g * 128 + hh * 64 + 64].rearrange("(c t) v -> t c v", t=CH), yst[hh * 64:hh * 64 + 64, 0:nch, hh * 64:hh * 64 + 64], reads=[yst], writes=[(YD, (d, g, hh, xt0))])


    items = []
    for g in range(4):
        for d in range(2):
            chain = (ctx_blocks + x_blocks) if d == 0 else (ctx_blocks + x_blocks[::-1])
            for bi_, (tok0, n) in enumerate(chain):
                items.append((g, d, tok0, n, bi_ == 0))
    N1s, N2s, N3s, Cs = [], [], [], []
    for k_, (g, d, tok0, n, first) in enumerate(items):
        Nl, N2l, N3l, Cl = [], [], [], []
        do_block(g, d, tok0, n, k_, first, Nl, N2l, N3l, Cl)
        N1s.append(Nl)
        N2s.append(N2l)
        N3s.append(N3l)
        Cs.append(Cl)
    P.defer = None

    def merge(*lists):
        lists = [l for l in lists if l]
        idx = [0] * len(lists)
        while True:
            best, bi_ = None, -1
            for i_, l in enumerate(lists):
                if idx[i_] < len(l):
                    frac = idx[i_] / len(l)
                    if best is None or frac < best:
                        best, bi_ = frac, i_
            if bi_ < 0:
                break
            P.replay([lists[bi_][idx[bi_]]])
            idx[bi_] += 1

    nit = len(items)
    gl = lambda L, i_: L[i_] if 0 <= i_ < nit else []
    for k_ in range(-3, nit):
        merge(gl(Cs, k_), gl(N3s, k_ + 1), gl(N2s, k_ + 2), gl(N1s, k_ + 3))

    if stop == 'C':
        return fin()
    P.pop()
    P.push()
    IOA = bass.IndirectOffsetOnAxis
    gupf = P.sb([128, DR], F32, "gupf")
    gupb = P.sb([128, DR], BF16, "gupb")
    P.dma("sp", gupf[:], gup_d.a, reads=[gup_d], writes=[gupf])
    P.op("pool", lambda e: e.tensor_copy(out=gupb[:], in_=gupf[:]), [gupf], [gupb])
    gng = P.sb([128, DR], F32, "gng")
    gnb = P.sb([128, DR], F32, "gnb")
    P.dma("pool", gng[:], gnrow_d.a[0:1, :].partition_broadcast(128), reads=[gnrow_d], writes=[gng])
    P.dma("pool", gnb[:], gnrow_d.a[1:2, :].partition_broadcast(128), reads=[gnrow_d], writes=[gnb])
    woutb = P.sb([128, 8, D], BF16, "woutb")
    wstage = P.sb([128, 4, D], F32, "wstage")
    wout_v = wout_d.a.rearrange("(k p) c -> p k c", p=128)
    for hf in range(2):
        P.dma("sp", wstage[:], wout_v[:, hf * 4:(hf + 1) * 4, :], reads=[wout_d], writes=[wstage])
        P.op("pool", lambda e, hf=hf: e.tensor_copy(out=woutb[:, hf * 4:(hf + 1) * 4, :], in_=wstage[:]), [wstage], [woutb])
    rwf = P.sb([128, 8, NE], F32, "rwf")
    P.dma("sp", rwf[:], rw_d.a.rearrange("(k p) c -> p k c", p=128), reads=[rw_d], writes=[rwf])
    omka2 = P.sb([128, 4], F32, "omka2")
    P.op("dve", lambda e: e.tensor_scalar(out=omka2[:], in0=kvec[:, 1, :], scalar1=-2.0, scalar2=2.0, op0=ALU.mult, op1=ALU.add), [kvec], [omka2])
    rkv4 = P.sb([128, 4, 3, NB], BF16, "rkv4")
    xgb = P.sb([128, NB], BF16, "xgb")
    sgb = P.sb([128, NB], BF16, "sgb")
    agf = P.sb([128, NB], F32, "agf")
    agb = P.sb([128, NB], F32, "agb")
    ksum = P.sb([128, NB], F32, "ksum")
    rkT = P.sb([128, 4, NB], BF16, "rkT")
    y0 = [P.sb([128, DR], F32, f"y0{i}") for i in range(2)]
    y1 = [P.sb([128, DR], F32, f"y1{i}") for i in range(2)]
    ysq = P.sb([128, DR], F32, "ysq")
    st1 = P.sb([128, 8], F32, "st1")
    st2 = P.sb([128, 8], F32, "st2")
    st3 = P.sb([128, 8], F32, "st3")
    rks = P.sb([128, 8], F32, "rks")
    bon = P.sb([128, DR], F32, "bon")
    axT = P.sb([128, 4, 128], BF16, "axT")
    xres = [P.sb([128, D], F32, f"xres{i}") for i in range(2)]
    x1t = P.sb([128, D], F32, "x1t")
    xh2f = P.sb([128, D], F32, "xh2f")
    xh2b = P.sb([128, D], BF16, "xh2b")
    hxT = P.sb([128, 8, 128], F32, "hxT")
    ssq2 = P.sb([128, 1], F32, "ssq2")
    std2 = P.sb([128, 1], F32, "std2")
    rstd2 = P.sb([128, 1], F32, "rstd2")
    lmx = P.sb([128, 1], F32, "lmx")
    lex = P.sb([128, NE], F32, "lex")
    lsum = P.sb([128, 1], F32, "lsum")
    headsel = cstb[:, 2, 0:128:64]
    psV, psG, psR, psAx, psO0, psO1, psH, psL = psT[0], psT[1], psT[2], psT[3], psA, psX[0], psX[1], psX[2]
    v38 = lambda ap: ap.rearrange("p (h v) -> p h v", v=HD)
    for bi in range(TX // NB):
        tokp = CL + bi * NB
        for q_ in range(3):
            P.dma(("sp", "pool", "sp")[q_], rkv4[:, :, q_, :], pxm_v[q_ * 512:(q_ + 1) * 512, tokp:tokp + NB].rearrange("(g c) t -> c g t", g=4), reads=[PXM], writes=[rkv4])
        P.dma("pool", wab[:, 0:NB], pxm_v[1536:1664, tokp:tokp + NB], reads=[PXM], writes=[wab])
        P.dma("pool", xgb[:], pxm_v[1664:1792, tokp:tokp + NB], reads=[PXM], writes=[xgb])
        P.op("act", lambda e: e.activation(out=sgb[:], in_=xgb[:], func=AF.Sigmoid), [xgb], [sgb])
        for g in range(4):
            for d, agt, pz in ((0, agf, psV), (1, agb, psG)):
                P.op("pe", lambda e, g=g, d=d, pz=pz: e.matmul(out=pz[:, 0:NB], lhsT=lorab[64:128, d, g * 128:(g + 1) * 128], rhs=wab[64:128, 0:NB], start=True, stop=True), [lorab, wab], [pz])
                P.op("act", lambda e, g=g, d=d, pz=pz, agt=agt: e.activation(out=agt[:], in_=pz[:, 0:NB], func=AF.Sigmoid, bias=wa0[:, d, 1, g:g + 1]), [pz, wa0], [agt])
            P.op("dve", lambda e: e.tensor_tensor(out=ksum[:], in0=agf[:], in1=agb[:], op=ALU.add), [agf, agb], [ksum])
            P.op("dve", lambda e, g=g: e.tensor_scalar(out=ksum[:], in0=ksum[:], scalar1=kvec[:, 1, g:g + 1], scalar2=omka2[:, g:g + 1], op0=ALU.mult, op1=ALU.add), [ksum, kvec, omka2], [ksum])
            P.op("pool", lambda e, g=g: e.tensor_tensor(out=ksum[:], in0=ksum[:], in1=rkv4[:, g, 1, :], op=ALU.mult), [ksum, rkv4], [ksum])
            P.op("dve", lambda e, g=g: e.scalar_tensor_tensor(out=rkT[:, g, :], in0=ksum[:], scalar=kvec[:, 2, g:g + 1], in1=rkv4[:, g, 0, :], op0=ALU.mult, op1=ALU.mult), [ksum, kvec, rkv4], [(rkT, g)])
        for ti in range(NB // 128):
            tt = bi * (NB // 128) + ti
            t0 = tt * 128
            ts = slice(ti * 128, (ti + 1) * 128)
            ya, yb, xr_ = y0[tt % 2], y1[tt % 2], xres[tt % 2]
            P.dma("sp", ya[:], YD.a[0, t0:t0 + 128, :], reads=[YD], writes=[ya])
            P.dma("pool", yb[:], YD.a[1, t0:t0 + 128, :], reads=[YD], writes=[yb])
            P.dma("sp", xr_[:], x_d.a[t0:t0 + 128, :], reads=[x_d], writes=[xr_])
            for g in range(4):
                P.op("pe", lambda e, g=g, ts=ts: e.matmul(out=psV[:, g * 128:(g + 1) * 128], lhsT=rkv4[:, g, 2, ts], rhs=identb, start=True, stop=True), [rkv4, cstb], [psV])
                P.op("pe", lambda e, g=g, ts=ts: e.matmul(out=psR[:, 2 * g:2 * g + 2], lhsT=rkT[:, g, ts], rhs=headsel, start=True, stop=True), [(rkT, g), cstb], [psR])
            P.op("pe", lambda e, ts=ts: e.matmul(out=psG[:, 0:DR], lhsT=sgb[:, ts], rhs=gupb[:], start=True, stop=True), [sgb, gupb], [psG])
            P.op("dve", lambda e, ya=ya, yb=yb: e.tensor_tensor(out=ya[:], in0=ya[:], in1=yb[:], op=ALU.add), [ya, yb], [ya])
            P.op("dve", lambda e, ya=ya: e.tensor_reduce(out=st1[:], in_=v38(ya[:]), axis=AX.X, op=ALU.add), [ya], [st1])
            P.op("act", lambda e, ya=ya: e.activation(out=ysq[:], in_=ya[:], func=AF.Square), [ya], [ysq])
            P.op("dve", lambda e: e.tensor_reduce(out=st2[:], in_=v38(ysq[:]), axis=AX.X, op=ALU.add), [ysq], [st2])
            P.op("dve", lambda e: e.tensor_scalar(out=st1[:], in0=st1[:], scalar1=1.0 / HD, scalar2=None, op0=ALU.mult), [st1], [st1])
            P.op("dve", lambda e: e.tensor_tensor(out=st3[:], in0=st1[:], in1=st1[:], op=ALU.mult), [st1], [st3])
            P.op("dve", lambda e: e.scalar_tensor_tensor(out=st2[:], in0=st2[:], scalar=1.0 / HD, in1=st3[:], op0=ALU.mult, op1=ALU.subtract), [st2, st3], [st2])
            P.op("act", lambda e: e.activation(out=st2[:], in_=st2[:], func=AF.Sqrt, bias=epsg[:]), [st2, epsg], [st2])
            P.op("dve", lambda e: e.reciprocal(out=st2[:], in_=st2[:]), [st2], [st2])
            P.op("dve", lambda e, ya=ya: e.tensor_tensor(out=v38(ya[:]), in0=v38(ya[:]), in1=st1[:].unsqueeze(2).to_broadcast([128, NH, HD]), op=ALU.subtract), [ya, st1], [ya])
            P.op("dve", lambda e, ya=ya: e.tensor_tensor(out=v38(ya[:]), in0=v38(ya[:]), in1=st2[:].unsqueeze(2).to_broadcast([128, NH, HD]), op=ALU.mult), [ya, st2], [ya])
            P.op("pool", lambda e, ya=ya: e.tensor_tensor(out=ya[:], in0=ya[:], in1=gng[:], op=ALU.mult), [ya, gng], [ya])
            P.op("pool", lambda e, ya=ya: e.tensor_tensor(out=ya[:], in0=ya[:], in1=gnb[:], op=ALU.add), [ya, gnb], [ya])
            P.op("act", lambda e: e.activation(out=rks[:], in_=psR[:, 0:8], func=AF.Copy), [psR], [rks])
            P.op("dve", lambda e: e.tensor_tensor(out=v38(bon[:]), in0=v38(psV[:, 0:DR]), in1=rks[:].unsqueeze(2).to_broadcast([128, NH, HD]), op=ALU.mult), [psV, rks], [bon])
            P.op("pool", lambda e, ya=ya: e.tensor_tensor(out=ya[:], in0=ya[:], in1=bon[:], op=ALU.add), [ya, bon], [ya])
            P.op("dve", lambda e, ya=ya: e.tensor_tensor(out=ya[:], in0=ya[:], in1=psG[:, 0:DR], op=ALU.mult), [ya, psG], [ya])
            for kc in range(4):
                P.op("pe", lambda e, kc=kc, ya=ya: e.transpose(out=psAx[:, kc * 128:(kc + 1) * 128], in_=ya[:, kc * 128:(kc + 1) * 128], identity=identf), [ya, cst], [psAx])
            P.op("act", lambda e: e.activation(out=axT[:].rearrange("p k t -> p (k t)"), in_=psAx[:], func=AF.Copy), [psAx], [axT])
            for hf, po_ in ((0, psO0), (1, psO1)):
                for kc in range(8):
                    lhs = axT[:, kc, :] if kc < 4 else bxT[:, kc - 4, t0:t0 + 128]
                    rdl = [axT] if kc < 4 else [(bxT, kc - 4)]
                    P.op("pe", lambda e, lhs=lhs, kc=kc, hf=hf, po_=po_: e.matmul(out=po_[:], lhsT=lhs, rhs=woutb[:, kc, hf * 512:(hf + 1) * 512], start=(kc == 0), stop=(kc == 7)), rdl + [woutb], [po_])
                P.op("dve", lambda e, hf=hf, po_=po_: e.tensor_tensor(out=x1t[:, hf * 512:(hf + 1) * 512], in0=po_[:], in1=g1bc[:, hf * 512:(hf + 1) * 512], op=ALU.mult), [po_, g1bc], [(x1t, hf)])
            P.op("pool", lambda e, xr_=xr_: e.tensor_tensor(out=x1t[:], in0=x1t[:], in1=xr_[:], op=ALU.add), [x1t, xr_], [x1t])
            P.dma("sp", X1.a[t0:t0 + 128, :], x1t[:], reads=[x1t], writes=[(X1, tt)])
            P.op("act", lambda e: e.activation(out=junk2[:], in_=x1t[:], func=AF.Square, accum_out=ssq2[:]), [x1t], [junk2, ssq2])
            P.op("act", lambda e: e.activation(out=std2[:], in_=ssq2[:], func=AF.Sqrt, bias=epsn[:], scale=1.0 / D), [ssq2, epsn], [std2])
            P.op("dve", lambda e: e.reciprocal(out=rstd2[:], in_=std2[:]), [std2], [rstd2])
            P.op("dve", lambda e: e.tensor_scalar(out=xh2f[:], in0=x1t[:], scalar1=rstd2[:, 0:1], scalar2=None, op0=ALU.mult), [x1t, rstd2], [xh2f])
            P.op("pool", lambda e: e.tensor_copy(out=xh2b[:], in_=xh2f[:]), [xh2f], [xh2b])
            P.dma("pool", XH2.a[t0:t0 + 128, :], xh2b[:], reads=[xh2b], writes=[(XH2, tt)])
            for k in range(8):
                pz = psH if k < 4 else psAx
                P.op("pe", lambda e, k=k, pz=pz: e.transpose(out=pz[:, (k % 4) * 128:(k % 4 + 1) * 128], in_=xh2f[:, k * 128:(k + 1) * 128], identity=identf), [xh2f, cst], [pz])
            for k in range(8):
                pz = psH if k < 4 else psAx
                P.op("act" if k >= 4 else "dve", (lambda e, k=k, pz=pz: e.activation(out=hxT[:, k, :], in_=pz[:, (k % 4) * 128:(k % 4 + 1) * 128], func=AF.Identity, bias=modT[:, 24 + k, 0:1], scale=sc2[:, k:k + 1])) if k >= 4 else (lambda e, k=k, pz=pz: e.tensor_scalar(out=hxT[:, k, :], in0=pz[:, (k % 4) * 128:(k % 4 + 1) * 128], scalar1=sc2[:, k:k + 1], scalar2=modT[:, 24 + k, 0:1], op0=ALU.mult, op1=ALU.add)), [pz, modT, sc2], [(hxT, k)])
            for k in range(8):
                P.op("pe", lambda e, k=k: e.matmul(out=psL[:, 0:NE], lhsT=hxT[:, k, :], rhs=rwf[:, k, :], start=(k == 0), stop=(k == 7)), [(hxT, k), rwf], [psL])
            P.op("dve", lambda e: e.tensor_reduce(out=lmx[:], in_=psL[:, 0:NE], axis=AX.X, op=ALU.max, negate=True), [psL], [lmx])
            P.op("act", lambda e: e.activation(out=lex[:], in_=psL[:, 0:NE], func=AF.Exp, bias=lmx[:], accum_out=lsum[:]), [psL, lmx], [lex, lsum])
            P.op("dve", lambda e: e.reciprocal(out=lsum[:], in_=lsum[:]), [lsum], [lsum])
            P.op("dve", lambda e, tt=tt: e.tensor_scalar(out=affall[:, tt, :], in0=lex[:], scalar1=lsum[:, 0:1], scalar2=None, op0=ALU.mult), [lex, lsum], [(affall, tt)])
    if debug:
        P.dma("sp", dbg["aff"].a, affall[:].rearrange("p t e -> p (t e)"), reads=[affall], writes=[dbg["aff"]])


    if stop == 'D':
        return fin()
    P.pop()
    P.pop()
    P.push()
    NTE = NTX * NE
    idxi_all = P.sb([128, NE, NST], I32, "idxi_all")
    valt_all = P.sb([128, NE, NST], F32, "valt_all")
    P.push()
    tokhl = P.sb([128, NTX, 2], F32, "tokhl")
    iot = P.sb([128, 512], F32, "iot")
    P.dma("sp", iot[:], iota_d.a[:, 0:512], reads=[iota_d], writes=[iot])
    P.dma("sp", tokhl[:].rearrange("p t c -> p (t c)"), tokhl_d.a, reads=[tokhl_d], writes=[tokhl])
    lo = P.sb([128, NE], F32, "lo")
    mid = P.sb([128, NE], F32, "mid")
    ge = P.sb([128, NE], F32, "ge")
    cntp = P.sb([128, NE], F32, "cntp")
    cmpt = P.sb([128, NTX, NE], F32, "cmpt")
    maskb = P.sb([128, NTX, NE], BF16, "maskb")
    wsel = P.sb([128, NTX, NE], F32, "wsel")
    offs = P.sb([128, NTX, NE], F32, "offs")
    pos = P.sb([128, NTX, NE], F32, "pos")
    P.op("dve", lambda e: e.memset(lo[:], 0.0), [], [lo])
    for k in range(30):
        h = 2.0 ** -(k + 1)
        P.op("dve", lambda e, h=h: e.tensor_scalar(out=mid[:], in0=lo[:], scalar1=h, scalar2=None, op0=ALU.add), [lo], [mid])
        P.op("dve", lambda e: e.tensor_tensor(out=cmpt[:], in0=affall[:], in1=mid[:].unsqueeze(1).to_broadcast([128, NTX, NE]), op=ALU.is_ge), [affall, mid], [cmpt])
        P.op("dve", lambda e: e.tensor_reduce(out=cntp[:], in_=cmpt[:].rearrange("p t e -> p e t"), axis=AX.X, op=ALU.add), [cmpt], [cntp])
        P.op("pe", lambda e: e.matmul(out=psL[:, 0:NE], lhsT=cst[:, 1, :], rhs=cntp[:], start=True, stop=True), [cst, cntp], [psL])
        P.op("dve", lambda e, h=h: e.tensor_scalar(out=ge[:], in0=psL[:, 0:NE], scalar1=float(CAP) - 0.5, scalar2=h, op0=ALU.is_ge, op1=ALU.mult), [psL], [ge])
        P.op("dve", lambda e: e.tensor_tensor(out=lo[:], in0=lo[:], in1=ge[:], op=ALU.add), [lo, ge], [lo])
    P.op("dve", lambda e: e.tensor_tensor(out=cmpt[:], in0=affall[:], in1=lo[:].unsqueeze(1).to_broadcast([128, NTX, NE]), op=ALU.is_ge), [affall, lo], [cmpt])
    P.op("pool", lambda e: e.tensor_copy(out=maskb[:], in_=cmpt[:]), [cmpt], [maskb])
    P.op("dve", lambda e: e.tensor_tensor(out=wsel[:], in0=cmpt[:], in1=affall[:], op=ALU.mult), [cmpt, affall], [wsel])
    P.op("pe", lambda e: e.matmul(out=psV[:, 0:NTE], lhsT=cstb[:, 8, :], rhs=maskb[:].rearrange("p t e -> p (t e)"), start=True, stop=True), [cstb, maskb], [psV])
    P.op("pe", lambda e: e.matmul(out=psG[:, 0:NTE], lhsT=onesb, rhs=maskb[:].rearrange("p t e -> p (t e)"), start=True, stop=True), [cstb, maskb], [psG])
    P.op("dve", lambda e: e.memset(offs[:, 0, :], 0.0), [], [offs])
    for tt in range(1, NTX):
        P.op("dve", lambda e, tt=tt: e.tensor_tensor(out=offs[:, tt, :], in0=offs[:, tt - 1, :], in1=psG[:, (tt - 1) * NE:tt * NE], op=ALU.add), [offs, psG], [offs])
    P.op("dve", lambda e: e.tensor_tensor(out=pos[:].rearrange("p t e -> p (t e)"), in0=offs[:].rearrange("p t e -> p (t e)"), in1=psV[:, 0:NTE], op=ALU.add), [offs, psV], [pos])
    OH2 = [P.sb([128, NTX, CAP], BF16, f"OH{i}") for i in range(2)]
    rhsE2 = [P.sb([128, NTX, 4], BF16, f"rhsE{i}") for i in range(2)]
    for rhsE in rhsE2:
        P.op("pool", lambda e, rhsE=rhsE: e.tensor_copy(out=rhsE[:, :, 0:2], in_=tokhl[:]), [tokhl], [rhsE])
    idxs = P.sb([128, 4 * NST], F32, "idxs")
    idxf = P.sb([128, NST], F32, "idxf")
    for ex in range(NE):
        OH = OH2[ex % 2]
        rhsE = rhsE2[ex % 2]
        P.op("dve", lambda e, ex=ex, rhsE=rhsE: e.tensor_copy(out=rhsE[:, :, 2], in_=wsel[:, :, ex]), [wsel], [rhsE])
        P.op("dve", lambda e, ex=ex, rhsE=rhsE: e.tensor_tensor(out=rhsE[:, :, 3], in0=wsel[:, :, ex], in1=rhsE[:, :, 2], op=ALU.subtract), [wsel, rhsE], [rhsE])
        for tt in range(NTX):
            P.op("dve" if tt % 4 else "pool", lambda e, tt=tt, ex=ex, OH=OH: e.tensor_scalar(out=OH[:, tt, :], in0=iot[:, 0:CAP], scalar1=pos[:, tt, ex:ex + 1], scalar2=cmpt[:, tt, ex:ex + 1], op0=ALU.is_equal, op1=ALU.mult), [iot, pos, cmpt], [(OH, tt)])
        for j in range(NST):
            for tt in range(NTX):
                P.op("pe", lambda e, j=j, tt=tt, OH=OH, rhsE=rhsE: e.matmul(out=psR[0:ST, 4 * j:4 * j + 4], lhsT=OH[:, tt, j * ST:(j + 1) * ST], rhs=rhsE[:, tt, :], start=(tt == 0), stop=(tt == NTX - 1)), [(OH, tt), rhsE], [psR])
        P.op("act", lambda e: e.activation(out=idxs[0:ST, :], in_=psR[0:ST, 0:4 * NST], func=AF.Copy), [psR], [idxs])
        i3 = idxs[0:ST, :].rearrange("p (j c) -> p j c", c=4)
        P.op("dve", lambda e, i3=i3: e.scalar_tensor_tensor(out=idxf[0:ST, :], in0=i3[:, :, 0], scalar=128.0, in1=i3[:, :, 1], op0=ALU.mult, op1=ALU.add), [idxs], [idxf])
        P.op("dve", lambda e, ex=ex: e.tensor_copy(out=idxi_all[0:ST, ex, :], in_=idxf[0:ST, :]), [idxf], [(idxi_all, ex)])
        P.op("dve", lambda e, i3=i3, ex=ex: e.tensor_tensor(out=valt_all[0:ST, ex, :], in0=i3[:, :, 2], in1=i3[:, :, 3], op=ALU.add), [idxs], [(valt_all, ex)])
    if stop == 'E1':
        return fin()
    P.pop()
    P.push()
    Xg = [P.sb([128, NST, D], BF16, f"Xg{i}") for i in range(2)]
    XeT = P.sb([128, 8, CAP], BF16, "XeT")
    hidT = P.sb([128, 8, CAP], BF16, "hidT")
    sgt = P.sb([128, CAP], F32, "sgt")
    NSTG = 4
    wstg = [P.sb([128, 2, D], F32, f"wstg{i}") for i in range(NSTG)]
    Wg2 = [P.sb([128, 8, D], BF16, f"Wg{i}") for i in range(2)]
    Wu2 = [P.sb([128, 8, D], BF16, f"Wu{i}") for i in range(2)]
    Wd1 = P.sb([128, 8, D], BF16, "Wd")
    rows = [P.sb([128, D], F32, f"rows{i}") for i in range(3)]
    wsrc = [wg_d, wu_d, wd_d]
    stg_c = [0]
    sc_i = 0

    def load_w(ex, which=(0, 1, 2)):
        for wi, dst in ((0, Wg2[ex % 2]), (1, Wu2[ex % 2]), (2, Wd1)):
            if wi not in which:
                continue
            wv = wsrc[wi].a[ex].rearrange("(k p) c -> p k c", p=128)
            for hq in range(4):
                stg = wstg[stg_c[0] % NSTG]
                stg_c[0] += 1
                P.dma("sp", stg[:], wv[:, hq * 2:(hq + 1) * 2, :], reads=[wsrc[wi]], writes=[stg])
                P.op("pool", lambda e, stg=stg, dst=dst, hq=hq: e.tensor_copy(out=dst[:, hq * 2:(hq + 1) * 2, :], in_=stg[:]), [stg], [(dst, hq // 2)])

    def gather(ex):
        for j in range(NST):
            P.op("pool", lambda e, j=j, ex=ex: e.indirect_dma_start(out=Xg[ex % 2][0:ST, j, :], out_offset=None, in_=XH2.a, in_offset=IOA(ap=idxi_all[0:ST, ex, j:j + 1], axis=0)), [XH2, (idxi_all, ex)], [(Xg[ex % 2], j)], dma=True)

    gather(0)
    load_w(0)
    for ex in range(NE):
        xg = Xg[ex % 2]
        Wg_, Wu_ = Wg2[ex % 2], Wu2[ex % 2]
        if ex + 1 < NE:
            gather(ex + 1)
            load_w(ex + 1, (0, 1))
        for k in range(8):
            pz = (psV, psG)[k % 2]
            for j in range(NST):
                P.op("pe", lambda e, k=k, j=j, pz=pz, xg=xg: e.matmul(out=pz[:, j * ST:(j + 1) * ST], lhsT=xg[0:ST, j, k * 128:(k + 1) * 128], rhs=cstb[0:ST, 0, 0:ST], start=True, stop=True), [(xg, j), cstb], [pz])
            P.op("dve" if k % 2 else "act", (lambda e, k=k, pz=pz: e.tensor_scalar(out=XeT[:, k, :], in0=pz[:, 0:CAP], scalar1=sc2[:, k:k + 1], scalar2=modT[:, 24 + k, 0:1], op0=ALU.mult, op1=ALU.add)) if k % 2 else (lambda e, k=k, pz=pz: e.activation(out=XeT[:, k, :], in_=pz[:, 0:CAP], func=AF.Identity, bias=modT[:, 24 + k, 0:1], scale=sc2[:, k:k + 1])), [pz, sc2, modT], [(XeT, k)])
        for fc in range(8):
            for k in range(8):
                P.op("pe", lambda e, fc=fc, k=k, Wg_=Wg_: e.matmul(out=psAx[:, 0:CAP], lhsT=Wg_[:, k, fc * 128:(fc + 1) * 128], rhs=XeT[:, k, :], start=(k == 0), stop=(k == 7)), [(Wg_, k // 4), (XeT, k)], [psAx])
            for k in range(8):
                P.op("pe", lambda e, fc=fc, k=k, Wu_=Wu_: e.matmul(out=psH[:, 0:CAP], lhsT=Wu_[:, k, fc * 128:(fc + 1) * 128], rhs=XeT[:, k, :], start=(k == 0), stop=(k == 7)), [(Wu_, k // 4), (XeT, k)], [psH])
            P.op("act", lambda e: e.activation(out=sgt[:], in_=psAx[:, 0:CAP], func=AF.Silu), [psAx], [sgt])
            P.op("dve", lambda e, fc=fc: e.tensor_tensor(out=hidT[:, fc, :], in0=sgt[:], in1=psH[:, 0:CAP], op=ALU.mult), [sgt, psH], [(hidT, fc)])
        for j in range(NST):
            rw_ = rows[sc_i % 3]
            sc_i += 1
            for hf, po_ in ((0, psO0), (1, psO1)):
                for fc in range(8):
                    P.op("pe", lambda e, j=j, hf=hf, fc=fc, po_=po_: e.matmul(out=po_[0:ST, :], lhsT=hidT[:, fc, j * ST:(j + 1) * ST], rhs=Wd1[:, fc, hf * 512:(hf + 1) * 512], start=(fc == 0), stop=(fc == 7)), [(hidT, fc), (Wd1, fc // 4)], [po_])
                P.op("dve", lambda e, j=j, hf=hf, po_=po_, rw_=rw_, ex=ex: e.scalar_tensor_tensor(out=rw_[0:ST, hf * 512:(hf + 1) * 512], in0=po_[0:ST, :], scalar=valt_all[0:ST, ex, j:j + 1], in1=g2bc[0:ST, hf * 512:(hf + 1) * 512], op0=ALU.mult, op1=ALU.mult), [po_, (valt_all, ex), g2bc], [(rw_, hf)])
            P.op("pool", lambda e, j=j, ex=ex, rw_=rw_: e.indirect_dma_start(out=X1.a, out_offset=IOA(ap=idxi_all[0:ST, ex, j:j + 1], axis=0), in_=rw_[0:ST, :], in_offset=None, compute_op=ALU.add), [rw_, (idxi_all, ex)], [X1], dma=True)
        if ex + 1 < NE:
            load_w(ex + 1, (2,))
    P.pop()

    if stop == 'E':
        return fin()
    P.pop()
    P.push()
    fx = [P.sb([128, D], F32, f"fx{i}") for i in range(2)]
    fo = [P.sb([128, D], F32, f"fo{i}") for i in range(2)]
    fs = P.sb([128, 1], F32, "fs")
    fd = P.sb([128, 1], F32, "fd")
    fr = P.sb([128, 1], F32, "fr")
    for tt in range(NTX):
        t0 = tt * 128
        a, o_ = fx[tt % 2], fo[tt % 2]
        P.dma("sp", a[:], X1.a[t0:t0 + 128, :], reads=[X1], writes=[a])
        P.op("act", lambda e, a=a: e.activation(out=junk2[:], in_=a[:], func=AF.Square, accum_out=fs[:]), [a], [junk2, fs])
        P.op("act", lambda e: e.activation(out=fd[:], in_=fs[:], func=AF.Sqrt, bias=epsn[:], scale=1.0 / D), [fs, epsn], [fd])
        P.op("dve", lambda e: e.reciprocal(out=fr[:], in_=fd[:]), [fd], [fr])
        P.op("dve", lambda e, a=a, o_=o_: e.scalar_tensor_tensor(out=o_[:], in0=a[:], scalar=fr[:, 0:1], in1=fgbc[:], op0=ALU.mult, op1=ALU.mult), [a, fr, fgbc], [o_])
        P.dma("pool", out_d.a[t0:t0 + 128, :], o_[:], reads=[o_], writes=[(out_d, tt)])

    P.pop()
    stats = P.emit()
    return nc, stats


def host_consts():
    c = np.zeros((128, 9, 128), np.float32)
    i = np.arange(128)
    c[:, 0, :] = np.eye(128)
    c[:, 1, :] = 1.0
    c[:, 2, :] = (i[:, None] // 64 == i[None, :] // 64)
    a, b = i[:, None] % 64, i[None, :] % 64
    c[:, 3, :] = a > b
    c[:, 4, :] = a < b
    c[:, 5, :] = a <= b
    c[:, 6, :] = a >= b
    c[:, 7, :] = (i[:, None] // 64 == i[None, :] // 64)
    c[:, 8, :] = i[:, None] < i[None, :]
    return c


def col_perm():
    cols = []
    for Q in range(3):
        for q in range(4):
            cols += [Q * 512 + 4 * j + q for j in range(128)]
    for qa in (0, 2):
        cols += [1536 + 4 * j + qa for j in range(64)]
        cols += [1536 + 4 * j + qa + 1 for j in range(64)]
    for i in range(4):
        pass
    cols += list(range(1792, 3328))
    return np.array(cols)


def prep_core(inp, b):
    f = lambda a: np.ascontiguousarray(a, dtype=np.float32)
    perm = col_perm()
    m = {}
    m["x"] = f(inp["x"][b])
    m["ctx"] = f(inp["ctx"][b])
    cT = np.zeros((128, 16), np.float32)
    cT[:, 0:8] = inp["c"][b].reshape(8, 128).T
    cT[:, 8:16] = inp["c_ctx"].reshape(8, 128).T
    m["cT"] = cT
    m["ada_w"] = f(inp["ada_w"][0])
    m["adabT"] = f(inp["ada_b"][0].reshape(48, 128).T)
    gnT = np.zeros((128, 24), np.float32)
    gnT[:, 0:8] = inp["norm1_g"][0].reshape(8, 128).T
    gnT[:, 8:16] = inp["norm2_g"][0].reshape(8, 128).T
    m["gnT"] = gnT
    m["final_g"] = f(inp["final_g"].reshape(1, D))
    m["w_in_p"] = f(inp["w_in"][0][:, perm])
    mu = inp["shift_mu"][0]
    m["muT"] = f(mu[perm[:1792]].reshape(14, 128).T)
    lw = np.zeros((2, 128, DR), np.float32)
    lw[:, 0:64] = inp["w_lora_up"][0]
    lw[:, 64:128] = inp["a_lora_up"][0]
    m["loraW"] = lw
    wa0 = np.zeros((128, 2, 2, 4), np.float32)
    for d in range(2):
        wa0[:, d, 0, :] = inp["w0"][0, d].reshape(4, 128).T
        wa0[:, d, 1, :] = inp["a0"][0, d].reshape(4, 128).T
    m["wa0T"] = wa0
    kv = np.zeros((128, 3, 4), np.float32)
    kv[:, 0, :] = inp["k_k"][0].reshape(4, 128).T
    kv[:, 1, :] = inp["k_a"][0].reshape(4, 128).T
    kv[:, 2, :] = inp["r_k"][0].reshape(4, 128).T
    m["kvecT"] = kv
    m["g_up"] = f(inp["g_lora_up"][0])
    m["gnrow"] = f(np.stack([inp["gn_g"][0], inp["gn_b"][0]]))
    m["cwT"] = f(inp["conv_w"][0].reshape(3, 4, 128).transpose(2, 1, 0))
    m["w_out"] = f(inp["w_out"][0])
    m["router_w"] = f(inp["router_w"][0])
    m["exp_w_gate"] = f(inp["exp_w_gate"][0])
    m["exp_w_up"] = f(inp["exp_w_up"][0])
    m["exp_w_down"] = f(inp["exp_w_down"][0])
    m["consts"] = host_consts()
    io = np.zeros((128, 514), np.float32)
    io[:, 0:512] = np.arange(512)[None, :]
    m["iotas"] = io
    rm = np.ones((128, 512), np.float32)
    rm[:, ::64] = 0.0
    m["rmask"] = rm
    ntx = m["x"].shape[0] // 128
    th = np.zeros((128, ntx, 2), np.float32)
    th[:, :, 0] = np.arange(ntx)[None, :]
    th[:, :, 1] = np.arange(128)[:, None]
    m["tokhl"] = th.reshape(128, ntx * 2)
    return m


def kernel(**inputs):
    nc, _ = build()
    in_maps = [prep_core(inputs, b) for b in range(8)]
    res = run_bass_kernel_spmd(nc, in_maps, core_ids=list(range(8)))
    return np.stack([np.asarray(r["out"], dtype=np.float32) for r in res.results], axis=0)
```

```python
import numpy as np
import ml_dtypes
from contextlib import ExitStack
import concourse.bass as bass
import concourse.mybir as mybir
from concourse.bass_utils import run_bass_kernel_spmd

F32 = mybir.dt.float32
BF16 = mybir.dt.bfloat16
I32 = mybir.dt.int32
U32 = mybir.dt.uint32
AF = mybir.ActivationFunctionType
ALU = mybir.AluOpType
AX = mybir.AxisListType

COMPUTE = ("pe", "act", "dve", "pool")
QUEUES = ("sp", "act", "pool")
NDMA_SEM = 8


class T:
    def __init__(self, handle, name):
        self.h = handle
        self.name = name
        self.st = {}

    def __getitem__(self, k):
        return self.h[k]


class Op:
    __slots__ = ("eng", "fn", "deps", "is_dma", "sig", "signo", "dsem", "dval", "idx", "bar")

    def __init__(self, eng, fn, is_dma):
        self.eng = eng
        self.fn = fn
        self.deps = []
        self.is_dma = is_dma
        self.sig = False
        self.signo = None
        self.dsem = None
        self.dval = None
        self.bar = False


class Prog:
    def __init__(self, nc):
        self.nc = nc
        self.ops = []
        self.ntile = 0
        self.stacks = []
        self.defer = None

    def sb(self, shape, dtype, name="t"):
        self.ntile += 1
        if self.stacks:
            h = self.stacks[-1].enter_context(self.nc.sbuf_tensor(f"{name}_{self.ntile}", list(shape), dtype))
        else:
            h = self.nc.alloc_sbuf_tensor(f"{name}_{self.ntile}", list(shape), dtype)
        return T(h, name)

    def scope(self):
        prog = self

        class _Scope:
            def __enter__(s):
                prog.stacks.append(ExitStack())

            def __exit__(s, *a):
                prog.barrier()
                prog.stacks.pop().close()
                return False
        return _Scope()

    def push(self):
        self.stacks.append(ExitStack())

    def pop(self):
        self.barrier()
        self.stacks.pop().close()

    def barrier(self):
        o = Op(None, None, False)
        o.bar = True
        self.ops.append(o)

    def ps(self, shape, dtype=F32, name="p"):
        self.ntile += 1
        return T(self.nc.alloc_psum_tensor(f"{name}_{self.ntile}", list(shape), dtype), name)

    def dram(self, name, shape, dtype, kind="Internal"):
        h = self.nc.dram_tensor(name, list(shape), dtype, kind=kind)
        t = T(h, name)
        t.a = h.ap()
        return t

    @staticmethod
    def _norm(acc):
        return [(a, None) if isinstance(a, T) else a for a in acc]

    def replay(self, lst):
        for (eng, fn, reads, writes, dma) in lst:
            self.op(eng, fn, reads, writes, dma)

    def op(self, eng, fn, reads=(), writes=(), dma=False):
        if self.defer is not None:
            self.defer.append((eng, fn, reads, writes, dma))
            return None
        o = Op(eng, fn, dma)
        deps = set()

        def matching(t, key):
            if key is None:
                return list(t.st.values())
            res = []
            if key in t.st:
                res.append(t.st[key])
            if None in t.st:
                res.append(t.st[None])
            return res

        reads = self._norm(reads)
        writes = self._norm(writes)
        for (t, key) in reads:
            for ent in matching(t, key):
                if ent[0] is not None:
                    deps.add(ent[0])
        for (t, key) in writes:
            for ent in matching(t, key):
                if ent[0] is not None:
                    deps.add(ent[0])
                deps.update(ent[1].values())
                deps.update(ent[2])
        for (t, key) in reads:
            ent = t.st.setdefault(key, [None, {}, []])
            if dma:
                ent[2].append(o)
            else:
                ent[1][eng] = o
        for (t, key) in writes:
            if key is None:
                t.st = {None: [o, {}, []]}
            else:
                t.st[key] = [o, {}, []]
        deps.discard(o)
        for d in deps:
            if (not d.is_dma) and (not o.is_dma) and d.eng == o.eng and d.eng == "pe":
                continue
            o.deps.append(d)
            d.sig = True
        self.ops.append(o)
        return o

    def dma(self, q, out, in_, reads=(), writes=(), **kw):
        return self.op(q, lambda e: e.dma_start(out=out, in_=in_, **kw), reads, writes, dma=True)

    def emit(self):
        nc = self.nc
        sems = {e: nc.alloc_semaphore(name=f"sem_{e}") for e in COMPUTE}
        dsems = {q: [nc.alloc_semaphore(name=f"dsem_{q}_{i}") for i in range(NDMA_SEM)] for q in QUEUES}
        cnt = {e: 0 for e in COMPUTE}
        dcnt = {q: 0 for q in QUEUES}
        lastop = {}
        for o in self.ops:
            if o.bar:
                for lo in lastop.values():
                    lo.sig = True
            elif not o.is_dma:
                lastop[o.eng] = o
        dlast = {}
        for o in self.ops:
            if o.bar:
                o.deps = (dict(cnt), dict(dlast))
                continue
            if o.is_dma:
                i = dcnt[o.eng]
                dcnt[o.eng] += 1
                o.dsem = dsems[o.eng][i % NDMA_SEM]
                o.dval = 16 * (i // NDMA_SEM + 1)
                dlast[id(o.dsem)] = (o.dsem, o.dval)
            elif o.sig:
                cnt[o.eng] += 1
                o.signo = cnt[o.eng]
        per_eng = {e: [] for e in set(COMPUTE) | set(QUEUES)}
        for o in self.ops:
            if o.bar:
                for e in per_eng:
                    per_eng[e].append(o)
            else:
                per_eng[o.eng].append(o)

        def run_engine(ename, eng):
            waited = {}

            def wait(sem, val):
                k = id(sem)
                if waited.get(k, 0) >= val:
                    return
                waited[k] = val
                eng.wait_ge(sem, val)

            last_dma = {}
            for o in per_eng[ename]:
                if o.bar:
                    for (ce, v) in o.deps[0].items():
                        if v > 0:
                            wait(sems[ce], v)
                    for (s, v) in o.deps[1].values():
                        wait(s, v)
                    continue
                for d in o.deps:
                    if d.is_dma:
                        wait(d.dsem, d.dval)
                    else:
                        wait(sems[d.eng], d.signo)
                if o.is_dma:
                    if o.dval > 16:
                        wait(o.dsem, o.dval - 16)
                    ins = o.fn(eng)
                    ins.then_inc(o.dsem, 16)
                    last_dma[id(o.dsem)] = (o.dsem, o.dval)
                else:
                    ins = o.fn(eng)
                    if o.sig:
                        ins.then_inc(sems[ename], 1)
            for (s, v) in last_dma.values():
                wait(s, v)

        with nc.Block() as block:
            @block.tensor
            def _(e):
                run_engine("pe", e)

            @block.scalar
            def _(e):
                run_engine("act", e)

            @block.vector
            def _(e):
                run_engine("dve", e)

            @block.gpsimd
            def _(e):
                run_engine("pool", e)

            @block.sync
            def _(e):
                run_engine("sp", e)
        return {e: len([o for o in v if not o.bar]) for e, v in per_eng.items()}


D = 1024
DR = 512
NH = 8
HD = 64
NE = 16
C0 = float(np.exp(-0.5))
GN_EPS = 64e-5
NORM_EPS = 1e-6
CH = 64
import os as _os
YVAR = int(_os.environ.get('YVAR', '0'))


def build(TX=4096, CL=256, debug=False, stop=None):
    nc = bass.Bass("TRN2", target_bir_lowering=False)
    P = Prog(nc)
    TT = CL + TX
    NTX = TX // 128
    CAP = 2 * TX // NE
    ST = min(128, CAP)
    NST = CAP // ST
    GW = 64
    ROWS = TX // GW

    def fin():
        while P.stacks:
            P.pop()
        return nc, P.emit()

    def din(name, shape, dt=F32):
        return P.dram(name, shape, dt, kind="ExternalInput")

    x_d = din("x", [TX, D])
    ctx_d = din("ctx", [CL, D])
    cT_d = din("cT", [128, 16])
    adaw_d = din("ada_w", [D, 6 * D])
    adab_d = din("adabT", [128, 48])
    gn_d = din("gnT", [128, 24])
    finalg_d = din("final_g", [1, D])
    win_d = din("w_in_p", [D, 3328])
    mu_d = din("muT", [128, 14])
    lora_d = din("loraW", [2, 128, DR])
    wa0_d = din("wa0T", [128, 2, 2, 4])
    kvec_d = din("kvecT", [128, 3, 4])
    gup_d = din("g_up", [128, DR])
    gnrow_d = din("gnrow", [2, DR])
    cw_d = din("cwT", [128, 4, 3])
    wout_d = din("w_out", [D, D])
    rw_d = din("router_w", [D, NE])
    wg_d = din("exp_w_gate", [NE, D, D])
    wu_d = din("exp_w_up", [NE, D, D])
    wd_d = din("exp_w_down", [NE, D, D])
    cst_d = din("consts", [128, 9, 128])
    iota_d = din("iotas", [128, 512 + 2])
    rmask_d = din("rmask", [128, 512])
    tokhl_d = din("tokhl", [128, (TX // 128) * 2])
    out_d = P.dram("out", [TX, D], F32, kind="ExternalOutput")
    dbg = {}
    if debug:
        dbg["pxm"] = P.dram("dbg_pxm", [1792, TT], BF16, kind="ExternalOutput")
        dbg["bx"] = P.dram("dbg_bx", [512, TX], F32, kind="ExternalOutput")
        dbg["y"] = P.dram("dbg_y", [2, TX, DR], F32, kind="ExternalOutput")
        dbg["x1"] = P.dram("dbg_x1", [TX, D], F32, kind="ExternalOutput")
        dbg["aff"] = P.dram("dbg_aff", [128, NTX * NE], F32, kind="ExternalOutput")

    PXM = dbg["pxm"] if debug else P.dram("pxm", [1792, TT], BF16)
    YD = dbg["y"] if debug else P.dram("yd", [2, TX, DR], F32)
    X1 = dbg["x1"] if debug else P.dram("x1", [TX, D], F32)
    XH2 = P.dram("xh2", [TX, D], BF16)

    cst = P.sb([128, 9, 128], F32, "cst")
    identf = cst[:, 0, :]
    cstb = P.sb([128, 9, 128], BF16, "cstb")
    identb = cstb[:, 0, :]
    onesb = cstb[:, 1, :]
    blkones = cstb[:, 2, :]
    modT = P.sb([128, 48, 2], F32, "modT")
    gnT = P.sb([128, 24], F32, "gnT")
    sc1x = P.sb([128, 8], F32, "sc1x")
    sc1c = P.sb([128, 8], F32, "sc1c")
    sc2 = P.sb([128, 8], F32, "sc2")
    epsn = P.sb([128, 1], F32, "epsn")
    epsg = P.sb([128, 1], F32, "epsg")
    g1bc = P.sb([128, D], F32, "g1bc")
    g2bc = P.sb([128, D], F32, "g2bc")
    fgbc = P.sb([128, D], F32, "fgbc")
    affall = P.sb([128, NTX, NE], F32, "affall")
    junk2 = P.sb([128, D], F32, "junk2")
    P.push()
    bxT = P.sb([128, 4, TX], BF16, "bxT")

    q2 = ["sp", "pool"]
    P.dma("sp", cst[:], cst_d.a, reads=[cst_d], writes=[cst])
    P.op("dve", lambda e: e.tensor_copy(out=cstb[:], in_=cst[:]), [cst], [cstb])
    P.dma("sp", gnT[:], gn_d.a, reads=[gn_d], writes=[gnT])
    P.op("dve", lambda e: e.memset(epsn[:], NORM_EPS), [], [epsn])
    P.op("dve", lambda e: e.memset(epsg[:], GN_EPS), [], [epsg])
    P.dma("pool", fgbc[:], finalg_d.a.partition_broadcast(128), reads=[finalg_d], writes=[fgbc])

    P.push()
    cT = P.sb([128, 16], F32, "cT")
    scT = P.sb([128, 16], F32, "scT")
    adab = P.sb([128, 48], F32, "adab")
    P.dma("sp", cT[:], cT_d.a, reads=[cT_d], writes=[cT])
    P.dma("sp", adab[:], adab_d.a, reads=[adab_d], writes=[adab])
    P.op("act", lambda e: e.activation(out=scT[:], in_=cT[:], func=AF.Silu), [cT], [scT])
    adaw = [P.sb([128, 8, 512], F32, f"adaw{i}") for i in range(2)]
    psA = P.ps([128, 512], F32, "psA")
    adaw_v = adaw_d.a.rearrange("(k p) c -> p k c", p=128)
    for jb in range(12):
        aw = adaw[jb % 2]
        P.dma(q2[jb % 2], aw[:], adaw_v[:, :, jb * 512:(jb + 1) * 512], reads=[adaw_d], writes=[aw])
        for jj in range(4):
            j = jb * 4 + jj
            for k in range(8):
                P.op("pe", lambda e, aw=aw, jj=jj, j=j, k=k: e.matmul(out=psA[:, 2 * j:2 * j + 2], lhsT=aw[:, k, jj * 128:(jj + 1) * 128], rhs=scT[:, k::8], start=(k == 0), stop=(k == 7)), [aw, scT], [psA])
    P.op("dve", lambda e: e.tensor_tensor(out=modT[:], in0=psA[:, 0:96].rearrange("p (j m) -> p j m", m=2), in1=adab[:].unsqueeze(2).to_broadcast([128, 48, 2]), op=ALU.add), [psA, adab], [modT])
    for (dst, gofs, cofs, m) in ((sc1x, 0, 8, 0), (sc1c, 0, 8, 1), (sc2, 8, 32, 0)):
        P.op("dve", lambda e, dst=dst, gofs=gofs, cofs=cofs, m=m: e.scalar_tensor_tensor(out=dst[:], in0=modT[:, cofs:cofs + 8, m], scalar=1.0, in1=gnT[:, gofs:gofs + 8], op0=ALU.add, op1=ALU.mult), [modT, gnT], [dst])
    dg = P.sb([128, 8, 128], F32, "dg")
    for (dst, ofs) in ((g1bc, 16), (g2bc, 40)):
        for k in range(8):
            P.op("dve", lambda e, k=k, ofs=ofs: e.tensor_scalar(out=dg[:, k, :], in0=identf, scalar1=modT[:, ofs + k, 0:1], scalar2=None, op0=ALU.mult), [cst, modT], [dg])
        for hf in range(2):
            P.op("pe", lambda e, hf=hf: e.matmul(out=psA[:], lhsT=cst[:, 1, :], rhs=dg[:, hf * 4:(hf + 1) * 4, :].rearrange("p k c -> p (k c)"), start=True, stop=True), [cst, dg], [psA])
            P.op("act", lambda e, hf=hf, dst=dst: e.activation(out=dst[:, hf * 512:(hf + 1) * 512], in_=psA[:], func=AF.Copy), [psA], [dst])

    if stop == 'A':
        return fin()
    P.pop()
    P.push()
    h1T = P.sb([128, 8, TT], BF16, "h1T")
    P.push()
    xin = [P.sb([128, 4, D], F32, f"xin{i}") for i in range(2)]
    xhf = [P.sb([128, 4, D], F32, f"xhf{i}") for i in range(2)]
    junk = P.sb([128, D], F32, "junk")
    ssq = P.sb([128, 4], F32, "ssq")
    stdv = P.sb([128, 4], F32, "stdv")
    rstd = P.sb([128, 4], F32, "rstd")
    psT = [P.ps([128, 512], F32, f"psT{i}") for i in range(4)]
    groups = [(ctx_d, t0, min(512, CL - t0), t0) for t0 in range(0, CL, 512)] + [(x_d, t0, 512, CL + t0) for t0 in range(0, TX, 512)]
    for gi, (src, t0, n, dst0) in enumerate(groups):
        nt = n // 128
        xi = xin[gi % 2]
        xh = xhf[gi % 2]
        isctx = src is ctx_d
        P.dma(q2[gi % 2], xi[:, 0:nt, :], src.a[t0:t0 + n, :].rearrange("(t p) d -> p t d", p=128), reads=[src], writes=[xi])
        for t in range(nt):
            P.op("act", lambda e, t=t, xi=xi: e.activation(out=junk[:], in_=xi[:, t, :], func=AF.Square, accum_out=ssq[:, t:t + 1]), [xi], [junk, ssq])
        P.op("act", lambda e, nt=nt: e.activation(out=stdv[:, 0:nt], in_=ssq[:, 0:nt], func=AF.Sqrt, bias=epsn[:], scale=1.0 / D), [ssq, epsn], [stdv])
        P.op("dve", lambda e, nt=nt: e.reciprocal(out=rstd[:, 0:nt], in_=stdv[:, 0:nt]), [stdv], [rstd])
        for t in range(nt):
            P.op("dve" if t % 2 else "pool", lambda e, t=t, xi=xi, xh=xh: e.tensor_scalar(out=xh[:, t, :], in0=xi[:, t, :], scalar1=rstd[:, t:t + 1], scalar2=None, op0=ALU.mult), [xi, rstd], [(xh, t)])
        for k in range(8):
            pt = psT[k % 4]
            for t in range(nt):
                P.op("pe", lambda e, t=t, k=k, pt=pt, xh=xh: e.transpose(out=pt[:, t * 128:(t + 1) * 128], in_=xh[:, t, k * 128:(k + 1) * 128], identity=identf), [(xh, t), cst], [pt])
            scv = sc1c if isctx else sc1x
            m = 1 if isctx else 0
            if k % 2:
                P.op("act", lambda e, k=k, pt=pt, n=n, dst0=dst0, scv=scv, m=m: e.activation(out=h1T[:, k, dst0:dst0 + n], in_=pt[:, 0:n], func=AF.Identity, bias=modT[:, k, m:m + 1], scale=scv[:, k:k + 1]), [pt, modT, scv], [(h1T, gi)])
            else:
                P.op("dve", lambda e, k=k, pt=pt, n=n, dst0=dst0, scv=scv, m=m: e.tensor_scalar(out=h1T[:, k, dst0:dst0 + n], in0=pt[:, 0:n], scalar1=scv[:, k:k + 1], scalar2=modT[:, k, m:m + 1], op0=ALU.mult, op1=ALU.add), [pt, modT, scv], [(h1T, gi)])

    P.pop()
    P.push()
    muT = P.sb([128, 14], F32, "muT")
    omuT = P.sb([128, 14], F32, "omuT")
    cwT = P.sb([128, 4, 3], F32, "cwT")
    P.dma("sp", muT[:], mu_d.a, reads=[mu_d], writes=[muT])
    P.dma("sp", cwT[:], cw_d.a, reads=[cw_d], writes=[cwT])
    P.op("dve", lambda e: e.tensor_scalar(out=omuT[:], in0=muT[:], scalar1=-1.0, scalar2=1.0, op0=ALU.mult, op1=ALU.add), [muT], [omuT])
    wst = [P.sb([128, 8, 128], F32, f"wst{i}") for i in range(2)]
    wbf = [P.sb([128, 8, 128], BF16, f"wbf{i}") for i in range(2)]
    pxc = P.sb([128, TT], F32, "pxc")
    pxo = [P.sb([128, TT], BF16, f"pxo{i}") for i in range(2)]
    cvb = P.sb([128, TX], BF16, "cvb")
    cvc = P.sb([128, TX], F32, "cvc")
    cva = pxc
    win_v = win_d.a.rearrange("(k p) c -> p k c", p=128)
    tok_groups = [(dst0, n) for (_, _, n, dst0) in groups]
    pxm_v = PXM.a

    def project(cc, col0, toks, sink):
        w = wst[cc % 2]
        wb = wbf[cc % 2]
        P.dma(q2[cc % 2], w[:], win_v[:, :, col0:col0 + 128], reads=[win_d], writes=[w])
        P.op("pool", lambda e, w=w, wb=wb: e.tensor_copy(out=wb[:], in_=w[:]), [w], [wb])
        for gi, (tok0, n) in enumerate(toks):
            pt = psT[gi % 4]
            for k in range(8):
                P.op("pe", lambda e, k=k, pt=pt, wb=wb, tok0=tok0, n=n: e.matmul(out=pt[:, 0:n], lhsT=wb[:, k, :], rhs=h1T[:, k, tok0:tok0 + n], start=(k == 0), stop=(k == 7)), [wb, h1T], [pt])
            sink(gi, pt, tok0, n)

    for ci in range(14):
        def sink(gi, pt, tok0, n):
            P.op("act" if gi % 2 else "dve", (lambda e, pt=pt, tok0=tok0, n=n: e.activation(out=pxc[:, tok0:tok0 + n], in_=pt[:, 0:n], func=AF.Copy)) if gi % 2 else (lambda e, pt=pt, tok0=tok0, n=n: e.tensor_copy(out=pxc[:, tok0:tok0 + n], in_=pt[:, 0:n])), [pt], [(pxc, gi)])
        project(ci, ci * 128, tok_groups, sink)
        po = pxo[ci % 2]
        P.op("act", lambda e, ci=ci, po=po: e.activation(out=po[:], in_=pxc[:], func=AF.Copy, scale=omuT[:, ci:ci + 1]), [pxc, omuT], [po])
        if ci < 12:
            parts = [(0, 128, ci % 4)]
        else:
            parts = [(0, 64, 0 if ci == 12 else 2), (64, 128, 1 if ci == 12 else 3)]
        for (p0, p1, q) in parts:
            mu_s = muT[p0:p1, ci:ci + 1]
            Xv = pxc[p0:p1, CL:TT].rearrange("p (r c) -> p r c", c=GW)
            Ov = po[p0:p1, CL:TT].rearrange("p (r c) -> p r c", c=GW)
            if q == 0:
                oa, ia = Ov[:, :, 1:], Xv[:, :, :-1]
            elif q == 1:
                oa, ia = Ov[:, :, :-1], Xv[:, :, 1:]
            elif q == 2:
                oa, ia = po[p0:p1, CL + GW:TT], pxc[p0:p1, CL:TT - GW]
            else:
                oa, ia = po[p0:p1, CL:TT - GW], pxc[p0:p1, CL + GW:TT]
            P.op("dve", lambda e, oa=oa, ia=ia, mu_s=mu_s: e.scalar_tensor_tensor(out=oa, in0=ia, scalar=mu_s, in1=oa, op0=ALU.mult, op1=ALU.add), [pxc, po, muT], [po])
            if q % 2 == 0:
                oc, ic = po[p0:p1, 1:CL], pxc[p0:p1, 0:CL - 1]
            else:
                oc, ic = po[p0:p1, 0:CL - 1], pxc[p0:p1, 1:CL]
            P.op("dve", lambda e, oc=oc, ic=ic, mu_s=mu_s: e.scalar_tensor_tensor(out=oc, in0=ic, scalar=mu_s, in1=oc, op0=ALU.mult, op1=ALU.add), [pxc, po, muT], [po])
        if ci < 12:
            Q, q = ci // 4, ci % 4
            P.dma("sp", pxm_v[Q * 512 + q:(Q + 1) * 512:4, :], po[:], reads=[po], writes=[(PXM, ci)])
        else:
            qa = 0 if ci == 12 else 2
            P.dma("sp", pxm_v[1536 + qa:1792:4, :], po[0:64, :], reads=[po], writes=[(PXM, ci)])
            P.dma("sp", pxm_v[1536 + qa + 1:1792:4, :], po[64:128, :], reads=[po], writes=[(PXM, ci)])

    x_groups = [(tok0, n) for (tok0, n) in tok_groups if tok0 >= CL]
    for i in range(4):
        def sink_b(gi, pt, tok0, n):
            P.op("act", lambda e, pt=pt, tok0=tok0, n=n: e.activation(out=cvb[:, tok0 - CL:tok0 - CL + n], in_=pt[:, 0:n], func=AF.Copy), [pt], [(cvb, gi)])
        project(14 + i * 3, 1792 + i * 128, x_groups, sink_b)

        def sink_c(gi, pt, tok0, n):
            P.op("act", lambda e, pt=pt, tok0=tok0, n=n: e.activation(out=cvc[:, tok0 - CL:tok0 - CL + n], in_=pt[:, 0:n], func=AF.Copy), [pt], [(cvc, gi)])
        project(15 + i * 3, 1792 + 512 + i * 128, x_groups, sink_c)

        def sink_u(gi, pt, tok0, n):
            P.op("dve", lambda e, pt=pt, tok0=tok0, n=n: e.tensor_tensor(out=cvc[:, tok0 - CL:tok0 - CL + n], in0=cvc[:, tok0 - CL:tok0 - CL + n], in1=pt[:, 0:n], op=ALU.mult), [pt, (cvc, gi)], [(cvc, gi)])
        project(16 + i * 3, 1792 + 1024 + i * 128, x_groups, sink_u)
        P.op("act", lambda e, i=i: e.activation(out=cva[:, 0:TX], in_=cvc[:], func=AF.Copy, scale=cwT[:, i, 1:2]), [cvc, cwT], [cva])
        P.op("dve", lambda e, i=i: e.scalar_tensor_tensor(out=cva[:, 1:TX], in0=cvc[:, 0:TX - 1], scalar=cwT[:, i, 0:1], in1=cva[:, 1:TX], op0=ALU.mult, op1=ALU.add), [cvc, cva, cwT], [cva])
        P.op("dve", lambda e, i=i: e.scalar_tensor_tensor(out=cva[:, 0:TX - 1], in0=cvc[:, 1:TX], scalar=cwT[:, i, 2:3], in1=cva[:, 0:TX - 1], op0=ALU.mult, op1=ALU.add), [cvc, cva, cwT], [cva])
        P.op("pool", lambda e, i=i: e.tensor_tensor(out=bxT[:, i, :], in0=cva[:, 0:TX], in1=cvb[:], op=ALU.mult), [cva, cvb], [(bxT, i)])
        if debug:
            P.op("pool", lambda e: e.tensor_tensor(out=cva[:, 0:TX], in0=cva[:, 0:TX], in1=cvb[:], op=ALU.mult), [cva, cvb], [cva])
            P.dma("sp", dbg["bx"].a[i * 128:(i + 1) * 128, :], cva[:, 0:TX], reads=[cva], writes=[dbg["bx"]])


    if stop == 'B':
        return fin()
    P.pop()
    P.pop()
    NB = 512
    lorab = P.sb([128, 2, DR], BF16, "lorab")
    wa0 = P.sb([128, 2, 2, 4], F32, "wa0")
    kvec = P.sb([128, 3, 4], F32, "kvec")
    omka = P.sb([128, 4], F32, "omka")
    wab = P.sb([128, NB], BF16, "wab")
    P.push()
    psX = [P.ps([128, 512], F32, f"psX{i}") for i in range(3)]
    rmask = P.sb([128, 512], F32, "rmask")
    P.dma("sp", rmask[:], rmask_d.a, reads=[rmask_d], writes=[rmask])
    loraf = P.sb([128, 2, DR], F32, "loraf")
    P.dma("sp", loraf[:], lora_d.a.rearrange("d p c -> p d c"), reads=[lora_d], writes=[loraf])
    P.op("pool", lambda e: e.tensor_copy(out=lorab[:], in_=loraf[:]), [loraf], [lorab])
    P.dma("sp", wa0[:], wa0_d.a, reads=[wa0_d], writes=[wa0])
    P.dma("sp", kvec[:], kvec_d.a, reads=[kvec_d], writes=[kvec])
    P.op("dve", lambda e: e.tensor_scalar(out=omka[:], in0=kvec[:, 1, :], scalar1=-1.0, scalar2=1.0, op0=ALU.mult, op1=ALU.add), [kvec], [omka])
    mpair = P.sb([128, 2, 2, 128], F32, "mpair")
    P.op("dve", lambda e: e.tensor_copy(out=mpair[:, 0, 0, :], in_=cst[:, 4, :]), [cst], [mpair])
    P.op("dve", lambda e: e.tensor_copy(out=mpair[:, 0, 1, :], in_=cst[:, 5, :]), [cst], [mpair])
    P.op("dve", lambda e: e.tensor_copy(out=mpair[:, 1, 0, :], in_=cst[:, 3, :]), [cst], [mpair])
    P.op("dve", lambda e: e.tensor_copy(out=mpair[:, 1, 1, :], in_=cst[:, 6, :]), [cst], [mpair])
    NB = 512
    rkv = P.sb([128, 3, NB], BF16, "rkv")
    twb = P.sb([64, NB], BF16, "twb")
    s_t = P.sb([128, NB], F32, "s_t")
    ag_t = P.sb([128, NB], F32, "ag_t")
    Pc = P.sb([128, NB], F32, "Pc")
    cin = P.sb([128, NB], F32, "cin")
    cex = P.sb([128, NB], F32, "cex")
    tmc = P.sb([128, NB], F32, "tmc")
    E1 = P.sb([128, NB], F32, "E1")
    E2 = P.sb([128, NB], F32, "E2")
    E3 = P.sb([128, NB], F32, "E3")
    E4 = P.sb([128, NB], F32, "E4")
    GC2 = [P.sb([128, 8], F32, f"GC{i}") for i in range(4)]
    kk = cex
    sqb = P.sb([128, NB], BF16, "sqb")
    nrm = tmc
    kkn = P.sb([128, NB], F32, "kkn")
    b_t = P.sb([128, NB], F32, "b_t")
    kd_t = cin
    ARbd2 = [P.sb([128, 8, 2, 128], BF16, f"ARbd{i}") for i in range(4)]
    BKbd2 = [P.sb([128, 8, 2, 128], BF16, f"BKbd{i}") for i in range(2)]
    HVbd = P.sb([128, 8, 3, 128], BF16, "HVbd")
    TM2 = [P.sb([128, 8, 3, 128], BF16, f"TM{i}") for i in range(4)]
    for tz in (ARbd2[0], ARbd2[1], ARbd2[2], ARbd2[3], BKbd2[0], BKbd2[1], HVbd):
        P.op("pool", lambda e, tz=tz: e.memset(tz[:], 0.0), [], [tz])
    Sx2 = [[[P.sb([128, 3, 128], BF16, f"S{c}_{i}_{b}") for i in range(2)] for c in range(8)] for b in range(2)]
    Lts2 = [[P.sb([128, 3, 128], BF16, f"Lt{c}_{i}") for c in range(8)] for i in range(3)]
    TTs2 = [[P.sb([128, 128], BF16, f"TT{c}_{i}") for c in range(8)] for i in range(2)]
    Qb = P.sb([128, 128], BF16, "Qb")
    Ub = P.sb([128, 128], BF16, "Ub")
    Hf = P.sb([128, 128], F32, "Hf")
    Hb = P.sb([128, 128], BF16, "Hb")
    Yst = [P.sb([128, 8, 128], F32, f"Yst{i}") for i in range(2)]
    psZw, psZa, psSS, psTM = psT[0], psT[1], psT[2], psT[3]
    psLA, psLB, psN, psC = psA, psX[0], psX[1], psX[2]
    pxm3 = pxm_v[0:1536, :].rearrange("(q c) t -> c q t", q=3)
    ctx_blocks = [(0, CL)]
    x_blocks = [(CL + i * NB, NB) for i in range(TX // NB)]
    ev = ["dve", "act"]
    def do_block(g, d, tok0, n, kk_, first, Nl, N2l, N3l, Cl):
                P.defer = Nl
                par = kk_ % 2
                ARbd, TM, GC = ARbd2[kk_ % 4], TM2[kk_ % 4], GC2[kk_ % 4]
                Sx = Sx2[par]
                BKbd = BKbd2[par]
                Lts, TTs = Lts2[kk_ % 3], TTs2[par]
                Mx = cst[:, 3, :] if d == 0 else cst[:, 4, :]
                nch = n // CH
                isx = tok0 >= CL
                yst = Yst[par]
                P.dma("sp", rkv[:, :, 0:n], pxm3[g * 128:(g + 1) * 128, :, tok0:tok0 + n], reads=[PXM], writes=[rkv])
                P.dma("pool", wab[:, 0:n], pxm_v[1536:1664, tok0:tok0 + n], reads=[PXM], writes=[wab])
                P.op("act", lambda e, n=n: e.activation(out=twb[:, 0:n], in_=wab[0:64, 0:n], func=AF.Tanh), [wab], [twb])
                P.op("pe", lambda e, n=n, g=g, d=d: e.matmul(out=psZw[:, 0:n], lhsT=lorab[0:64, d, g * 128:(g + 1) * 128], rhs=twb[0:64, 0:n], start=True, stop=True), [lorab, twb], [psZw])
                P.op("act", lambda e, n=n, g=g, d=d: e.activation(out=s_t[:, 0:n], in_=psZw[:, 0:n], func=AF.Sigmoid, bias=wa0[:, d, 0, g:g + 1]), [psZw, wa0], [s_t])
                P.op("pe", lambda e, n=n, g=g, d=d: e.matmul(out=psZw[:, 0:n], lhsT=lorab[64:128, d, g * 128:(g + 1) * 128], rhs=wab[64:128, 0:n], start=True, stop=True), [lorab, wab], [psZw])
                P.op("act", lambda e, n=n, g=g, d=d: e.activation(out=ag_t[:, 0:n], in_=psZw[:, 0:n], func=AF.Sigmoid, bias=wa0[:, d, 1, g:g + 1]), [psZw, wa0], [ag_t])
                P.op("dve", lambda e, n=n: e.tensor_tensor_scan(out=Pc[:, 0:n], data0=rmask[:, 0:n], data1=s_t[:, 0:n], initial=0.0, op0=ALU.mult, op1=ALU.add), [rmask, s_t], [Pc])
                v3 = lambda t_, n=n: t_[:, 0:n].rearrange("p (c t) -> p c t", t=CH)
                totb = v3(Pc)[:, :, CH - 1:CH].to_broadcast([128, nch, CH])
                if d == 0:
                    P.op("pool", lambda e, n=n: e.tensor_tensor(out=cex[:, 0:n], in0=Pc[:, 0:n], in1=s_t[:, 0:n], op=ALU.subtract), [Pc, s_t], [cex])
                    cin_ = Pc
                else:
                    P.op("dve", lambda e, totb=totb, v3=v3: e.tensor_tensor(out=v3(cex), in0=totb, in1=v3(Pc), op=ALU.subtract), [Pc], [cex])
                    P.op("pool", lambda e, n=n: e.tensor_tensor(out=cin[:, 0:n], in0=cex[:, 0:n], in1=s_t[:, 0:n], op=ALU.add), [cex, s_t], [cin])
                    cin_ = cin
                P.op("dve", lambda e, totb=totb, v3=v3, cin_=cin_: e.tensor_tensor(out=v3(tmc), in0=totb, in1=v3(cin_), op=ALU.subtract), [Pc, cin_], [tmc])
                P.op("act", lambda e, n=n, cin_=cin_: e.activation(out=E1[:, 0:n], in_=cin_[:, 0:n], func=AF.Exp, scale=-C0), [cin_], [E1])
                P.op("act", lambda e, n=n: e.activation(out=E2[:, 0:n], in_=cex[:, 0:n], func=AF.Exp, scale=-C0), [cex], [E2])
                P.op("act", lambda e, n=n, cin_=cin_: e.activation(out=E3[:, 0:n], in_=cin_[:, 0:n], func=AF.Exp, scale=C0), [cin_], [E3])
                P.op("act", lambda e, n=n: e.activation(out=E4[:, 0:n], in_=tmc[:, 0:n], func=AF.Exp, scale=-C0), [tmc], [E4])
                P.op("act", lambda e, nch=nch, v3=v3: e.activation(out=GC[:, 0:nch], in_=v3(Pc)[:, :, CH - 1], func=AF.Exp, scale=-C0), [Pc], [GC])
                P.op("dve", lambda e, n=n, g=g: e.tensor_scalar(out=kk[:, 0:n], in0=rkv[:, 1, 0:n], scalar1=kvec[:, 0, g:g + 1], scalar2=None, op0=ALU.mult), [rkv, kvec], [kk])
                P.op("pool", lambda e, n=n: e.tensor_tensor(out=sqb[:, 0:n], in0=kk[:, 0:n], in1=kk[:, 0:n], op=ALU.mult), [kk], [sqb])
                P.op("pe", lambda e, n=n: e.matmul(out=psZw[:, 0:n], lhsT=blkones, rhs=sqb[:, 0:n], start=True, stop=True), [cstb, sqb], [psZw])
                P.op("act", lambda e, n=n: e.activation(out=nrm[:, 0:n], in_=psZw[:, 0:n], func=AF.Sqrt), [psZw], [nrm])
                P.op("dve", lambda e, n=n: e.tensor_scalar(out=nrm[:, 0:n], in0=nrm[:, 0:n], scalar1=1e-12, scalar2=None, op0=ALU.max), [nrm], [nrm])
                P.op("dve", lambda e, n=n: e.reciprocal(out=nrm[:, 0:n], in_=nrm[:, 0:n]), [nrm], [nrm])
                P.op("dve", lambda e, n=n: e.tensor_tensor(out=kkn[:, 0:n], in0=kk[:, 0:n], in1=nrm[:, 0:n], op=ALU.mult), [kk, nrm], [kkn])
                P.op("pool", lambda e, n=n: e.tensor_tensor(out=b_t[:, 0:n], in0=kkn[:, 0:n], in1=ag_t[:, 0:n], op=ALU.mult), [kkn, ag_t], [b_t])
                P.op("dve", lambda e, n=n, g=g: e.tensor_scalar(out=kd_t[:, 0:n], in0=ag_t[:, 0:n], scalar1=kvec[:, 1, g:g + 1], scalar2=omka[:, g:g + 1], op0=ALU.mult, op1=ALU.add), [ag_t, kvec, omka], [kd_t])
                P.op("dve", lambda e, n=n: e.tensor_tensor(out=kd_t[:, 0:n], in0=kd_t[:, 0:n], in1=rkv[:, 1, 0:n], op=ALU.mult), [kd_t, rkv], [kd_t])
                oi = 0
                for hh in range(2):
                    ps_ = slice(hh * 64, hh * 64 + 64)
                    v3h = lambda t_, n=n, ps_=ps_: t_[ps_, 0:n].rearrange("p (c t) -> p c t", t=CH)
                    r3 = rkv[ps_, 0, 0:n].rearrange("p (c t) -> p c t", t=CH)
                    vv3 = rkv[ps_, 2, 0:n].rearrange("p (c t) -> p c t", t=CH)
                    specs = [
                        (ARbd, 0, None, kkn, E2, -1.0), (ARbd, 1, r3, None, E1, None),
                        (BKbd, 0, None, b_t, E3, None), (BKbd, 1, None, kd_t, E3, None),
                        (HVbd, 1, None, b_t, E4, None), (HVbd, 2, None, kd_t, E4, None),
                    ]
                    for (dst, slot, a_ap, a_t, e_t, neg) in specs:
                        o_ap = dst[ps_, 0:nch, slot, hh * 64:hh * 64 + 64]
                        in0 = a_ap if a_ap is not None else v3h(a_t)
                        rd = [e_t] + ([rkv] if a_t is None else [a_t])
                        eng = ("dve", "pool")[oi % 2]
                        oi += 1
                        if neg is not None:
                            P.op("dve", lambda e, o_ap=o_ap, in0=in0, e_t=e_t, v3h=v3h: e.scalar_tensor_tensor(out=o_ap, in0=in0, scalar=-1.0, in1=v3h(e_t), op0=ALU.mult, op1=ALU.mult), rd, [(dst, slot)])
                        else:
                            P.op(eng, lambda e, o_ap=o_ap, in0=in0, e_t=e_t, v3h=v3h: e.tensor_tensor(out=o_ap, in0=in0, in1=v3h(e_t), op=ALU.mult), rd, [(dst, slot)])
                    P.op("pool", lambda e, hh=hh, ps_=ps_, vv3=vv3, nch=nch: e.tensor_copy(out=HVbd[ps_, 0:nch, 0, hh * 64:hh * 64 + 64], in_=vv3), [rkv], [(HVbd, 0)])
                for c in range(nch):
                    for j in range(3):
                        P.op("pe", lambda e, c=c, j=j: e.matmul(out=psTM[:, j * 128:(j + 1) * 128], lhsT=HVbd[:, c, j, :], rhs=identb, start=True, stop=True), [HVbd, cstb], [psTM])
                    P.op(ev[c % 2], (lambda e, c=c: e.activation(out=TM[:, c, :, :].rearrange("p j t -> p (j t)"), in_=psTM[:, 0:384], func=AF.Copy)) if ev[c % 2] == "act" else (lambda e, c=c: e.tensor_copy(out=TM[:, c, :, :].rearrange("p j t -> p (j t)"), in_=psTM[:, 0:384])), [psTM], [(TM, c)])
                P.defer = N2l
                for c in range(nch):
                    AT = ARbd[:, c, 0, :]
                    AR2 = ARbd[:, c, :, :].rearrange("p j t -> p (j t)")
                    bA, bB = psLA, psLB
                    S0 = Sx[c][0]
                    Lc = Lts[c]
                    P.op("pe", lambda e, AT=AT, c=c, bA=bA: e.matmul(out=bA[:, 0:128], lhsT=AT, rhs=BKbd[:, c, 0, :], start=True, stop=True), [ARbd, BKbd], [bA])
                    P.op("pe", lambda e, AR2=AR2, c=c, bA=bA: e.matmul(out=bA[:, 128:384], lhsT=BKbd[:, c, 0, :], rhs=AR2, start=True, stop=True), [ARbd, BKbd], [bA])
                    P.op("pe", lambda e, AR2=AR2, c=c, bB=bB: e.matmul(out=bB[:, 0:256], lhsT=BKbd[:, c, 1, :], rhs=AR2, start=True, stop=True), [ARbd, BKbd], [bB])
                    P.op("dve", lambda e, Mx=Mx, S0=S0, bA=bA: e.tensor_tensor(out=S0[:, 2, :], in0=bA[:, 0:128], in1=Mx, op=ALU.mult), [bA, cst], [S0])
                    P.op("dve", lambda e, d=d, S0=S0, bA=bA: e.tensor_tensor(out=S0[:, 0, :], in0=bA[:, 128:256], in1=mpair[:, d, 0, :], op=ALU.mult), [bA, mpair], [S0])
                    P.op("pool", lambda e, S0=S0: e.tensor_copy(out=S0[:, 1, :], in_=identb), [cstb], [S0])
                    P.op("dve", lambda e, d=d, Lc=Lc, bA=bA: e.tensor_tensor(out=Lc[:, 0, :], in0=bA[:, 256:384], in1=mpair[:, d, 1, :], op=ALU.mult), [bA, mpair], [Lc])
                    P.op("dve", lambda e, d=d, Lc=Lc, bB=bB: e.tensor_tensor(out=Lc[:, 1:3, :].rearrange("p j t -> p (j t)"), in0=bB[:, 0:256], in1=mpair[:, d, :, :].rearrange("p j t -> p (j t)"), op=ALU.mult), [bB, mpair], [Lc])
                P.defer = N3l
                NBK = (psN, psZa, psSS)
                for lv in range(6):
                    for c in range(nch):
                        cur, nxt = Sx[c][lv % 2], Sx[c][(lv + 1) % 2]
                        pn = NBK[c % 3]
                        engn = ("act", "act", "dve")[c % 3]
                        if lv < 5:
                            P.op("pe", lambda e, cur=cur, pn=pn: e.matmul(out=pn[:, 0:128], lhsT=cur[:, 2, :], rhs=cur[:, 0, :], start=True, stop=True), [cur], [pn])
                            P.op("pe", lambda e, cur=cur, pn=pn: e.matmul(out=pn[:, 256:384], lhsT=cur[:, 0, :], rhs=cur[:, 2, :], start=True, stop=True), [cur], [pn])
                        P.op("pe", lambda e, cur=cur, pn=pn: e.matmul(out=pn[:, 128:256], lhsT=cur[:, 2, :], rhs=cur[:, 1, :], start=True, stop=False), [cur], [pn])
                        P.op("pe", lambda e, cur=cur, pn=pn: e.matmul(out=pn[:, 128:256], lhsT=identb, rhs=cur[:, 1, :], start=False, stop=True), [cur, cstb], [pn])
                        if lv < 5:
                            P.op(engn, (lambda e, nxt=nxt, pn=pn: e.activation(out=nxt[:].rearrange("p j t -> p (j t)"), in_=pn[:, 0:384], func=AF.Copy)) if engn == "act" else (lambda e, nxt=nxt, pn=pn: e.tensor_copy(out=nxt[:].rearrange("p j t -> p (j t)"), in_=pn[:, 0:384])), [pn], [nxt])
                        else:
                            TTc = TTs[c]
                            P.op(engn, (lambda e, TTc=TTc, pn=pn: e.activation(out=TTc[:], in_=pn[:, 128:256], func=AF.Copy)) if engn == "act" else (lambda e, TTc=TTc, pn=pn: e.tensor_copy(out=TTc[:], in_=pn[:, 128:256])), [pn], [TTc])
                P.defer = Cl
                if first:
                    P.op("dve", lambda e: e.memset(Hf[:], 0.0), [], [Hf])
                    P.op("dve", lambda e: e.memset(Hb[:], 0.0), [], [Hb])
                corder = range(nch) if d == 0 else range(nch - 1, -1, -1)
                for c in corder:
                    AT = ARbd[:, c, 0, :]
                    RT = ARbd[:, c, 1, :]
                    Lc = Lts[c]
                    TTc = TTs[c]
                    Vb = TM[:, c, 0, :]
                    P.op("pe", lambda e, AT=AT: e.matmul(out=psC[:, 0:128], lhsT=AT, rhs=Hb[:], start=True, stop=False), [ARbd, Hb], [psC])
                    P.op("pe", lambda e, Vb=Vb, Lc=Lc: e.matmul(out=psC[:, 0:128], lhsT=Lc[:, 1, :], rhs=Vb, start=False, stop=True), [Lc, (TM, c)], [psC])
                    P.op("dve", lambda e: e.tensor_copy(out=Qb[:], in_=psC[:, 0:128]), [psC], [Qb])
                    P.op("pe", lambda e, TTc=TTc: e.matmul(out=psC[:, 128:256], lhsT=TTc[:], rhs=Qb[:], start=True, stop=True), [TTc, Qb], [psC])
                    P.op("dve", lambda e: e.tensor_copy(out=Ub[:], in_=psC[:, 128:256]), [psC], [Ub])
                    if isx:
                        P.op("pe", lambda e, RT=RT: e.matmul(out=psC[:, 256:384], lhsT=RT, rhs=Hb[:], start=True, stop=False), [ARbd, Hb], [psC])
                        P.op("pe", lambda e, Lc=Lc: e.matmul(out=psC[:, 256:384], lhsT=Lc[:, 0, :], rhs=Ub[:], start=False, stop=False), [Lc, Ub], [psC])
                        P.op("pe", lambda e, Vb=Vb, Lc=Lc: e.matmul(out=psC[:, 256:384], lhsT=Lc[:, 2, :], rhs=Vb, start=False, stop=True), [Lc, (TM, c)], [psC])
                    P.op("pe", lambda e, c=c: e.matmul(out=psC[:, 384:512], lhsT=TM[:, c, 1, :], rhs=Ub[:], start=True, stop=False), [(TM, c), Ub], [psC])
                    P.op("pe", lambda e, c=c, Vb=Vb: e.matmul(out=psC[:, 384:512], lhsT=TM[:, c, 2, :], rhs=Vb, start=False, stop=True), [(TM, c)], [psC])
                    P.op("dve", lambda e, c=c: e.scalar_tensor_tensor(out=Hb[:], in0=Hf[:], scalar=GC[:, c:c + 1], in1=psC[:, 384:512], op0=ALU.mult, op1=ALU.add), [Hf, GC, psC], [Hb])
                    P.op("dve", lambda e, c=c: e.scalar_tensor_tensor(out=Hf[:], in0=Hf[:], scalar=GC[:, c:c + 1], in1=psC[:, 384:512], op0=ALU.mult, op1=ALU.add), [Hf, GC, psC], [Hf])
                    if isx:
                        P.op("dve", lambda e, c=c, yst=yst: e.tensor_copy(out=yst[:, c, :], in_=psC[:, 256:384]), [psC], [yst])
                if isx:
                    xt0 = tok0 - CL
                    for hh in range(2):
                        P.dma("sp" if hh else "pool", YD.a[d, xt0:xt0 + n, g * 128 + hh * 64:g * 128 + hh * 64 + 64].rearrange("(c t) v -> t c v", t=CH), yst[hh * 64:hh * 64 + 64, 0:nch, hh * 64:hh * 64 + 64], reads=[yst], writes=[(YD, (d, g, hh, xt0))])


    items = []
    for g in range(4):
        for d in range(2):
            chain = (ctx_blocks + x_blocks) if d == 0 else (ctx_blocks + x_blocks[::-1])
            for bi_, (tok0, n) in enumerate(chain):
                items.append((g, d, tok0, n, bi_ == 0))
    N1s, N2s, N3s, Cs = [], [], [], []
    for k_, (g, d, tok0, n, first) in enumerate(items):
        Nl, N2l, N3l, Cl = [], [], [], []
        do_block(g, d, tok0, n, k_, first, Nl, N2l, N3l, Cl)
        N1s.append(Nl)
        N2s.append(N2l)
        N3s.append(N3l)
        Cs.append(Cl)
    P.defer = None

    def merge(*lists):
        lists = [l for l in lists if l]
        idx = [0] * len(lists)
        while True:
            best, bi_ = None, -1
            for i_, l in enumerate(lists):
                if idx[i_] < len(l):
                    frac = idx[i_] / len(l)
                    if best is None or frac < best:
                        best, bi_ = frac, i_
            if bi_ < 0:
                break
            P.replay([lists[bi_][idx[bi_]]])
            idx[bi_] += 1

    nit = len(items)
    gl = lambda L, i_: L[i_] if 0 <= i_ < nit else []
    for k_ in range(-3, nit):
        merge(gl(Cs, k_), gl(N3s, k_ + 1), gl(N2s, k_ + 2), gl(N1s, k_ + 3))

    if stop == 'C':
        return fin()
    P.pop()
    P.push()
    IOA = bass.IndirectOffsetOnAxis
    gupf = P.sb([128, DR], F32, "gupf")
    gupb = P.sb([128, DR], BF16, "gupb")
    P.dma("sp", gupf[:], gup_d.a, reads=[gup_d], writes=[gupf])
    P.op("pool", lambda e: e.tensor_copy(out=gupb[:], in_=gupf[:]), [gupf], [gupb])
    gng = P.sb([128, DR], F32, "gng")
    gnb = P.sb([128, DR], F32, "gnb")
    P.dma("pool", gng[:], gnrow_d.a[0:1, :].partition_broadcast(128), reads=[gnrow_d], writes=[gng])
    P.dma("pool", gnb[:], gnrow_d.a[1:2, :].partition_broadcast(128), reads=[gnrow_d], writes=[gnb])
    woutb = P.sb([128, 8, D], BF16, "woutb")
    wstage = P.sb([128, 4, D], F32, "wstage")
    wout_v = wout_d.a.rearrange("(k p) c -> p k c", p=128)
    for hf in range(2):
        P.dma("sp", wstage[:], wout_v[:, hf * 4:(hf + 1) * 4, :], reads=[wout_d], writes=[wstage])
        P.op("pool", lambda e, hf=hf: e.tensor_copy(out=woutb[:, hf * 4:(hf + 1) * 4, :], in_=wstage[:]), [wstage], [woutb])
    rwf = P.sb([128, 8, NE], F32, "rwf")
    P.dma("sp", rwf[:], rw_d.a.rearrange("(k p) c -> p k c", p=128), reads=[rw_d], writes=[rwf])
    omka2 = P.sb([128, 4], F32, "omka2")
    P.op("dve", lambda e: e.tensor_scalar(out=omka2[:], in0=kvec[:, 1, :], scalar1=-2.0, scalar2=2.0, op0=ALU.mult, op1=ALU.add), [kvec], [omka2])
    rkv4 = P.sb([128, 4, 3, NB], BF16, "rkv4")
    xgb = P.sb([128, NB], BF16, "xgb")
    sgb = P.sb([128, NB], BF16, "sgb")
    agf = P.sb([128, NB], F32, "agf")
    agb = P.sb([128, NB], F32, "agb")
    ksum = P.sb([128, NB], F32, "ksum")
    rkT = P.sb([128, 4, NB], BF16, "rkT")
    y0 = [P.sb([128, DR], F32, f"y0{i}") for i in range(2)]
    y1 = [P.sb([128, DR], F32, f"y1{i}") for i in range(2)]
    ysq = P.sb([128, DR], F32, "ysq")
    st1 = P.sb([128, 8], F32, "st1")
    st2 = P.sb([128, 8], F32, "st2")
    st3 = P.sb([128, 8], F32, "st3")
    rks = P.sb([128, 8], F32, "rks")
    bon = P.sb([128, DR], F32, "bon")
    axT = P.sb([128, 4, 128], BF16, "axT")
    xres = [P.sb([128, D], F32, f"xres{i}") for i in range(2)]
    x1t = P.sb([128, D], F32, "x1t")
    xh2f = P.sb([128, D], F32, "xh2f")
    xh2b = P.sb([128, D], BF16, "xh2b")
    hxT = P.sb([128, 8, 128], F32, "hxT")
    ssq2 = P.sb([128, 1], F32, "ssq2")
    std2 = P.sb([128, 1], F32, "std2")
    rstd2 = P.sb([128, 1], F32, "rstd2")
    lmx = P.sb([128, 1], F32, "lmx")
    lex = P.sb([128, NE], F32, "lex")
    lsum = P.sb([128, 1], F32, "lsum")
    headsel = cstb[:, 2, 0:128:64]
    psV, psG, psR, psAx, psO0, psO1, psH, psL = psT[0], psT[1], psT[2], psT[3], psA, psX[0], psX[1], psX[2]
    v38 = lambda ap: ap.rearrange("p (h v) -> p h v", v=HD)
    for bi in range(TX // NB):
        tokp = CL + bi * NB
        for q_ in range(3):
            P.dma(("sp", "pool", "sp")[q_], rkv4[:, :, q_, :], pxm_v[q_ * 512:(q_ + 1) * 512, tokp:tokp + NB].rearrange("(g c) t -> c g t", g=4), reads=[PXM], writes=[rkv4])
        P.dma("pool", wab[:, 0:NB], pxm_v[1536:1664, tokp:tokp + NB], reads=[PXM], writes=[wab])
        P.dma("pool", xgb[:], pxm_v[1664:1792, tokp:tokp + NB], reads=[PXM], writes=[xgb])
        P.op("act", lambda e: e.activation(out=sgb[:], in_=xgb[:], func=AF.Sigmoid), [xgb], [sgb])
        for g in range(4):
            for d, agt, pz in ((0, agf, psV), (1, agb, psG)):
                P.op("pe", lambda e, g=g, d=d, pz=pz: e.matmul(out=pz[:, 0:NB], lhsT=lorab[64:128, d, g * 128:(g + 1) * 128], rhs=wab[64:128, 0:NB], start=True, stop=True), [lorab, wab], [pz])
                P.op("act", lambda e, g=g, d=d, pz=pz, agt=agt: e.activation(out=agt[:], in_=pz[:, 0:NB], func=AF.Sigmoid, bias=wa0[:, d, 1, g:g + 1]), [pz, wa0], [agt])
            P.op("dve", lambda e: e.tensor_tensor(out=ksum[:], in0=agf[:], in1=agb[:], op=ALU.add), [agf, agb], [ksum])
            P.op("dve", lambda e, g=g: e.tensor_scalar(out=ksum[:], in0=ksum[:], scalar1=kvec[:, 1, g:g + 1], scalar2=omka2[:, g:g + 1], op0=ALU.mult, op1=ALU.add), [ksum, kvec, omka2], [ksum])
            P.op("pool", lambda e, g=g: e.tensor_tensor(out=ksum[:], in0=ksum[:], in1=rkv4[:, g, 1, :], op=ALU.mult), [ksum, rkv4], [ksum])
            P.op("dve", lambda e, g=g: e.scalar_tensor_tensor(out=rkT[:, g, :], in0=ksum[:], scalar=kvec[:, 2, g:g + 1], in1=rkv4[:, g, 0, :], op0=ALU.mult, op1=ALU.mult), [ksum, kvec, rkv4], [(rkT, g)])
        for ti in range(NB // 128):
            tt = bi * (NB // 128) + ti
            t0 = tt * 128
            ts = slice(ti * 128, (ti + 1) * 128)
            ya, yb, xr_ = y0[tt % 2], y1[tt % 2], xres[tt % 2]
            P.dma("sp", ya[:], YD.a[0, t0:t0 + 128, :], reads=[YD], writes=[ya])
            P.dma("pool", yb[:], YD.a[1, t0:t0 + 128, :], reads=[YD], writes=[yb])
            P.dma("sp", xr_[:], x_d.a[t0:t0 + 128, :], reads=[x_d], writes=[xr_])
            for g in range(4):
                P.op("pe", lambda e, g=g, ts=ts: e.matmul(out=psV[:, g * 128:(g + 1) * 128], lhsT=rkv4[:, g, 2, ts], rhs=identb, start=True, stop=True), [rkv4, cstb], [psV])
                P.op("pe", lambda e, g=g, ts=ts: e.matmul(out=psR[:, 2 * g:2 * g + 2], lhsT=rkT[:, g, ts], rhs=headsel, start=True, stop=True), [(rkT, g), cstb], [psR])
            P.op("pe", lambda e, ts=ts: e.matmul(out=psG[:, 0:DR], lhsT=sgb[:, ts], rhs=gupb[:], start=True, stop=True), [sgb, gupb], [psG])
            P.op("dve", lambda e, ya=ya, yb=yb: e.tensor_tensor(out=ya[:], in0=ya[:], in1=yb[:], op=ALU.add), [ya, yb], [ya])
            P.op("dve", lambda e, ya=ya: e.tensor_reduce(out=st1[:], in_=v38(ya[:]), axis=AX.X, op=ALU.add), [ya], [st1])
            P.op("act", lambda e, ya=ya: e.activation(out=ysq[:], in_=ya[:], func=AF.Square), [ya], [ysq])
            P.op("dve", lambda e: e.tensor_reduce(out=st2[:], in_=v38(ysq[:]), axis=AX.X, op=ALU.add), [ysq], [st2])
            P.op("dve", lambda e: e.tensor_scalar(out=st1[:], in0=st1[:], scalar1=1.0 / HD, scalar2=None, op0=ALU.mult), [st1], [st1])
            P.op("dve", lambda e: e.tensor_tensor(out=st3[:], in0=st1[:], in1=st1[:], op=ALU.mult), [st1], [st3])
            P.op("dve", lambda e: e.scalar_tensor_tensor(out=st2[:], in0=st2[:], scalar=1.0 / HD, in1=st3[:], op0=ALU.mult, op1=ALU.subtract), [st2, st3], [st2])
            P.op("act", lambda e: e.activation(out=st2[:], in_=st2[:], func=AF.Sqrt, bias=epsg[:]), [st2, epsg], [st2])
            P.op("dve", lambda e: e.reciprocal(out=st2[:], in_=st2[:]), [st2], [st2])
            P.op("dve", lambda e, ya=ya: e.tensor_tensor(out=v38(ya[:]), in0=v38(ya[:]), in1=st1[:].unsqueeze(2).to_broadcast([128, NH, HD]), op=ALU.subtract), [ya, st1], [ya])
            P.op("dve", lambda e, ya=ya: e.tensor_tensor(out=v38(ya[:]), in0=v38(ya[:]), in1=st2[:].unsqueeze(2).to_broadcast([128, NH, HD]), op=ALU.mult), [ya, st2], [ya])
            P.op("pool", lambda e, ya=ya: e.tensor_tensor(out=ya[:], in0=ya[:], in1=gng[:], op=ALU.mult), [ya, gng], [ya])
            P.op("pool", lambda e, ya=ya: e.tensor_tensor(out=ya[:], in0=ya[:], in1=gnb[:], op=ALU.add), [ya, gnb], [ya])
            P.op("act", lambda e: e.activation(out=rks[:], in_=psR[:, 0:8], func=AF.Copy), [psR], [rks])
            P.op("dve", lambda e: e.tensor_tensor(out=v38(bon[:]), in0=v38(psV[:, 0:DR]), in1=rks[:].unsqueeze(2).to_broadcast([128, NH, HD]), op=ALU.mult), [psV, rks], [bon])
            P.op("pool", lambda e, ya=ya: e.tensor_tensor(out=ya[:], in0=ya[:], in1=bon[:], op=ALU.add), [ya, bon], [ya])
            P.op("dve", lambda e, ya=ya: e.tensor_tensor(out=ya[:], in0=ya[:], in1=psG[:, 0:DR], op=ALU.mult), [ya, psG], [ya])
            for kc in range(4):
                P.op("pe", lambda e, kc=kc, ya=ya: e.transpose(out=psAx[:, kc * 128:(kc + 1) * 128], in_=ya[:, kc * 128:(kc + 1) * 128], identity=identf), [ya, cst], [psAx])
            P.op("act", lambda e: e.activation(out=axT[:].rearrange("p k t -> p (k t)"), in_=psAx[:], func=AF.Copy), [psAx], [axT])
            for hf, po_ in ((0, psO0), (1, psO1)):
                for kc in range(8):
                    lhs = axT[:, kc, :] if kc < 4 else bxT[:, kc - 4, t0:t0 + 128]
                    rdl = [axT] if kc < 4 else [(bxT, kc - 4)]
                    P.op("pe", lambda e, lhs=lhs, kc=kc, hf=hf, po_=po_: e.matmul(out=po_[:], lhsT=lhs, rhs=woutb[:, kc, hf * 512:(hf + 1) * 512], start=(kc == 0), stop=(kc == 7)), rdl + [woutb], [po_])
                P.op("dve", lambda e, hf=hf, po_=po_: e.tensor_tensor(out=x1t[:, hf * 512:(hf + 1) * 512], in0=po_[:], in1=g1bc[:, hf * 512:(hf + 1) * 512], op=ALU.mult), [po_, g1bc], [(x1t, hf)])
            P.op("pool", lambda e, xr_=xr_: e.tensor_tensor(out=x1t[:], in0=x1t[:], in1=xr_[:], op=ALU.add), [x1t, xr_], [x1t])
            P.dma("sp", X1.a[t0:t0 + 128, :], x1t[:], reads=[x1t], writes=[(X1, tt)])
            P.op("act", lambda e: e.activation(out=junk2[:], in_=x1t[:], func=AF.Square, accum_out=ssq2[:]), [x1t], [junk2, ssq2])
            P.op("act", lambda e: e.activation(out=std2[:], in_=ssq2[:], func=AF.Sqrt, bias=epsn[:], scale=1.0 / D), [ssq2, epsn], [std2])
            P.op("dve", lambda e: e.reciprocal(out=rstd2[:], in_=std2[:]), [std2], [rstd2])
            P.op("dve", lambda e: e.tensor_scalar(out=xh2f[:], in0=x1t[:], scalar1=rstd2[:, 0:1], scalar2=None, op0=ALU.mult), [x1t, rstd2], [xh2f])
            P.op("pool", lambda e: e.tensor_copy(out=xh2b[:], in_=xh2f[:]), [xh2f], [xh2b])
            P.dma("pool", XH2.a[t0:t0 + 128, :], xh2b[:], reads=[xh2b], writes=[(XH2, tt)])
            for k in range(8):
                pz = psH if k < 4 else psAx
                P.op("pe", lambda e, k=k, pz=pz: e.transpose(out=pz[:, (k % 4) * 128:(k % 4 + 1) * 128], in_=xh2f[:, k * 128:(k + 1) * 128], identity=identf), [xh2f, cst], [pz])
            for k in range(8):
                pz = psH if k < 4 else psAx
                P.op("act" if k >= 4 else "dve", (lambda e, k=k, pz=pz: e.activation(out=hxT[:, k, :], in_=pz[:, (k % 4) * 128:(k % 4 + 1) * 128], func=AF.Identity, bias=modT[:, 24 + k, 0:1], scale=sc2[:, k:k + 1])) if k >= 4 else (lambda e, k=k, pz=pz: e.tensor_scalar(out=hxT[:, k, :], in0=pz[:, (k % 4) * 128:(k % 4 + 1) * 128], scalar1=sc2[:, k:k + 1], scalar2=modT[:, 24 + k, 0:1], op0=ALU.mult, op1=ALU.add)), [pz, modT, sc2], [(hxT, k)])
            for k in range(8):
                P.op("pe", lambda e, k=k: e.matmul(out=psL[:, 0:NE], lhsT=hxT[:, k, :], rhs=rwf[:, k, :], start=(k == 0), stop=(k == 7)), [(hxT, k), rwf], [psL])
            P.op("dve", lambda e: e.tensor_reduce(out=lmx[:], in_=psL[:, 0:NE], axis=AX.X, op=ALU.max, negate=True), [psL], [lmx])
            P.op("act", lambda e: e.activation(out=lex[:], in_=psL[:, 0:NE], func=AF.Exp, bias=lmx[:], accum_out=lsum[:]), [psL, lmx], [lex, lsum])
            P.op("dve", lambda e: e.reciprocal(out=lsum[:], in_=lsum[:]), [lsum], [lsum])
            P.op("dve", lambda e, tt=tt: e.tensor_scalar(out=affall[:, tt, :], in0=lex[:], scalar1=lsum[:, 0:1], scalar2=None, op0=ALU.mult), [lex, lsum], [(affall, tt)])
    if debug:
        P.dma("sp", dbg["aff"].a, affall[:].rearrange("p t e -> p (t e)"), reads=[affall], writes=[dbg["aff"]])


    if stop == 'D':
        return fin()
    P.pop()
    P.pop()
    P.push()
    NTE = NTX * NE
    idxi_all = P.sb([128, NE, NST], I32, "idxi_all")
    valt_all = P.sb([128, NE, NST], F32, "valt_all")
    P.push()
    tokhl = P.sb([128, NTX, 2], F32, "tokhl")
    iot = P.sb([128, 512], F32, "iot")
    P.dma("sp", iot[:], iota_d.a[:, 0:512], reads=[iota_d], writes=[iot])
    P.dma("sp", tokhl[:].rearrange("p t c -> p (t c)"), tokhl_d.a, reads=[tokhl_d], writes=[tokhl])
    lo = P.sb([128, NE], F32, "lo")
    mid = P.sb([128, NE], F32, "mid")
    ge = P.sb([128, NE], F32, "ge")
    cntp = P.sb([128, NE], F32, "cntp")
    cmpt = P.sb([128, NTX, NE], F32, "cmpt")
    maskb = P.sb([128, NTX, NE], BF16, "maskb")
    wsel = P.sb([128, NTX, NE], F32, "wsel")
    offs = P.sb([128, NTX, NE], F32, "offs")
    pos = P.sb([128, NTX, NE], F32, "pos")
    P.op("dve", lambda e: e.memset(lo[:], 0.0), [], [lo])
    for k in range(30):
        h = 2.0 ** -(k + 1)
        P.op("dve", lambda e, h=h: e.tensor_scalar(out=mid[:], in0=lo[:], scalar1=h, scalar2=None, op0=ALU.add), [lo], [mid])
        P.op("dve", lambda e: e.tensor_tensor(out=cmpt[:], in0=affall[:], in1=mid[:].unsqueeze(1).to_broadcast([128, NTX, NE]), op=ALU.is_ge), [affall, mid], [cmpt])
        P.op("dve", lambda e: e.tensor_reduce(out=cntp[:], in_=cmpt[:].rearrange("p t e -> p e t"), axis=AX.X, op=ALU.add), [cmpt], [cntp])
        P.op("pe", lambda e: e.matmul(out=psL[:, 0:NE], lhsT=cst[:, 1, :], rhs=cntp[:], start=True, stop=True), [cst, cntp], [psL])
        P.op("dve", lambda e, h=h: e.tensor_scalar(out=ge[:], in0=psL[:, 0:NE], scalar1=float(CAP) - 0.5, scalar2=h, op0=ALU.is_ge, op1=ALU.mult), [psL], [ge])
        P.op("dve", lambda e: e.tensor_tensor(out=lo[:], in0=lo[:], in1=ge[:], op=ALU.add), [lo, ge], [lo])
    P.op("dve", lambda e: e.tensor_tensor(out=cmpt[:], in0=affall[:], in1=lo[:].unsqueeze(1).to_broadcast([128, NTX, NE]), op=ALU.is_ge), [affall, lo], [cmpt])
    P.op("pool", lambda e: e.tensor_copy(out=maskb[:], in_=cmpt[:]), [cmpt], [maskb])
    P.op("dve", lambda e: e.tensor_tensor(out=wsel[:], in0=cmpt[:], in1=affall[:], op=ALU.mult), [cmpt, affall], [wsel])
    P.op("pe", lambda e: e.matmul(out=psV[:, 0:NTE], lhsT=cstb[:, 8, :], rhs=maskb[:].rearrange("p t e -> p (t e)"), start=True, stop=True), [cstb, maskb], [psV])
    P.op("pe", lambda e: e.matmul(out=psG[:, 0:NTE], lhsT=onesb, rhs=maskb[:].rearrange("p t e -> p (t e)"), start=True, stop=True), [cstb, maskb], [psG])
    P.op("dve", lambda e: e.memset(offs[:, 0, :], 0.0), [], [offs])
    for tt in range(1, NTX):
        P.op("dve", lambda e, tt=tt: e.tensor_tensor(out=offs[:, tt, :], in0=offs[:, tt - 1, :], in1=psG[:, (tt - 1) * NE:tt * NE], op=ALU.add), [offs, psG], [offs])
    P.op("dve", lambda e: e.tensor_tensor(out=pos[:].rearrange("p t e -> p (t e)"), in0=offs[:].rearrange("p t e -> p (t e)"), in1=psV[:, 0:NTE], op=ALU.add), [offs, psV], [pos])
    OH2 = [P.sb([128, NTX, CAP], BF16, f"OH{i}") for i in range(2)]
    rhsE2 = [P.sb([128, NTX, 4], BF16, f"rhsE{i}") for i in range(2)]
    for rhsE in rhsE2:
        P.op("pool", lambda e, rhsE=rhsE: e.tensor_copy(out=rhsE[:, :, 0:2], in_=tokhl[:]), [tokhl], [rhsE])
    idxs = P.sb([128, 4 * NST], F32, "idxs")
    idxf = P.sb([128, NST], F32, "idxf")
    for ex in range(NE):
        OH = OH2[ex % 2]
        rhsE = rhsE2[ex % 2]
        P.op("dve", lambda e, ex=ex, rhsE=rhsE: e.tensor_copy(out=rhsE[:, :, 2], in_=wsel[:, :, ex]), [wsel], [rhsE])
        P.op("dve", lambda e, ex=ex, rhsE=rhsE: e.tensor_tensor(out=rhsE[:, :, 3], in0=wsel[:, :, ex], in1=rhsE[:, :, 2], op=ALU.subtract), [wsel, rhsE], [rhsE])
        for tt in range(NTX):
            P.op("dve" if tt % 4 else "pool", lambda e, tt=tt, ex=ex, OH=OH: e.tensor_scalar(out=OH[:, tt, :], in0=iot[:, 0:CAP], scalar1=pos[:, tt, ex:ex + 1], scalar2=cmpt[:, tt, ex:ex + 1], op0=ALU.is_equal, op1=ALU.mult), [iot, pos, cmpt], [(OH, tt)])
        for j in range(NST):
            for tt in range(NTX):
                P.op("pe", lambda e, j=j, tt=tt, OH=OH, rhsE=rhsE: e.matmul(out=psR[0:ST, 4 * j:4 * j + 4], lhsT=OH[:, tt, j * ST:(j + 1) * ST], rhs=rhsE[:, tt, :], start=(tt == 0), stop=(tt == NTX - 1)), [(OH, tt), rhsE], [psR])
        P.op("act", lambda e: e.activation(out=idxs[0:ST, :], in_=psR[0:ST, 0:4 * NST], func=AF.Copy), [psR], [idxs])
        i3 = idxs[0:ST, :].rearrange("p (j c) -> p j c", c=4)
        P.op("dve", lambda e, i3=i3: e.scalar_tensor_tensor(out=idxf[0:ST, :], in0=i3[:, :, 0], scalar=128.0, in1=i3[:, :, 1], op0=ALU.mult, op1=ALU.add), [idxs], [idxf])
        P.op("dve", lambda e, ex=ex: e.tensor_copy(out=idxi_all[0:ST, ex, :], in_=idxf[0:ST, :]), [idxf], [(idxi_all, ex)])
        P.op("dve", lambda e, i3=i3, ex=ex: e.tensor_tensor(out=valt_all[0:ST, ex, :], in0=i3[:, :, 2], in1=i3[:, :, 3], op=ALU.add), [idxs], [(valt_all, ex)])
    if stop == 'E1':
        return fin()
    P.pop()
    P.push()
    Xg = [P.sb([128, NST, D], BF16, f"Xg{i}") for i in range(2)]
    XeT = P.sb([128, 8, CAP], BF16, "XeT")
    hidT = P.sb([128, 8, CAP], BF16, "hidT")
    sgt = P.sb([128, CAP], F32, "sgt")
    NSTG = 4
    wstg = [P.sb([128, 2, D], F32, f"wstg{i}") for i in range(NSTG)]
    Wg2 = [P.sb([128, 8, D], BF16, f"Wg{i}") for i in range(2)]
    Wu2 = [P.sb([128, 8, D], BF16, f"Wu{i}") for i in range(2)]
    Wd1 = P.sb([128, 8, D], BF16, "Wd")
    rows = [P.sb([128, D], F32, f"rows{i}") for i in range(3)]
    wsrc = [wg_d, wu_d, wd_d]
    stg_c = [0]
    sc_i = 0

    def load_w(ex, which=(0, 1, 2)):
        for wi, dst in ((0, Wg2[ex % 2]), (1, Wu2[ex % 2]), (2, Wd1)):
            if wi not in which:
                continue
            wv = wsrc[wi].a[ex].rearrange("(k p) c -> p k c", p=128)
            for hq in range(4):
                stg = wstg[stg_c[0] % NSTG]
                stg_c[0] += 1
                P.dma("sp", stg[:], wv[:, hq * 2:(hq + 1) * 2, :], reads=[wsrc[wi]], writes=[stg])
                ce_ = ("pool", "act", "dve")[stg_c[0] % 3]
                if ce_ == "act":
                    P.op("act", lambda e, stg=stg, dst=dst, hq=hq: e.activation(out=dst[:, hq * 2:(hq + 1) * 2, :], in_=stg[:], func=AF.Copy), [stg], [(dst, hq // 2)])
                else:
                    P.op(ce_, lambda e, stg=stg, dst=dst, hq=hq: e.tensor_copy(out=dst[:, hq * 2:(hq + 1) * 2, :], in_=stg[:]), [stg], [(dst, hq // 2)])

    def gather(ex):
        for j in range(NST):
            P.op("pool", lambda e, j=j, ex=ex: e.indirect_dma_start(out=Xg[ex % 2][0:ST, j, :], out_offset=None, in_=XH2.a, in_offset=IOA(ap=idxi_all[0:ST, ex, j:j + 1], axis=0)), [XH2, (idxi_all, ex)], [(Xg[ex % 2], j)], dma=True)

    gather(0)
    load_w(0)
    for ex in range(NE):
        xg = Xg[ex % 2]
        Wg_, Wu_ = Wg2[ex % 2], Wu2[ex % 2]
        LA_, LB_ = [], []
        if ex + 1 < NE:
            gather(ex + 1)
            P.defer = LA_
            load_w(ex + 1, (0, 1))
        P.defer = LB_
        for k in range(8):
            pz = (psV, psG)[k % 2]
            for j in range(NST):
                P.op("pe", lambda e, k=k, j=j, pz=pz, xg=xg: e.matmul(out=pz[:, j * ST:(j + 1) * ST], lhsT=xg[0:ST, j, k * 128:(k + 1) * 128], rhs=cstb[0:ST, 0, 0:ST], start=True, stop=True), [(xg, j), cstb], [pz])
            P.op("dve" if k % 2 else "act", (lambda e, k=k, pz=pz: e.tensor_scalar(out=XeT[:, k, :], in0=pz[:, 0:CAP], scalar1=sc2[:, k:k + 1], scalar2=modT[:, 24 + k, 0:1], op0=ALU.mult, op1=ALU.add)) if k % 2 else (lambda e, k=k, pz=pz: e.activation(out=XeT[:, k, :], in_=pz[:, 0:CAP], func=AF.Identity, bias=modT[:, 24 + k, 0:1], scale=sc2[:, k:k + 1])), [pz, sc2, modT], [(XeT, k)])
        for fc in range(8):
            for k in range(8):
                P.op("pe", lambda e, fc=fc, k=k, Wg_=Wg_: e.matmul(out=psAx[:, 0:CAP], lhsT=Wg_[:, k, fc * 128:(fc + 1) * 128], rhs=XeT[:, k, :], start=(k == 0), stop=(k == 7)), [(Wg_, k // 4), (XeT, k)], [psAx])
            for k in range(8):
                P.op("pe", lambda e, fc=fc, k=k, Wu_=Wu_: e.matmul(out=psH[:, 0:CAP], lhsT=Wu_[:, k, fc * 128:(fc + 1) * 128], rhs=XeT[:, k, :], start=(k == 0), stop=(k == 7)), [(Wu_, k // 4), (XeT, k)], [psH])
            P.op("act", lambda e: e.activation(out=sgt[:], in_=psAx[:, 0:CAP], func=AF.Silu), [psAx], [sgt])
            P.op("dve", lambda e, fc=fc: e.tensor_tensor(out=hidT[:, fc, :], in0=sgt[:], in1=psH[:, 0:CAP], op=ALU.mult), [sgt, psH], [(hidT, fc)])
        for j in range(NST):
            rw_ = rows[sc_i % 3]
            sc_i += 1
            for hf, po_ in ((0, psO0), (1, psO1)):
                for fc in range(8):
                    P.op("pe", lambda e, j=j, hf=hf, fc=fc, po_=po_: e.matmul(out=po_[0:ST, :], lhsT=hidT[:, fc, j * ST:(j + 1) * ST], rhs=Wd1[:, fc, hf * 512:(hf + 1) * 512], start=(fc == 0), stop=(fc == 7)), [(hidT, fc), (Wd1, fc // 4)], [po_])
                P.op("dve", lambda e, j=j, hf=hf, po_=po_, rw_=rw_, ex=ex: e.scalar_tensor_tensor(out=rw_[0:ST, hf * 512:(hf + 1) * 512], in0=po_[0:ST, :], scalar=valt_all[0:ST, ex, j:j + 1], in1=g2bc[0:ST, hf * 512:(hf + 1) * 512], op0=ALU.mult, op1=ALU.mult), [po_, (valt_all, ex), g2bc], [(rw_, hf)])
            P.op("pool", lambda e, j=j, ex=ex, rw_=rw_: e.indirect_dma_start(out=X1.a, out_offset=IOA(ap=idxi_all[0:ST, ex, j:j + 1], axis=0), in_=rw_[0:ST, :], in_offset=None, compute_op=ALU.add), [rw_, (idxi_all, ex)], [X1], dma=True)
        P.defer = None
        merge(LB_, LA_)
        if ex + 1 < NE:
            load_w(ex + 1, (2,))
    P.pop()

    if stop == 'E':
        return fin()
    P.pop()
    P.push()
    fx = [P.sb([128, D], F32, f"fx{i}") for i in range(2)]
    fo = [P.sb([128, D], F32, f"fo{i}") for i in range(2)]
    fs = P.sb([128, 1], F32, "fs")
    fd = P.sb([128, 1], F32, "fd")
    fr = P.sb([128, 1], F32, "fr")
    for tt in range(NTX):
        t0 = tt * 128
        a, o_ = fx[tt % 2], fo[tt % 2]
        P.dma("sp", a[:], X1.a[t0:t0 + 128, :], reads=[X1], writes=[a])
        P.op("act", lambda e, a=a: e.activation(out=junk2[:], in_=a[:], func=AF.Square, accum_out=fs[:]), [a], [junk2, fs])
        P.op("act", lambda e: e.activation(out=fd[:], in_=fs[:], func=AF.Sqrt, bias=epsn[:], scale=1.0 / D), [fs, epsn], [fd])
        P.op("dve", lambda e: e.reciprocal(out=fr[:], in_=fd[:]), [fd], [fr])
        P.op("dve", lambda e, a=a, o_=o_: e.scalar_tensor_tensor(out=o_[:], in0=a[:], scalar=fr[:, 0:1], in1=fgbc[:], op0=ALU.mult, op1=ALU.mult), [a, fr, fgbc], [o_])
        P.dma("pool", out_d.a[t0:t0 + 128, :], o_[:], reads=[o_], writes=[(out_d, tt)])

    P.pop()
    stats = P.emit()
    return nc, stats


def host_consts():
    c = np.zeros((128, 9, 128), np.float32)
    i = np.arange(128)
    c[:, 0, :] = np.eye(128)
    c[:, 1, :] = 1.0
    c[:, 2, :] = (i[:, None] // 64 == i[None, :] // 64)
    a, b = i[:, None] % 64, i[None, :] % 64
    c[:, 3, :] = a > b
    c[:, 4, :] = a < b
    c[:, 5, :] = a <= b
    c[:, 6, :] = a >= b
    c[:, 7, :] = (i[:, None] // 64 == i[None, :] // 64)
    c[:, 8, :] = i[:, None] < i[None, :]
    return c


def col_perm():
    cols = []
    for Q in range(3):
        for q in range(4):
            cols += [Q * 512 + 4 * j + q for j in range(128)]
    for qa in (0, 2):
        cols += [1536 + 4 * j + qa for j in range(64)]
        cols += [1536 + 4 * j + qa + 1 for j in range(64)]
    for i in range(4):
        pass
    cols += list(range(1792, 3328))
    return np.array(cols)


def prep_core(inp, b):
    f = lambda a: np.ascontiguousarray(a, dtype=np.float32)
    perm = col_perm()
    m = {}
    m["x"] = f(inp["x"][b])
    m["ctx"] = f(inp["ctx"][b])
    cT = np.zeros((128, 16), np.float32)
    cT[:, 0:8] = inp["c"][b].reshape(8, 128).T
    cT[:, 8:16] = inp["c_ctx"].reshape(8, 128).T
    m["cT"] = cT
    m["ada_w"] = f(inp["ada_w"][0])
    m["adabT"] = f(inp["ada_b"][0].reshape(48, 128).T)
    gnT = np.zeros((128, 24), np.float32)
    gnT[:, 0:8] = inp["norm1_g"][0].reshape(8, 128).T
    gnT[:, 8:16] = inp["norm2_g"][0].reshape(8, 128).T
    m["gnT"] = gnT
    m["final_g"] = f(inp["final_g"].reshape(1, D))
    m["w_in_p"] = f(inp["w_in"][0][:, perm])
    mu = inp["shift_mu"][0]
    m["muT"] = f(mu[perm[:1792]].reshape(14, 128).T)
    lw = np.zeros((2, 128, DR), np.float32)
    lw[:, 0:64] = inp["w_lora_up"][0]
    lw[:, 64:128] = inp["a_lora_up"][0]
    m["loraW"] = lw
    wa0 = np.zeros((128, 2, 2, 4), np.float32)
    for d in range(2):
        wa0[:, d, 0, :] = inp["w0"][0, d].reshape(4, 128).T
        wa0[:, d, 1, :] = inp["a0"][0, d].reshape(4, 128).T
    m["wa0T"] = wa0
    kv = np.zeros((128, 3, 4), np.float32)
    kv[:, 0, :] = inp["k_k"][0].reshape(4, 128).T
    kv[:, 1, :] = inp["k_a"][0].reshape(4, 128).T
    kv[:, 2, :] = inp["r_k"][0].reshape(4, 128).T
    m["kvecT"] = kv
    m["g_up"] = f(inp["g_lora_up"][0])
    m["gnrow"] = f(np.stack([inp["gn_g"][0], inp["gn_b"][0]]))
    m["cwT"] = f(inp["conv_w"][0].reshape(3, 4, 128).transpose(2, 1, 0))
    m["w_out"] = f(inp["w_out"][0])
    m["router_w"] = f(inp["router_w"][0])
    m["exp_w_gate"] = f(inp["exp_w_gate"][0])
    m["exp_w_up"] = f(inp["exp_w_up"][0])
    m["exp_w_down"] = f(inp["exp_w_down"][0])
    m["consts"] = host_consts()
    io = np.zeros((128, 514), np.float32)
    io[:, 0:512] = np.arange(512)[None, :]
    m["iotas"] = io
    rm = np.ones((128, 512), np.float32)
    rm[:, ::64] = 0.0
    m["rmask"] = rm
    ntx = m["x"].shape[0] // 128
    th = np.zeros((128, ntx, 2), np.float32)
    th[:, :, 0] = np.arange(ntx)[None, :]
    th[:, :, 1] = np.arange(128)[:, None]
    m["tokhl"] = th.reshape(128, ntx * 2)
    return m


def kernel(**inputs):
    nc, _ = build()
    in_maps = [prep_core(inputs, b) for b in range(8)]
    res = run_bass_kernel_spmd(nc, in_maps, core_ids=list(range(8)))
    return np.stack([np.asarray(r["out"], dtype=np.float32) for r in res.results], axis=0)
```
